# Optimizing a Trainium2 kernel written in Bass

```python
import math
import jax, jax.numpy as jnp
from jax import lax
import numpy as np

D_MODEL = 2048
BATCH = 8
SEQ = 2048
DEPTH = 2

MEM_LEN = 256
NORM_EPS = 1e-6
RWKV_WIDTH = 1024
RWKV_HEAD = 64
RWKV_HEADS = RWKV_WIDTH // RWKV_HEAD
DECAY_RANK = 64
ICLR_RANK = 64
GN_EPS = 64e-5
SWA_HEAD = 64
SWA_Q_HEADS = 16
SWA_KV_HEADS = 2
SWA_WIDTH = SWA_Q_HEADS * SWA_HEAD
WINDOW = 128
BLOCK = 128
XA_HEADS = 4
XA_HEAD = 256
XA_WIDTH = XA_HEADS * XA_HEAD
N_BRANCH = 3

SHIFT_COLS = 3 * RWKV_WIDTH + DECAY_RANK + ICLR_RANK
COL_SIZES = (
    SHIFT_COLS,
    RWKV_WIDTH,
    SWA_WIDTH,
    2 * SWA_KV_HEADS * SWA_HEAD,
    SWA_WIDTH,
    XA_WIDTH,
    XA_WIDTH,
    N_BRANCH * D_MODEL,
)
D_IN = SHIFT_COLS + RWKV_WIDTH + 2 * SWA_WIDTH + 2 * SWA_KV_HEADS * SWA_HEAD + 2 * XA_WIDTH + N_BRANCH * D_MODEL

kernel_name = "hybrid_rwkv7_swa_sink_memxattn_gated"


def _split(p, sizes):
    idx = np.cumsum(np.array(sizes))[:-1].tolist()
    return jnp.split(p, idx, axis=-1)


def rmsnorm(x, g):
    xf = x.astype(jnp.float32)
    y = xf * lax.rsqrt(jnp.mean(xf * xf, axis=-1, keepdims=True) + NORM_EPS)
    return (y * g.astype(jnp.float32)).astype(x.dtype)


def rwkv7_time_mix(p, mu, decay_base, decay_up, iclr_base, iclr_up, k_k, k_a, r_k, gn_w, gn_b):
    B, T, _ = p.shape
    f32 = jnp.float32
    H, N = RWKV_HEADS, RWKV_HEAD
    prev = jnp.pad(p, ((0, 0), (1, 0), (0, 0)))[:, :-1]
    ps = p + mu * (prev - p)
    r, k, v, wd, ad = _split(ps, (RWKV_WIDTH, RWKV_WIDTH, RWKV_WIDTH, DECAY_RANK, ICLR_RANK))
    w_log = -jax.nn.softplus(-(decay_base + jnp.tanh(wd) @ decay_up).astype(f32)) - 0.5
    decay = jnp.exp(-jnp.exp(w_log))
    a = jax.nn.sigmoid((iclr_base + ad @ iclr_up).astype(f32))
    hs = lambda t: t.astype(f32).reshape(B, T, H, N)
    r, k, v, a, decay = hs(r), hs(k), hs(v), hs(a), hs(decay)
    kk = k * k_k.astype(f32).reshape(H, N)
    kk = kk / jnp.maximum(jnp.sqrt(jnp.sum(kk * kk, axis=-1, keepdims=True)), 1e-12)
    k = k * (1.0 + (a - 1.0) * k_a.astype(f32).reshape(H, N))

    def step(S, inp):
        r_t, w_t, k_t, v_t, kk_t, a_t = inp
        sa = jnp.einsum('bhvk,bhk->bhv', S, -kk_t)
        S = (S * w_t[:, :, None, :] + sa[..., None] * (kk_t * a_t)[:, :, None, :]
             + v_t[..., None] * k_t[:, :, None, :])
        return S, jnp.einsum('bhvk,bhk->bhv', S, r_t)

    seq = tuple(jnp.moveaxis(t, 1, 0) for t in (r, decay, k, v, kk, a))
    S0 = jnp.zeros((B, H, N, N), f32)
    _, y = lax.scan(step, S0, seq)
    y = jnp.moveaxis(y, 0, 1)
    mean = jnp.mean(y, axis=-1, keepdims=True)
    var = jnp.mean(jnp.square(y - mean), axis=-1, keepdims=True)
    y = ((y - mean) * lax.rsqrt(var + GN_EPS)).reshape(B, T, RWKV_WIDTH)
    y = y * gn_w.astype(f32) + gn_b.astype(f32)
    bonus = jnp.sum(r * k * r_k.astype(f32), axis=-1, keepdims=True) * v
    return (y + bonus.reshape(B, T, RWKV_WIDTH)).astype(p.dtype)


def sliding_window_gqa_sinks(q, kv, sinks):
    B, T, _ = q.shape
    n = T // BLOCK
    G = SWA_Q_HEADS // SWA_KV_HEADS
    f32 = jnp.float32
    q = q.reshape(B, n, BLOCK, SWA_KV_HEADS, G, SWA_HEAD)
    k, v = jnp.split(kv, 2, axis=-1)
    k = k.reshape(B, n, BLOCK, SWA_KV_HEADS, SWA_HEAD)
    v = v.reshape(B, n, BLOCK, SWA_KV_HEADS, SWA_HEAD)

    def band(t):
        prev = jnp.pad(t, ((0, 0), (1, 0), (0, 0), (0, 0), (0, 0)))[:, :-1]
        return jnp.concatenate([prev, t], axis=2)

    kw, vw = band(k), band(v)
    s = jnp.einsum('bnqhgd,bnkhd->bnhgqk', q, kw).astype(f32) * (SWA_HEAD ** -0.5)
    blk = jnp.arange(n)[:, None, None]
    qpos = blk * BLOCK + jnp.arange(BLOCK)[None, :, None]
    kpos = (blk - 1) * BLOCK + jnp.arange(2 * BLOCK)[None, None, :]
    rel = qpos - kpos
    mask = (rel >= 0) & (rel < WINDOW) & (kpos >= 0)
    s = jnp.where(mask[None, :, None, None], s, -jnp.inf)
    sink = sinks.astype(f32).reshape(SWA_KV_HEADS, G)[None, None, :, :, None, None]
    m = jnp.maximum(jnp.max(s, axis=-1, keepdims=True), sink)
    e = jnp.exp(s - m)
    p = e / (jnp.sum(e, axis=-1, keepdims=True) + jnp.exp(sink - m))
    o = jnp.einsum('bnhgqk,bnkhd->bnqhgd', p.astype(vw.dtype), vw)
    return o.reshape(B, T, SWA_WIDTH)


def memory_cross_attention(q, mem_n, w_mem_kv):
    B, T, _ = q.shape
    M = mem_n.shape[1]
    q = q.reshape(B, T, XA_HEADS, XA_HEAD)
    k, v = jnp.split(mem_n @ w_mem_kv, 2, axis=-1)
    k = k.reshape(B, M, XA_HEADS, XA_HEAD)
    v = v.reshape(B, M, XA_HEADS, XA_HEAD)
    s = jnp.einsum('bthd,bmhd->bhtm', q, k).astype(jnp.float32) * (XA_HEAD ** -0.5)
    p = jax.nn.softmax(s, axis=-1)
    o = jnp.einsum('bhtm,bmhd->bthd', p.astype(v.dtype), v)
    return o.reshape(B, T, XA_WIDTH)


def setup_inputs(seed: int = 0) -> dict:
    key = jax.random.key(seed)
    ks = jax.random.split(key, 24)
    f32 = jnp.float32
    nrm = lambda k, shape, s: jax.random.normal(k, shape, f32) * s
    L, D = DEPTH, D_MODEL
    return {
        "x": nrm(ks[0], (BATCH, SEQ, D), 1.0),
        "mem": nrm(ks[1], (BATCH, MEM_LEN, D), 1.0),
        "g_pre": 1.0 + nrm(ks[2], (L, D), 0.02),
        "w_in": nrm(ks[3], (L, D, D_IN), D ** -0.5),
        "mu_shift": jax.random.uniform(ks[4], (L, SHIFT_COLS), f32, 0.0, 1.0),
        "decay_base": jax.random.uniform(ks[5], (L, RWKV_WIDTH), f32, -6.0, 1.0),
        "decay_up": nrm(ks[6], (L, DECAY_RANK, RWKV_WIDTH), 0.1),
        "iclr_base": nrm(ks[7], (L, RWKV_WIDTH), 0.1),
        "iclr_up": nrm(ks[8], (L, ICLR_RANK, RWKV_WIDTH), 0.1),
        "k_k": 0.85 + nrm(ks[9], (L, RWKV_WIDTH), 0.02),
        "k_a": 1.0 + nrm(ks[10], (L, RWKV_WIDTH), 0.02),
        "r_k": nrm(ks[11], (L, RWKV_HEADS, RWKV_HEAD), 0.1),
        "gn_w": 1.0 + nrm(ks[12], (L, RWKV_WIDTH), 0.02),
        "gn_b": nrm(ks[13], (L, RWKV_WIDTH), 0.02),
        "attn_sinks": nrm(ks[14], (L, SWA_Q_HEADS), 0.5),
        "g_mem": 1.0 + nrm(ks[15], (L, D), 0.02),
        "w_mem_kv": nrm(ks[16], (L, D, 2 * XA_WIDTH), D ** -0.5),
        "w_up_rwkv": nrm(ks[17], (L, RWKV_WIDTH, D), RWKV_WIDTH ** -0.5),
        "w_up_swa": nrm(ks[18], (L, SWA_WIDTH, D), SWA_WIDTH ** -0.5),
        "w_up_xattn": nrm(ks[19], (L, XA_WIDTH, D), XA_WIDTH ** -0.5),
        "w_out": nrm(ks[20], (L, D, D), D ** -0.5),
        "g_post": 1.0 + nrm(ks[21], (L, D), 0.02),
    }


def reference(x, mem, g_pre, w_in, mu_shift, decay_base, decay_up, iclr_base, iclr_up,
              k_k, k_a, r_k, gn_w, gn_b, attn_sinks, g_mem, w_mem_kv,
              w_up_rwkv, w_up_swa, w_up_xattn, w_out, g_post):
    B, T, D = x.shape
    for l in range(DEPTH):
        h = rmsnorm(x, g_pre[l])
        p = h @ w_in[l]
        (c_rwkv, c_rwkv_gate, c_swa_q, c_swa_kv, c_swa_gate,
         c_xa_q, c_xa_gate, c_merge) = _split(p, COL_SIZES)
        y_a = rwkv7_time_mix(c_rwkv, mu_shift[l], decay_base[l], decay_up[l], iclr_base[l],
                             iclr_up[l], k_k[l], k_a[l], r_k[l], gn_w[l], gn_b[l])
        y_a = y_a * jax.nn.silu(c_rwkv_gate)
        y_b = sliding_window_gqa_sinks(c_swa_q, c_swa_kv, attn_sinks[l]) * jax.nn.silu(c_swa_gate)
        mem_n = rmsnorm(mem, g_mem[l])
        y_c = memory_cross_attention(c_xa_q, mem_n, w_mem_kv[l]) * jax.nn.silu(c_xa_gate)
        gates = jax.nn.sigmoid(c_merge.astype(jnp.float32)).astype(x.dtype).reshape(B, T, N_BRANCH, D)
        merged = (gates[:, :, 0] * (y_a @ w_up_rwkv[l])
                  + gates[:, :, 1] * (y_b @ w_up_swa[l])
                  + gates[:, :, 2] * (y_c @ w_up_xattn[l]))
        o = merged @ w_out[l]
        x = x + rmsnorm(o, g_post[l])
    return x
```

```python
import contextlib
import numpy as np
import concourse.bass as bass
import concourse.mybir as mybir
from concourse.bass_utils import run_bass_kernel_spmd

F32 = mybir.dt.float32
BF16 = mybir.dt.bfloat16
AF = mybir.ActivationFunctionType
ALU = mybir.AluOpType

D = 2048
DIN = 14720
MEM = 256
TT = 512
EPOCH = 30000

C_R, C_K, C_V, C_WD = 0, 1024, 2048, 3072
C_RG = 3200
C_SQ = 4224
C_SK = 5248
C_SV = 5376
C_SG = 5504
C_XQ = 6528
C_XG = 7552
C_MG = 8576

PV_GPRE, PV_GPOST, PV_GMEM, PV_MU = 0, 16, 32, 48
PV_DB, PV_IB, PV_KK, PV_KA, PV_RK, PV_GW, PV_GB, PV_SINK = 73, 81, 89, 97, 105, 113, 121, 129
NPV = 137
CC_ONES, CC_BLK, CC_ID, CC_MA, CC_ML, CC_II, CC_MS, CC_MS0, CC_SCAN = 0, 128, 256, 384, 512, 576, 640, 896, 1152
NCC = 1664


class Prog:
    COMPUTE = ("pe", "act", "dve", "pool")

    def __init__(self, nc, stack):
        self.nc, self.stack = nc, stack
        self.eng_names = ("pe", "act", "dve", "pool", "sp")
        self.ops = {e: [] for e in self.eng_names}
        self.cnt = {e: 0 for e in self.COMPUTE}
        self.sem_objs, self.sem_id, self.cur_sem = {}, 0, {}
        for e in self.COMPUTE:
            self.cur_sem[e] = self._new_sem()
        self.waited = {e: {} for e in self.eng_names}
        self.buf, self.dma_sems = {}, {}
        self.n_inst = 0
        self.E = {"pe": nc.tensor, "act": nc.scalar, "dve": nc.vector, "pool": nc.gpsimd, "sp": nc.sync}

    def _new_sem(self):
        s = self.stack.enter_context(self.nc.semaphore(f"s{self.sem_id}"))
        self.sem_objs[self.sem_id] = s
        self.sem_id += 1
        return self.sem_id - 1

    def _deps(self, eng, reads, writes):
        need = {}

        def add(tok):
            sidx, val, teng = tok
            if teng == "pe" and eng == "pe":
                return
            if need.get(sidx, 0) < val:
                need[sidx] = val
        for k in reads:
            st = self.buf.get(k)
            if st and st[0] is not None:
                add(st[0])
        for k in writes:
            st = self.buf.get(k)
            if st:
                if st[0] is not None:
                    add(st[0])
                for t in st[1]:
                    add(t)
        for sidx, val in need.items():
            if self.waited[eng].get(sidx, 0) >= val:
                continue
            self.waited[eng][sidx] = val
            self.E[eng].wait_ge(self.sem_objs[sidx], val)

    def _record(self, tok, reads, writes):
        for k in reads:
            self.buf.setdefault(k, [None, []])[1].append(tok)
        for k in writes:
            self.buf[k] = [tok, []]

    def op(self, eng, fn, rd=(), wr=()):
        self._deps(eng, rd, wr)
        if self.cnt[eng] >= EPOCH:
            self.cur_sem[eng] = self._new_sem()
            self.cnt[eng] = 0
        self.cnt[eng] += 1
        tok = (self.cur_sem[eng], self.cnt[eng], eng)
        fn(self.E[eng]).then_inc(self.sem_objs[self.cur_sem[eng]], 1)
        self._record(tok, rd, wr)
        self.n_inst += 1

    def dma(self, eng, fn, semkey, rd=(), wr=()):
        self._deps(eng, rd, wr)
        if semkey not in self.dma_sems:
            self.dma_sems[semkey] = [self._new_sem(), 0]
        ds = self.dma_sems[semkey]
        ds[1] += 16
        tok = (ds[0], ds[1], "dma")
        fn(self.E[eng]).then_inc(self.sem_objs[ds[0]], 16)
        self._record(tok, rd, wr)
        self.n_inst += 1

    def wait_all(self, eng, keys):
        self._deps(eng, keys, ())

    def emit(self):
        return

    def emit_old(self):
        engmap = {"pe": "tensor", "act": "scalar", "dve": "vector", "pool": "gpsimd", "sp": "sync"}
        with self.nc.Block() as block:
            for e in self.eng_names:
                ops = self.ops[e]
                if not ops:
                    continue

                def body(eng, ops=ops):
                    for o in ops:
                        if o[0] == "wait":
                            eng.wait_ge(self.sem_objs[o[1]], o[2])
                        else:
                            o[1](eng).then_inc(self.sem_objs[o[2]], o[3])
                getattr(block, engmap[e])(body)


def build(T, L):
    NT = T // TT
    nc = bass.Bass("TRN2", target_bir_lowering=False)
    dr = lambda name, shape, kind="ExternalInput": nc.dram_tensor(name, shape, F32, kind=kind).ap()
    xT = dr("xT", [D, T])
    memT = dr("memT", [D, MEM])
    w_in = dr("w_in", [L, D, DIN])
    w_mkv = dr("w_mkv", [L, D, 2048])
    w_up = [dr(f"w_up{i}", [L, 1024, D]) for i in range(3)]
    w_out = dr("w_out", [L, D, D])
    dwd = dr("dwd", [L, 128, 1024])
    pvd = dr("pvd", [128, L * NPV])
    ccd = dr("ccd", [128, NCC])
    outT = dr("outT", [D, T], kind="ExternalOutput")

    with contextlib.ExitStack() as st:
        P = Prog(nc, st)
        sb = lambda name, shape, dt: st.enter_context(nc.sbuf_tensor(name, shape, dt))
        x_res = sb("x_res", [128, 16, TT], F32)
        HY = sb("HY", [128, 10240], F32)
        hT = HY[:, 0:4096].bitcast(BF16).rearrange("p (a b) -> p a b", b=TT)
        ybr = HY[:, 4096:10240].bitcast(BF16).rearrange("p (a b) -> p a b", b=TT)
        o_f = HY[:, 0:8192].rearrange("p (a b) -> p a b", b=TT)

        def okeys(dc):
            return [("h", 2 * dc), ("h", 2 * dc + 1)] if dc < 8 else [("y", 2 * (dc - 8)), ("y", 2 * (dc - 8) + 1)]
        wbuf = [sb(f"wbuf{i}", [128, 16, 512], BF16) for i in range(2)]
        NF, NB = 14, 18
        ft = [sb(f"ft{i}", [128, 516], F32) for i in range(NF)]
        bt = [sb(f"bt{i}", [128, 512], BF16) for i in range(NB)]
        kmT = sb("kmT", [128, L * 8, MEM], BF16)
        vm = sb("vm", [128, L * 2, 1024], BF16)
        kd = sb("kd", [128, L * 2, 640], BF16)
        vt = sb("vt", [128, L * 5, 128], BF16)
        dw = sb("dw", [128, L, 1024], BF16)
        pv = sb("pv", [128, L * NPV], F32)
        omka = sb("omka", [128, L * 8], F32)
        esink = sb("esink", [128, L * 8], F32)
        ccf = sb("ccf", [128, NCC], F32)
        ccb = sb("ccb", [128, NCC], BF16)
        shp = sb("shp", [128, L * 25], F32)
        Hf = sb("Hf", [128, L * 8, 64], F32)
        Hb = sb("Hb", [128, L * 8, 64], BF16)
        ec = sb("ec", [128, 8], F32)
        sm = sb("sm", [128, 256], BF16)
        pp = [st.enter_context(nc.psum_tensor(f"pp{i}", [128, 512], F32)) for i in range(4)]
        pq = [st.enter_context(nc.psum_tensor(f"pq{i}", [128, 512], F32)) for i in range(4)]
        PPK = [("pp", i) for i in range(4)]
        PQK = [("pq", i) for i in range(4)]

        onesF = ccf[:, CC_ONES:CC_ONES + 128]
        onesB = ccb[:, CC_ONES:CC_ONES + 128]
        blkB = ccb[:, CC_BLK:CC_BLK + 128]
        identB = ccb[:, CC_ID:CC_ID + 128]
        maskA = ccf[:, CC_MA:CC_MA + 128]
        maskL = ccf[:, CC_ML:CC_ML + 64]
        identI = ccf[:, CC_II:CC_II + 64]
        maskS = ccb[:, CC_MS:CC_MS + 256]
        maskS0 = ccb[:, CC_MS0:CC_MS0 + 256]
        scanm = ccf[:, CC_SCAN:CC_SCAN + 512]

        P.dma("sp", lambda e: e.dma_start(out=pv[:], in_=pvd), "pv", wr=["pv"])
        P.dma("sp", lambda e: e.dma_start(out=ccf[:], in_=ccd), "ccf", wr=["ccf"])
        P.dma("pool", lambda e: e.dma_start(out=ccb[:], in_=ccd), "ccb", wr=["ccb"])
        for l in range(L):
            P.dma("pool", lambda e, l=l: e.dma_start(out=dw[:, l, :], in_=dwd[l]), "dw", wr=["dw"])
        P.op("dve", lambda e: e.memset(shp[:], 0.0), wr=["shp"])
        P.op("dve", lambda e: e.memset(Hf[:], 0.0), wr=["Hf"])
        P.op("dve", lambda e: e.memset(Hb[:], 0.0), wr=["Hb"])
        P.op("dve", lambda e: e.memset(kd[:], 0.0), wr=["kd"])
        P.op("dve", lambda e: e.memset(vt[:], 0.0), wr=["vt"])
        for l in range(L):
            b = l * NPV
            P.op("dve", lambda e, l=l, b=b: e.tensor_scalar(out=omka[:, l * 8:(l + 1) * 8], in0=pv[:, b + PV_KA:b + PV_KA + 8],
                                                             scalar1=-1.0, scalar2=1.0, op0=ALU.mult, op1=ALU.add), rd=["pv"], wr=["omka"])
            P.op("act", lambda e, l=l, b=b: e.activation(out=esink[:, l * 8:(l + 1) * 8], in_=pv[:, b + PV_SINK:b + PV_SINK + 8], func=AF.Exp),
                 rd=["pv"], wr=["esink"])

        wstate = {"slot": 0}

        def load_w(src3, pieces, nk=16):
            s = wstate["slot"]
            wstate["slot"] = 1 - s
            for (doff, c0, n) in pieces:
                P.dma("pool", lambda e, s=s, doff=doff, c0=c0, n=n: e.dma_start(
                    out=wbuf[s][:, 0:nk, doff:doff + n],
                    in_=src3[:, c0:c0 + n].rearrange("(kc p) c -> p kc c", p=128)), f"w{s}", wr=[("w", s)])
            return s

        def proj(s, j, out_ps, out_key, rhs_fn, rhs_keys, nk=16, ncol=128):
            for kc in range(nk):
                P.op("pe", lambda e, kc=kc: e.matmul(out_ps, lhsT=wbuf[s][:, kc, j * 128:j * 128 + ncol], rhs=rhs_fn(kc),
                                                      start=(kc == 0), stop=(kc == nk - 1)),
                     rd=[("w", s)] + rhs_keys(kc), wr=[out_key])

        HK = [("h", i) for i in range(16)]

        def proj_h(s, j, bank):
            proj(s, j, pp[bank][:], ("pp", bank), lambda kc: hT[:, kc, :], lambda kc: [("h", kc)])

        def rms_stats(src_fn, src_keys, ncols, nchunks, out_rstd, out_key, tmpi):
            for dc in range(nchunks):
                t = ft[tmpi + (dc % 2)]
                P.op("act", lambda e, dc=dc, t=t: e.activation(out=t[:, 0:ncols], in_=src_fn(dc), func=AF.Square),
                     rd=src_keys(dc), wr=[("f", tmpi + (dc % 2))])
                P.op("pe", lambda e, dc=dc, t=t: e.matmul(pq[0][:, 0:ncols], lhsT=onesF, rhs=t[:, 0:ncols],
                                                           start=(dc == 0), stop=(dc == nchunks - 1)),
                     rd=[("f", tmpi + (dc % 2)), "ccf"], wr=[("pq", 0)])
            P.op("act", lambda e: e.activation(out=out_rstd, in_=pq[0][:, 0:ncols], func=AF.Sqrt, scale=1.0 / D, bias=1e-6),
                 rd=[("pq", 0)], wr=[out_key])
            P.op("dve", lambda e: e.reciprocal(out=out_rstd, in_=out_rstd), rd=[out_key], wr=[out_key])

        mT = x_res
        P.dma("sp", lambda e: e.dma_start(out=mT[:, :, 0:MEM], in_=memT.rearrange("(dc p) m -> p dc m", p=128)), "x0",
              wr=[("x", i) for i in range(16)])
        rstd_m = ft[2]
        rms_stats(lambda dc: mT[:, dc, 0:MEM], lambda dc: [("x", dc)], MEM, 16, rstd_m[:, 0:MEM], ("f", 2), 0)
        for l in range(L):
            b = l * NPV
            for dc in range(16):
                P.op("dve", lambda e, dc=dc, b=b: e.scalar_tensor_tensor(out=hT[:, dc, 0:MEM], in0=mT[:, dc, 0:MEM],
                                                                          scalar=pv[:, b + PV_GMEM + dc:b + PV_GMEM + dc + 1],
                                                                          in1=rstd_m[:, 0:MEM], op0=ALU.mult, op1=ALU.mult),
                     rd=[("x", dc), "pv", ("f", 2)], wr=[("h", dc)])
            for g in range(4):
                s = load_w(w_mkv[l], [(0, g * 512, 512)])
                if g < 2:
                    for j in range(4):
                        proj(s, j, pp[j][:, 0:MEM], ("pp", j), lambda kc: hT[:, kc, 0:MEM], lambda kc: [("h", kc)])
                        ci = l * 8 + g * 4 + j
                        P.op("act", lambda e, j=j, ci=ci: e.activation(out=kmT[:, ci, :], in_=pp[j][:, 0:MEM], func=AF.Copy),
                             rd=[("pp", j)], wr=["kmT"])
                else:
                    for mt in range(2):
                        for kc in range(16):
                            P.op("pe", lambda e, kc=kc, mt=mt, s=s: e.matmul(pp[mt][:], lhsT=hT[:, kc, mt * 128:(mt + 1) * 128],
                                                                                rhs=wbuf[s][:, kc, :], start=(kc == 0), stop=(kc == 15)),
                                 rd=[("w", s), ("h", kc)], wr=[("pp", mt)])
                        P.op("act", lambda e, mt=mt, l=l, g=g: e.activation(out=vm[:, l * 2 + mt, (g - 2) * 512:(g - 1) * 512], in_=pp[mt][:],
                                                                             func=AF.Copy), rd=[("pp", mt)], wr=["vm"])

        def block(ti, l):
            b = l * NPV
            pvc = lambda off, c: pv[:, b + off + c:b + off + c + 1]
            t0 = ti * TT
            if l == 0:
                for q in range(4):
                    P.dma("sp", lambda e, q=q: e.dma_start(out=x_res[:, q * 4:(q + 1) * 4, :],
                                                           in_=xT[q * 512:(q + 1) * 512, t0:t0 + TT].rearrange("(dc p) t -> p dc t", p=128)),
                          f"x{q}", wr=[("x", q * 4 + i) for i in range(4)])
            rstd = ft[2]
            rms_stats(lambda dc: x_res[:, dc, :], lambda dc: [("x", dc)], TT, 16, rstd[:, 0:TT], ("f", 2), 0)
            for dc in range(16):
                P.op("dve", lambda e, dc=dc: e.scalar_tensor_tensor(out=hT[:, dc, :], in0=x_res[:, dc, :], scalar=pvc(PV_GPRE, dc),
                                                                     in1=rstd[:, 0:TT], op0=ALU.mult, op1=ALU.mult),
                     rd=[("x", dc), "pv", ("f", 2)], wr=[("h", dc)])

            def shift(bank, fi_raw, fi_out, chunk_idx, rows=slice(0, 128)):
                raw = ft[fi_raw]
                si = l * 25 + chunk_idx
                P.op("act", lambda e: e.activation(out=raw[:, 1:513], in_=pp[bank][:], func=AF.Copy), rd=[("pp", bank)], wr=[("f", fi_raw)])
                P.op("dve", lambda e: e.tensor_copy(out=raw[:, 0:1], in_=shp[:, si:si + 1]), rd=["shp"], wr=[("f", fi_raw)])
                P.op("dve", lambda e: e.tensor_copy(out=shp[:, si:si + 1], in_=raw[:, 512:513]), rd=[("f", fi_raw)], wr=["shp"])
                P.op("dve", lambda e: e.tensor_tensor(out=ft[fi_out][:, 0:512], in0=raw[:, 0:512], in1=raw[:, 1:513], op=ALU.subtract),
                     rd=[("f", fi_raw)], wr=[("f", fi_out)])
                P.op("dve", lambda e: e.scalar_tensor_tensor(out=ft[fi_out][:, 0:512], in0=ft[fi_out][:, 0:512], scalar=pvc(PV_MU, chunk_idx),
                                                             in1=raw[:, 1:513], op0=ALU.mult, op1=ALU.add),
                     rd=[("f", fi_out), ("f", fi_raw), "pv"], wr=[("f", fi_out)])

            s = load_w(w_in[l], [(0, C_WD, 128)])
            proj_h(s, 0, 0)
            shift(0, 0, 1, 24)
            twd, adb = bt[16], bt[17]
            P.op("act", lambda e: e.activation(out=twd[0:64, :], in_=ft[1][0:64, 0:512], func=AF.Tanh), rd=[("f", 1)], wr=[("b", 16)])
            P.op("dve", lambda e: e.tensor_copy(out=adb[64:128, :], in_=ft[1][64:128, 0:512]), rd=[("f", 1)], wr=[("b", 17)])
            v3 = lambda ap: ap.rearrange("p (n t) -> p n t", t=64)
            for hp in range(8):
                s = load_w(w_in[l], [(0, C_R + hp * 128, 128), (128, C_K + hp * 128, 128), (256, C_V + hp * 128, 128),
                                     (384, C_RG + hp * 128, 128)])
                for j in range(4):
                    proj_h(s, j, j)
                Rs, Ks, Vs = ft[3], ft[4], ft[5]
                shift(0, 0, 3, hp)
                shift(1, 1, 4, 8 + hp)
                shift(2, 0, 5, 16 + hp)
                sg = ft[6]
                P.op("act", lambda e: e.activation(out=sg[:, 0:512], in_=pp[3][:], func=AF.Silu), rd=[("pp", 3)], wr=[("f", 6)])
                P.op("pe", lambda e, hp=hp: e.matmul(pq[0][:], lhsT=dw[0:64, l, hp * 128:(hp + 1) * 128], rhs=twd[0:64, :], start=True, stop=True),
                     rd=["dw", ("b", 16)], wr=[("pq", 0)])
                P.op("pe", lambda e, hp=hp: e.matmul(pq[1][:], lhsT=dw[64:128, l, hp * 128:(hp + 1) * 128], rhs=adb[64:128, :], start=True, stop=True),
                     rd=["dw", ("b", 17)], wr=[("pq", 1)])
                LW, A = ft[7], ft[8]
                P.op("act", lambda e, hp=hp: e.activation(out=LW[:, 0:512], in_=pq[0][:], func=AF.Sigmoid, bias=pvc(PV_DB, hp)),
                     rd=[("pq", 0), "pv"], wr=[("f", 7)])
                P.op("act", lambda e, hp=hp: e.activation(out=A[:, 0:512], in_=pq[1][:], func=AF.Sigmoid, bias=pvc(PV_IB, hp)),
                     rd=[("pq", 1), "pv"], wr=[("f", 8)])
                KK, TMP = ft[9], ft[0]
                P.op("dve", lambda e, hp=hp: e.tensor_scalar(out=KK[:, 0:512], in0=Ks[:, 0:512], scalar1=pvc(PV_KK, hp), scalar2=None, op0=ALU.mult),
                     rd=[("f", 4), "pv"], wr=[("f", 9)])
                P.op("dve", lambda e: e.tensor_tensor(out=bt[0][:], in0=KK[:, 0:512], in1=KK[:, 0:512], op=ALU.mult), rd=[("f", 9)], wr=[("b", 0)])
                P.op("pe", lambda e: e.matmul(pq[2][:], lhsT=blkB, rhs=bt[0][:], start=True, stop=True), rd=["ccb", ("b", 0)], wr=[("pq", 2)])
                P.op("act", lambda e: e.activation(out=TMP[:, 0:512], in_=pq[2][:], func=AF.Sqrt), rd=[("pq", 2)], wr=[("f", 0)])
                P.op("dve", lambda e: e.tensor_scalar(out=TMP[:, 0:512], in0=TMP[:, 0:512], scalar1=1e-12, scalar2=None, op0=ALU.max),
                     rd=[("f", 0)], wr=[("f", 0)])
                P.op("dve", lambda e: e.reciprocal(out=TMP[:, 0:512], in_=TMP[:, 0:512]), rd=[("f", 0)], wr=[("f", 0)])
                P.op("dve", lambda e: e.tensor_tensor(out=KK[:, 0:512], in0=KK[:, 0:512], in1=TMP[:, 0:512], op=ALU.mult),
                     rd=[("f", 9), ("f", 0)], wr=[("f", 9)])
                K2 = ft[10]
                P.op("dve", lambda e, hp=hp: e.tensor_scalar(out=K2[:, 0:512], in0=A[:, 0:512], scalar1=pvc(PV_KA, hp),
                                                             scalar2=omka[:, l * 8 + hp:l * 8 + hp + 1], op0=ALU.mult, op1=ALU.add),
                     rd=[("f", 8), "pv", "omka"], wr=[("f", 10)])
                P.op("dve", lambda e: e.tensor_tensor(out=K2[:, 0:512], in0=K2[:, 0:512], in1=Ks[:, 0:512], op=ALU.mult),
                     rd=[("f", 10), ("f", 4)], wr=[("f", 10)])
                P.op("dve", lambda e, hp=hp: e.scalar_tensor_tensor(out=bt[0][:], in0=Rs[:, 0:512], scalar=pvc(PV_RK, hp), in1=K2[:, 0:512],
                                                                    op0=ALU.mult, op1=ALU.mult), rd=[("f", 3), ("f", 10), "pv"], wr=[("b", 0)])
                P.op("pe", lambda e: e.matmul(pq[3][:], lhsT=blkB, rhs=bt[0][:], start=True, stop=True), rd=["ccb", ("b", 0)], wr=[("pq", 3)])
                BON = ft[11]
                P.op("dve", lambda e: e.tensor_tensor(out=BON[:, 0:512], in0=pq[3][:], in1=Vs[:, 0:512], op=ALU.mult),
                     rd=[("pq", 3), ("f", 5)], wr=[("f", 11)])
                CUM = ft[12]
                P.op("act", lambda e: e.mul(out=LW[:, 0:512], in_=LW[:, 0:512], mul=-0.6065306597126334), rd=[("f", 7)], wr=[("f", 7)])
                P.op("dve", lambda e: e.tensor_tensor_scan(out=CUM[:, 0:512], data0=scanm, data1=LW[:, 0:512], initial=0.0,
                                                           op0=ALU.mult, op1=ALU.add), rd=["ccf", ("f", 7)], wr=[("f", 12)])
                EP, EM = ft[13], ft[0]
                P.op("act", lambda e: e.activation(out=EP[:, 0:512], in_=CUM[:, 0:512], func=AF.Exp), rd=[("f", 12)], wr=[("f", 13)])
                P.op("dve", lambda e: e.tensor_copy(out=ec[:], in_=v3(EP[:, 0:512])[:, :, 63]), rd=[("f", 13)], wr=["ec"])
                RAh, KBh = [bt[1], bt[3]], [bt[2], bt[4]]
                RA4 = [x_[:].rearrange("p (n w t) -> p n w t", w=2, t=64) for x_ in RAh]
                KB4 = [x_[:].rearrange("p (n w t) -> p n w t", w=2, t=64) for x_ in KBh]
                RAK, KBK = [("b", 1), ("b", 3)], [("b", 2), ("b", 4)]
                for h in range(2):
                    P.op("dve", lambda e, h=h: e.tensor_tensor(out=RA4[h][:, :, 0, :], in0=v3(Rs[:, h * 256:(h + 1) * 256]),
                                                               in1=v3(EP[:, h * 256:(h + 1) * 256]), op=ALU.mult),
                         rd=[("f", 3), ("f", 13)], wr=[RAK[h]])
                P.op("dve", lambda e: e.tensor_tensor(out=EM[:, 0:512], in0=CUM[:, 0:512], in1=LW[:, 0:512], op=ALU.subtract),
                     rd=[("f", 12), ("f", 7)], wr=[("f", 0)])
                P.op("act", lambda e: e.activation(out=EM[:, 0:512], in_=EM[:, 0:512], func=AF.Exp), rd=[("f", 0)], wr=[("f", 0)])
                for h in range(2):
                    P.op("dve", lambda e, h=h: e.scalar_tensor_tensor(out=RA4[h][:, :, 1, :], in0=v3(KK[:, h * 256:(h + 1) * 256]), scalar=-1.0,
                                                                      in1=v3(EM[:, h * 256:(h + 1) * 256]), op0=ALU.mult, op1=ALU.mult),
                         rd=[("f", 9), ("f", 0)], wr=[RAK[h]])
                EN = ft[13]
                P.op("act", lambda e: e.activation(out=EN[:, 0:512], in_=CUM[:, 0:512], func=AF.Exp, scale=-1.0), rd=[("f", 12)], wr=[("f", 13)])
                BP = ft[7]
                P.op("dve", lambda e: e.tensor_tensor(out=BP[:, 0:512], in0=KK[:, 0:512], in1=A[:, 0:512], op=ALU.mult),
                     rd=[("f", 9), ("f", 8)], wr=[("f", 7)])
                for h in range(2):
                    sl = slice(h * 256, (h + 1) * 256)
                    P.op("dve", lambda e, h=h, sl=sl: e.tensor_tensor(out=KB4[h][:, :, 0, :], in0=v3(K2[:, sl]), in1=v3(EN[:, sl]), op=ALU.mult),
                         rd=[("f", 10), ("f", 13)], wr=[KBK[h]])
                    P.op("dve", lambda e, h=h, sl=sl: e.tensor_tensor(out=KB4[h][:, :, 1, :], in0=v3(BP[:, sl]), in1=v3(EN[:, sl]), op=ALU.mult),
                         rd=[("f", 7), ("f", 13)], wr=[KBK[h]])
                Vb = bt[5]
                P.op("act", lambda e: e.activation(out=Vb[:], in_=Vs[:, 0:512], func=AF.Copy), rd=[("f", 5)], wr=[("b", 5)])
                VT, KIT, BIT = bt[6], bt[7], bt[8]
                pqb = [pq[i][:].bitcast(BF16) for i in range(4)]
                for (dst, di, bank, srcfn, skeys) in (
                        (VT, 6, 0, lambda n, hs: Vb[hs, n * 64:(n + 1) * 64], lambda n: [("b", 5)]),
                        (KIT, 7, 1, lambda n, hs: KB4[n // 4][hs, n % 4, 0, :], lambda n: [KBK[n // 4]]),
                        (BIT, 8, 2, lambda n, hs: KB4[n // 4][hs, n % 4, 1, :], lambda n: [KBK[n // 4]])):
                    for n in range(8):
                        for h2 in range(2):
                            hs = slice(h2 * 64, (h2 + 1) * 64)
                            P.op("pe", lambda e, n=n, hs=hs, bank=bank, srcfn=srcfn: e.transpose(
                                out=pqb[bank][hs, n * 64:(n + 1) * 64], in_=srcfn(n, hs), identity=identB[hs, hs]),
                                rd=skeys(n) + ["ccb"], wr=[("pq", bank)])
                    P.op("act" if di != 7 else "dve",
                         (lambda e, dst=dst, bank=bank: e.activation(out=dst[:], in_=pqb[bank][:, 0:512], func=AF.Copy)) if di != 7 else
                         (lambda e, dst=dst, bank=bank: e.tensor_copy(out=dst[:], in_=pqb[bank][:, 0:512])),
                         rd=[("pq", bank)], wr=[("b", di)])
                ATk, ATb, X0 = [bt[9], bt[10]], [bt[11], bt[12]], bt[13]
                for h in range(2):
                    for n4 in range(4):
                        for h2 in range(2):
                            hs = slice(h2 * 64, (h2 + 1) * 64)
                            P.op("pe", lambda e, h=h, n4=n4, hs=hs: e.matmul(pq[0][hs, n4 * 128:(n4 + 1) * 128], lhsT=KB4[h][hs, n4, 0, :],
                                                                             rhs=RA4[h][hs, n4, :, :], start=True, stop=True),
                                 rd=[KBK[h], RAK[h]], wr=[("pq", 0)])
                            P.op("pe", lambda e, h=h, n4=n4, hs=hs: e.matmul(pq[1][hs, n4 * 128:(n4 + 1) * 128], lhsT=KB4[h][hs, n4, 1, :],
                                                                             rhs=RA4[h][hs, n4, :, :], start=True, stop=True),
                                 rd=[KBK[h], RAK[h]], wr=[("pq", 1)])
                            P.op("pe", lambda e, h=h, n4=n4, hs=hs: e.matmul(pq[2][hs, n4 * 64:(n4 + 1) * 64], lhsT=RA4[h][hs, n4, 1, :],
                                                                             rhs=KB4[h][hs, n4, 1, :], start=True, stop=True),
                                 rd=[KBK[h], RAK[h]], wr=[("pq", 2)])
                    mA = maskA.unsqueeze(1).broadcast_to([128, 4, 128])
                    mL = maskL.unsqueeze(1).broadcast_to([128, 4, 64])
                    P.op("dve", lambda e, h=h, mA=mA: e.tensor_tensor(out=ATk[h][:].rearrange("p (n c) -> p n c", c=128),
                                                                     in0=pq[0][:].rearrange("p (n c) -> p n c", c=128), in1=mA, op=ALU.mult),
                         rd=[("pq", 0), "ccf"], wr=[("b", 9 + h)])
                    P.op("dve", lambda e, h=h, mA=mA: e.tensor_tensor(out=ATb[h][:].rearrange("p (n c) -> p n c", c=128),
                                                                     in0=pq[1][:].rearrange("p (n c) -> p n c", c=128), in1=mA, op=ALU.mult),
                         rd=[("pq", 1), "ccf"], wr=[("b", 11 + h)])
                    P.op("dve", lambda e, h=h, mL=mL: e.tensor_tensor(out=X0[:, h * 256:(h + 1) * 256].rearrange("p (n c) -> p n c", c=64),
                                                                     in0=pq[2][:, 0:256].rearrange("p (n c) -> p n c", c=64), in1=mL, op=ALU.mult),
                         rd=[("pq", 2), "ccf"], wr=[("b", 13)])
                ATk3 = [a[:].rearrange("p (n c) -> p n c", c=128) for a in ATk]
                ATb3 = [a[:].rearrange("p (n c) -> p n c", c=128) for a in ATb]
                Xt0 = bt[14]
                for h in range(2):
                    P.op("act", lambda e, h=h: e.activation(out=Xt0[:, h * 256:(h + 1) * 256].rearrange("p (n c) -> p n c", c=64),
                                                            in_=ATb3[h][:, :, 64:128], func=AF.Copy), rd=[("b", 11 + h)], wr=[("b", 14)])
                Mt = bt[15]
                iI = identI.unsqueeze(1).broadcast_to([128, 8, 64])
                P.op("dve", lambda e: e.tensor_tensor(out=v3(Mt[:]), in0=v3(Xt0[:]), in1=iI, op=ALU.add), rd=[("b", 14), "ccf"], wr=[("b", 15)])
                Xc, Xtc, xi, xti = X0, Xt0, 13, 14
                alt = {13: 0, 14: 16 - 16}
                pingX, pingXt = [(bt[13], 13), (bt[0], 0)], [(bt[14], 14), (bt[5], 5)]
                for lvl in range(1, 6):
                    Xn, xni = pingX[lvl % 2]
                    Xtn, xtni = pingXt[lvl % 2]
                    for n in range(8):
                        for h2 in range(2):
                            hs = slice(h2 * 64, (h2 + 1) * 64)
                            cs = slice(n * 64, (n + 1) * 64)
                            P.op("pe", lambda e, hs=hs, cs=cs, Xc=Xc, Xtc=Xtc: e.matmul(pq[0][hs, cs], lhsT=Xc[hs, cs], rhs=Xtc[hs, cs], start=True, stop=True),
                                 rd=[("b", xi), ("b", xti)], wr=[("pq", 0)])
                            P.op("pe", lambda e, hs=hs, cs=cs, Xc=Xc, Xtc=Xtc: e.matmul(pq[1][hs, cs], lhsT=Xtc[hs, cs], rhs=Xc[hs, cs], start=True, stop=True),
                                 rd=[("b", xi), ("b", xti)], wr=[("pq", 1)])
                    P.op("act", lambda e, Xtn=Xtn: e.activation(out=Xtn[:], in_=pq[0][:], func=AF.Copy), rd=[("pq", 0)], wr=[("b", xtni)])
                    P.op("dve", lambda e, Xn=Xn: e.tensor_copy(out=Xn[:], in_=pq[1][:]), rd=[("pq", 1)], wr=[("b", xni)])
                    for n in range(8):
                        for h2 in range(2):
                            hs = slice(h2 * 64, (h2 + 1) * 64)
                            cs = slice(n * 64, (n + 1) * 64)
                            P.op("pe", lambda e, hs=hs, cs=cs, Xn=Xn: e.matmul(pq[2][hs, cs], lhsT=Xn[hs, cs], rhs=Mt[hs, cs], start=True, stop=True),
                                 rd=[("b", xni), ("b", 15)], wr=[("pq", 2)])
                    P.op("dve", lambda e: e.tensor_tensor(out=Mt[:], in0=pq[2][:], in1=Mt[:], op=ALU.add), rd=[("pq", 2), ("b", 15)], wr=[("b", 15)])
                    Xc, Xtc, xi, xti = Xn, Xtn, xni, xtni
                hh = l * 8 + hp
                R0b, Ub = sm[:, 0:64], sm[:, 64:128]
                TH = ft[0]
                for n in range(8):
                    h, n4 = n // 4, n % 4
                    cs = slice(n * 64, (n + 1) * 64)
                    for h2 in range(2):
                        hs = slice(h2 * 64, (h2 + 1) * 64)
                        P.op("pe", lambda e, hs=hs, h=h, n4=n4: e.matmul(pq[3][hs, 0:64], lhsT=RA4[h][hs, n4, 1, :], rhs=Hb[hs, hh, :], start=True, stop=False),
                             rd=[RAK[h], "Hb"], wr=[("pq", 3)])
                        P.op("pe", lambda e, hs=hs, h=h, n4=n4, cs=cs: e.matmul(pq[3][hs, 0:64], lhsT=ATk3[h][hs, n4, 64:128], rhs=VT[hs, cs], start=False, stop=True),
                             rd=[("b", 9 + h), ("b", 6)], wr=[("pq", 3)])
                    P.op("act", lambda e: e.activation(out=R0b, in_=pq[3][:, 0:64], func=AF.Copy), rd=[("pq", 3)], wr=["sm0"])
                    for h2 in range(2):
                        hs = slice(h2 * 64, (h2 + 1) * 64)
                        P.op("pe", lambda e, hs=hs, cs=cs: e.matmul(pq[3][hs, 64:128], lhsT=Mt[hs, cs], rhs=sm[hs, 0:64], start=True, stop=True),
                             rd=[("b", 15), "sm0"], wr=[("pq", 3)])
                    P.op("dve", lambda e: e.tensor_copy(out=Ub, in_=pq[3][:, 64:128]), rd=[("pq", 3)], wr=["sm1"])
                    for h2 in range(2):
                        hs = slice(h2 * 64, (h2 + 1) * 64)
                        P.op("pe", lambda e, hs=hs, h=h, n4=n4, cs=cs: e.matmul(pp[0][hs, cs], lhsT=Hb[hs, hh, :], rhs=RA4[h][hs, n4, 0, :], start=True, stop=False),
                             rd=["Hb", RAK[h]], wr=[("pp", 0)])
                        P.op("pe", lambda e, hs=hs, h=h, n4=n4, cs=cs: e.matmul(pp[0][hs, cs], lhsT=sm[hs, 64:128], rhs=ATb3[h][hs, n4, 0:64], start=False, stop=False),
                             rd=["sm1", ("b", 11 + h)], wr=[("pp", 0)])
                        P.op("pe", lambda e, hs=hs, h=h, n4=n4, cs=cs: e.matmul(pp[0][hs, cs], lhsT=VT[hs, cs], rhs=ATk3[h][hs, n4, 0:64], start=False, stop=True),
                             rd=[("b", 6), ("b", 9 + h)], wr=[("pp", 0)])
                        P.op("pe", lambda e, hs=hs, cs=cs: e.matmul(pq[3][hs, 128:192], lhsT=KIT[hs, cs], rhs=VT[hs, cs], start=True, stop=False),
                             rd=[("b", 7), ("b", 6)], wr=[("pq", 3)])
                        P.op("pe", lambda e, hs=hs, cs=cs: e.matmul(pq[3][hs, 128:192], lhsT=BIT[hs, cs], rhs=sm[hs, 64:128], start=False, stop=True),
                             rd=[("b", 8), "sm1"], wr=[("pq", 3)])
                    P.op("dve", lambda e: e.tensor_tensor(out=TH[:, 0:64], in0=pq[3][:, 128:192], in1=Hf[:, hh, :], op=ALU.add),
                         rd=[("pq", 3), "Hf"], wr=[("f", 0)])
                    P.op("dve", lambda e, n=n: e.tensor_scalar(out=Hf[:, hh, :], in0=TH[:, 0:64], scalar1=ec[:, n:n + 1], scalar2=None, op0=ALU.mult),
                         rd=[("f", 0), "ec"], wr=["Hf"])
                    P.op("act", lambda e: e.activation(out=Hb[:, hh, :], in_=Hf[:, hh, :], func=AF.Copy), rd=["Hf"], wr=["Hb"])
                Yf, Yb, Y2 = ft[1], bt[0], bt[5]
                P.op("act", lambda e: e.activation(out=Yf[:, 0:512], in_=pp[0][:], func=AF.Copy), rd=[("pp", 0)], wr=[("f", 1)])
                P.op("dve", lambda e: e.tensor_copy(out=Yb[:], in_=Yf[:, 0:512]), rd=[("f", 1)], wr=[("b", 0)])
                P.op("dve", lambda e: e.tensor_tensor(out=Y2[:], in0=Yf[:, 0:512], in1=Yf[:, 0:512], op=ALU.mult), rd=[("f", 1)], wr=[("b", 5)])
                P.op("pe", lambda e: e.matmul(pq[0][:], lhsT=blkB, rhs=Yb[:], start=True, stop=True), rd=["ccb", ("b", 0)], wr=[("pq", 0)])
                P.op("pe", lambda e: e.matmul(pq[1][:], lhsT=blkB, rhs=Y2[:], start=True, stop=True), rd=["ccb", ("b", 5)], wr=[("pq", 1)])
                MEAN, VAR = ft[0], ft[9]
                P.op("dve", lambda e: e.tensor_scalar(out=MEAN[:, 0:512], in0=pq[0][:], scalar1=1.0 / 64, scalar2=None, op0=ALU.mult),
                     rd=[("pq", 0)], wr=[("f", 0)])
                P.op("dve", lambda e: e.tensor_tensor(out=VAR[:, 0:512], in0=MEAN[:, 0:512], in1=MEAN[:, 0:512], op=ALU.mult), rd=[("f", 0)], wr=[("f", 9)])
                P.op("dve", lambda e: e.scalar_tensor_tensor(out=VAR[:, 0:512], in0=pq[1][:], scalar=1.0 / 64, in1=VAR[:, 0:512],
                                                             op0=ALU.mult, op1=ALU.subtract), rd=[("pq", 1), ("f", 9)], wr=[("f", 9)])
                P.op("act", lambda e: e.activation(out=VAR[:, 0:512], in_=VAR[:, 0:512], func=AF.Sqrt, bias=64e-5), rd=[("f", 9)], wr=[("f", 9)])
                P.op("dve", lambda e: e.reciprocal(out=VAR[:, 0:512], in_=VAR[:, 0:512]), rd=[("f", 9)], wr=[("f", 9)])
                P.op("dve", lambda e: e.tensor_tensor(out=Yf[:, 0:512], in0=Yf[:, 0:512], in1=MEAN[:, 0:512], op=ALU.subtract),
                     rd=[("f", 1), ("f", 0)], wr=[("f", 1)])
                P.op("dve", lambda e: e.tensor_tensor(out=Yf[:, 0:512], in0=Yf[:, 0:512], in1=VAR[:, 0:512], op=ALU.mult),
                     rd=[("f", 1), ("f", 9)], wr=[("f", 1)])
                P.op("dve", lambda e, hp=hp: e.tensor_scalar(out=Yf[:, 0:512], in0=Yf[:, 0:512], scalar1=pvc(PV_GW, hp), scalar2=pvc(PV_GB, hp),
                                                             op0=ALU.mult, op1=ALU.add), rd=[("f", 1), "pv"], wr=[("f", 1)])
                P.op("dve", lambda e: e.tensor_tensor(out=Yf[:, 0:512], in0=Yf[:, 0:512], in1=BON[:, 0:512], op=ALU.add),
                     rd=[("f", 1), ("f", 11)], wr=[("f", 1)])
                P.op("dve", lambda e, hp=hp: e.tensor_tensor(out=ybr[:, hp, :], in0=Yf[:, 0:512], in1=sg[:, 0:512], op=ALU.mult),
                     rd=[("f", 1), ("f", 6)], wr=[("y", hp)])

            s = load_w(w_in[l], [(0, C_SK, 64), (64, C_SK, 64), (128, C_SK + 64, 64), (192, C_SK + 64, 64), (256, C_SV, 128)])
            for g in range(2):
                proj_h(s, g, g)
                P.op("act", lambda e, g=g: e.activation(out=kd[:, l * 2 + g, 128:640], in_=pp[g][:], func=AF.Copy), rd=[("pp", g)], wr=[("kd", g)])
            for blk in range(4):
                for kc in range(16):
                    P.op("pe", lambda e, kc=kc, blk=blk, s=s: e.matmul(pp[2][:, blk * 128:(blk + 1) * 128], lhsT=hT[:, kc, blk * 128:(blk + 1) * 128],
                                                                       rhs=wbuf[s][:, kc, 256:384], start=(kc == 0), stop=(kc == 15)),
                         rd=[("w", s), ("h", kc)], wr=[("pp", 2)])
            P.op("act", lambda e: e.activation(out=vt[:, l * 5 + 1:l * 5 + 5, :], in_=pp[2][:].rearrange("p (a b) -> p a b", b=128), func=AF.Copy),
                 rd=[("pp", 2)], wr=["vt"])
            for cp in range(4):
                c0 = 2 * cp
                s = load_w(w_in[l], [(0, C_SQ + c0 * 128, 128), (128, C_SG + c0 * 128, 128), (256, C_SQ + (c0 + 1) * 128, 128),
                                     (384, C_SG + (c0 + 1) * 128, 128)])
                for j in range(4):
                    proj_h(s, j, j)
                for ci in range(2):
                    c = c0 + ci
                    g = c // 4
                    qTb, sgs = bt[0], ft[3]
                    P.op("act", lambda e, ci=ci: e.activation(out=qTb[:], in_=pp[2 * ci][:], func=AF.Copy), rd=[("pp", 2 * ci)], wr=[("b", 0)])
                    P.op("act", lambda e, ci=ci: e.activation(out=sgs[:, 0:512], in_=pp[2 * ci + 1][:], func=AF.Silu), rd=[("pp", 2 * ci + 1)], wr=[("f", 3)])
                    for blk in range(4):
                        bs = slice(blk * 128, (blk + 1) * 128)
                        ex, exm = bt[1], bt[2]
                        for h2 in range(2):
                            hs = slice(h2 * 64, (h2 + 1) * 64)
                            for w_ in range(2):
                                P.op("pe", lambda e, hs=hs, h2=h2, w_=w_, blk=blk, bs=bs, g=g: e.matmul(
                                    pq[h2][:, w_ * 128:(w_ + 1) * 128], lhsT=kd[hs, l * 2 + g, (blk + w_) * 128:(blk + w_ + 1) * 128],
                                    rhs=qTb[hs, bs], start=True, stop=True), rd=[("kd", g), ("b", 0)], wr=[("pq", h2)])
                            P.op("act", lambda e, h2=h2: e.activation(out=ex[:, h2 * 256:(h2 + 1) * 256], in_=pq[h2][:, 0:256], func=AF.Exp, scale=0.125),
                                 rd=[("pq", h2)], wr=[("b", 1)])
                        mk = (maskS0 if (ti == 0 and blk == 0) else maskS).unsqueeze(1).broadcast_to([128, 2, 256])
                        P.op("dve", lambda e, mk=mk: e.tensor_tensor(out=exm[:].rearrange("p (a b) -> p a b", b=256),
                                                                   in0=ex[:].rearrange("p (a b) -> p a b", b=256), in1=mk, op=ALU.mult),
                             rd=[("b", 1), "ccb"], wr=[("b", 2)])
                        for h2 in range(2):
                            hs = slice(h2 * 64, (h2 + 1) * 64)
                            for w_ in range(2):
                                P.op("pe", lambda e, hs=hs, h2=h2, w_=w_, blk=blk, bs=bs, g=g: e.matmul(
                                    pq[2][hs, bs], lhsT=vt[:, l * 5 + blk + w_, g * 64:(g + 1) * 64],
                                    rhs=exm[:, h2 * 256 + w_ * 128:h2 * 256 + (w_ + 1) * 128], start=(w_ == 0), stop=(w_ == 1)),
                                    rd=["vt", ("b", 2)], wr=[("pq", 2)])
                            for w_ in range(2):
                                P.op("pe", lambda e, hs=hs, h2=h2, w_=w_, bs=bs: e.matmul(
                                    pq[3][hs, bs], lhsT=onesB[:, 0:64], rhs=exm[:, h2 * 256 + w_ * 128:h2 * 256 + (w_ + 1) * 128],
                                    start=(w_ == 0), stop=(w_ == 1)), rd=["ccb", ("b", 2)], wr=[("pq", 3)])
                    DEN = ft[4]
                    P.op("dve", lambda e, c=c: e.tensor_scalar(out=DEN[:, 0:512], in0=pq[3][:], scalar1=esink[:, l * 8 + c:l * 8 + c + 1], scalar2=None,
                                                               op0=ALU.add), rd=[("pq", 3), "esink"], wr=[("f", 4)])
                    P.op("dve", lambda e: e.reciprocal(out=DEN[:, 0:512], in_=DEN[:, 0:512]), rd=[("f", 4)], wr=[("f", 4)])
                    P.op("dve", lambda e: e.tensor_tensor(out=DEN[:, 0:512], in0=pq[2][:], in1=DEN[:, 0:512], op=ALU.mult),
                         rd=[("pq", 2), ("f", 4)], wr=[("f", 4)])
                    P.op("dve", lambda e, c=c: e.tensor_tensor(out=ybr[:, 8 + c, :], in0=DEN[:, 0:512], in1=sgs[:, 0:512], op=ALU.mult),
                         rd=[("f", 4), ("f", 3)], wr=[("y", 8 + c)])
            for g in range(2):
                P.op("dve", lambda e, g=g: e.tensor_copy(out=kd[:, l * 2 + g, 0:128], in_=kd[:, l * 2 + g, 512:640]), rd=[("kd", g)], wr=[("kd", g)])
            P.op("dve", lambda e: e.tensor_copy(out=vt[:, l * 5, :], in_=vt[:, l * 5 + 4, :]), rd=["vt"], wr=["vt"])

            for h in range(4):
                s = load_w(w_in[l], [(0, C_XQ + h * 256, 256), (256, C_XG + h * 256, 256)])
                for j in range(4):
                    proj_h(s, j, j)
                qx, sgx = [bt[0], bt[1]], [ft[3], ft[4]]
                for j in range(2):
                    P.op("act", lambda e, j=j: e.activation(out=qx[j][:], in_=pp[j][:], func=AF.Copy), rd=[("pp", j)], wr=[("b", j)])
                    P.op("act", lambda e, j=j: e.activation(out=sgx[j][:, 0:512], in_=pp[2 + j][:], func=AF.Silu), rd=[("pp", 2 + j)], wr=[("f", 3 + j)])
                exx = [bt[2], bt[3]]
                for mt in range(2):
                    for j in range(2):
                        P.op("pe", lambda e, mt=mt, j=j, h=h: e.matmul(pq[mt][:], lhsT=kmT[:, l * 8 + h * 2 + j, mt * 128:(mt + 1) * 128], rhs=qx[j][:],
                                                                       start=(j == 0), stop=(j == 1)), rd=["kmT", ("b", j)], wr=[("pq", mt)])
                    P.op("act", lambda e, mt=mt: e.activation(out=exx[mt][:], in_=pq[mt][:], func=AF.Exp, scale=1.0 / 16), rd=[("pq", mt)], wr=[("b", 2 + mt)])
                for j in range(2):
                    for mt in range(2):
                        P.op("pe", lambda e, mt=mt, j=j, h=h: e.matmul(pp[j][:], lhsT=vm[:, l * 2 + mt, h * 256 + j * 128:h * 256 + (j + 1) * 128],
                                                                       rhs=exx[mt][:], start=(mt == 0), stop=(mt == 1)),
                             rd=["vm", ("b", 2 + mt)], wr=[("pp", j)])
                for mt in range(2):
                    P.op("pe", lambda e, mt=mt: e.matmul(pq[2][:], lhsT=onesB, rhs=exx[mt][:], start=(mt == 0), stop=(mt == 1)),
                         rd=["ccb", ("b", 2 + mt)], wr=[("pq", 2)])
                REC = ft[5]
                P.op("dve", lambda e: e.reciprocal(out=REC[:, 0:512], in_=pq[2][:]), rd=[("pq", 2)], wr=[("f", 5)])
                for j in range(2):
                    P.op("dve", lambda e, j=j: e.tensor_tensor(out=sgx[j][:, 0:512], in0=sgx[j][:, 0:512], in1=REC[:, 0:512], op=ALU.mult),
                         rd=[("f", 3 + j), ("f", 5)], wr=[("f", 3 + j)])
                    P.op("dve", lambda e, j=j, h=h: e.tensor_tensor(out=ybr[:, 16 + h * 2 + j, :], in0=pp[j][:], in1=sgx[j][:, 0:512], op=ALU.mult),
                         rd=[("pp", j), ("f", 3 + j)], wr=[("y", 16 + h * 2 + j)])

            for dg in range(4):
                for br in range(3):
                    s = load_w(w_in[l], [(0, C_MG + br * 2048 + dg * 512, 512)])
                    for j in range(4):
                        proj_h(s, j, j)
                        P.op("act", lambda e, j=j: e.activation(out=ft[3 + j][:, 0:512], in_=pp[j][:], func=AF.Sigmoid), rd=[("pp", j)], wr=[("f", 3 + j)])
                    s = load_w(w_up[br][l], [(0, dg * 512, 512)], nk=8)
                    for j in range(4):
                        proj(s, j, pq[j][:], ("pq", j), lambda kc, br=br: ybr[:, br * 8 + kc, :], lambda kc, br=br: [("y", br * 8 + kc)], nk=8)
                        acc = ft[7 + j]
                        if br == 0:
                            P.op("dve", lambda e, j=j, acc=acc: e.tensor_tensor(out=acc[:, 0:512], in0=pq[j][:], in1=ft[3 + j][:, 0:512], op=ALU.mult),
                                 rd=[("pq", j), ("f", 3 + j)], wr=[("f", 7 + j)])
                        else:
                            P.op("dve", lambda e, j=j: e.tensor_tensor(out=ft[3 + j][:, 0:512], in0=pq[j][:], in1=ft[3 + j][:, 0:512], op=ALU.mult),
                                 rd=[("pq", j), ("f", 3 + j)], wr=[("f", 3 + j)])
                            if br == 1:
                                P.op("dve", lambda e, j=j, acc=acc: e.tensor_tensor(out=acc[:, 0:512], in0=acc[:, 0:512], in1=ft[3 + j][:, 0:512], op=ALU.add),
                                     rd=[("f", 7 + j), ("f", 3 + j)], wr=[("f", 7 + j)])
                            else:
                                dc = dg * 4 + j
                                P.op("dve", lambda e, j=j, acc=acc, dc=dc: e.tensor_tensor(out=bt[dc][:], in0=acc[:, 0:512], in1=ft[3 + j][:, 0:512], op=ALU.add),
                                     rd=[("f", 7 + j), ("f", 3 + j)], wr=[("b", dc)])
            for dg in range(4):
                s = load_w(w_out[l], [(0, dg * 512, 512)])
                for j in range(4):
                    dc = dg * 4 + j
                    proj(s, j, pp[j][:], ("pp", j), lambda kc: bt[kc][:], lambda kc: [("b", kc)])
                    P.op("act", lambda e, j=j, dc=dc: e.activation(out=o_f[:, dc, :], in_=pp[j][:], func=AF.Copy), rd=[("pp", j)], wr=okeys(dc))
                    t = ft[dc % 2]
                    P.op("act", lambda e, dc=dc, t=t: e.activation(out=t[:, 0:512], in_=o_f[:, dc, :], func=AF.Square), rd=okeys(dc), wr=[("f", dc % 2)])
                    P.op("pe", lambda e, dc=dc, t=t: e.matmul(pq[0][:], lhsT=onesF, rhs=t[:, 0:512], start=(dc == 0), stop=(dc == 15)),
                         rd=[("f", dc % 2), "ccf"], wr=[("pq", 0)])
            rs2 = ft[2]
            P.op("act", lambda e: e.activation(out=rs2[:, 0:512], in_=pq[0][:], func=AF.Sqrt, scale=1.0 / D, bias=1e-6), rd=[("pq", 0)], wr=[("f", 2)])
            P.op("dve", lambda e: e.reciprocal(out=rs2[:, 0:512], in_=rs2[:, 0:512]), rd=[("f", 2)], wr=[("f", 2)])
            for dc in range(16):
                t = ft[3 + (dc % 2)]
                P.op("dve", lambda e, dc=dc, t=t: e.scalar_tensor_tensor(out=t[:, 0:512], in0=o_f[:, dc, :], scalar=pvc(PV_GPOST, dc), in1=rs2[:, 0:512],
                                                                         op0=ALU.mult, op1=ALU.mult), rd=okeys(dc) + ["pv", ("f", 2)], wr=[("f", 3 + dc % 2)])
                P.op("dve", lambda e, dc=dc, t=t: e.tensor_tensor(out=x_res[:, dc, :], in0=x_res[:, dc, :], in1=t[:, 0:512], op=ALU.add),
                     rd=[("x", dc), ("f", 3 + dc % 2)], wr=[("x", dc)])
            if l == L - 1:
                for q in range(4):
                    P.dma("sp", lambda e, q=q: e.dma_start(out=outT[q * 512:(q + 1) * 512, t0:t0 + TT].rearrange("(dc p) t -> p dc t", p=128),
                                                           in_=x_res[:, q * 4:(q + 1) * 4, :]),
                          f"o{q}", rd=[("x", q * 4 + i) for i in range(4)], wr=[("out", q)])

        for ti in range(NT):
            for l in range(L):
                block(ti, l)
        P.wait_all("sp", [("out", q) for q in range(4)])
        P.emit()
        print("instructions:", P.n_inst, "sems:", P.sem_id)
    return nc


def _consts():
    cc = np.zeros((128, NCC), np.float32)
    p = np.arange(128)[:, None]
    c = np.arange(128)[None, :]
    cc[:, CC_ONES:CC_ONES + 128] = 1.0
    cc[:, CC_BLK:CC_BLK + 128] = (p // 64 == c // 64)
    cc[:, CC_ID:CC_ID + 128] = (p == c)
    j = p % 64
    t = np.arange(64)[None, :]
    cc[:, CC_MA:CC_MA + 64] = (j <= t)
    cc[:, CC_MA + 64:CC_MA + 128] = (j < t)
    cc[:, CC_ML:CC_ML + 64] = (t < j)
    cc[:, CC_II:CC_II + 64] = (j == t)
    q = np.arange(128)[None, :]
    cc[:, CC_MS:CC_MS + 128] = (p > q)
    cc[:, CC_MS + 128:CC_MS + 256] = (q >= p)
    cc[:, CC_MS0 + 128:CC_MS0 + 256] = (q >= p)
    sc = np.ones((128, 512), np.float32)
    sc[:, 0::64] = 0.0
    cc[:, CC_SCAN:CC_SCAN + 512] = sc
    return cc


def _layout(inputs, L):
    col = lambda v, n: np.ascontiguousarray(v.reshape(n, 128).T)
    pvs = []
    for l in range(L):
        sk = np.repeat(inputs["attn_sinks"][l], 64)
        pvs += [col(inputs["g_pre"][l], 16), col(inputs["g_post"][l], 16), col(inputs["g_mem"][l], 16),
                col(inputs["mu_shift"][l], 25), col(inputs["decay_base"][l], 8), col(inputs["iclr_base"][l], 8),
                col(inputs["k_k"][l], 8), col(inputs["k_a"][l], 8), col(inputs["r_k"][l].reshape(-1), 8),
                col(inputs["gn_w"][l], 8), col(inputs["gn_b"][l], 8), col(sk, 8)]
    pvd = np.ascontiguousarray(np.concatenate(pvs, axis=1), dtype=np.float32)
    dwd = np.ascontiguousarray(np.concatenate([inputs["decay_up"][:L], inputs["iclr_up"][:L]], axis=1), dtype=np.float32)
    return pvd, dwd


def run(inputs, T, L, B, trace=False):
    inputs = {k: np.asarray(v, dtype=np.float32) for k, v in inputs.items()}
    nc = build(T, L)
    pvd, dwd = _layout(inputs, L)
    cc = _consts()
    shared = {
        "w_in": np.ascontiguousarray(inputs["w_in"][:L]), "w_mkv": np.ascontiguousarray(inputs["w_mem_kv"][:L]),
        "w_up0": np.ascontiguousarray(inputs["w_up_rwkv"][:L]), "w_up1": np.ascontiguousarray(inputs["w_up_swa"][:L]),
        "w_up2": np.ascontiguousarray(inputs["w_up_xattn"][:L]), "w_out": np.ascontiguousarray(inputs["w_out"][:L]),
        "dwd": dwd, "pvd": pvd, "ccd": cc,
    }
    in_maps = []
    for b in range(B):
        m = dict(shared)
        m["xT"] = np.ascontiguousarray(inputs["x"][b].T)
        m["memT"] = np.ascontiguousarray(inputs["mem"][b].T)
        in_maps.append(m)
    res = run_bass_kernel_spmd(nc, in_maps, core_ids=list(range(B)), trace=trace)
    out = np.stack([np.ascontiguousarray(r["outT"].T) for r in res.results], axis=0)
    return out.astype(np.float32), res


def kernel(**inputs):
    out, _ = run(inputs, 2048, 2, 8)
    return out
```

```python
import contextlib
import numpy as np
import concourse.bass as bass
import concourse.mybir as mybir
from concourse.bass_utils import run_bass_kernel_spmd

F32 = mybir.dt.float32
BF16 = mybir.dt.bfloat16
AF = mybir.ActivationFunctionType
ALU = mybir.AluOpType

D = 2048
DIN = 14720
MEM = 256
TT = 512
EPOCH = 30000

C_R, C_K, C_V, C_WD = 0, 1024, 2048, 3072
C_RG = 3200
C_SQ = 4224
C_SK = 5248
C_SV = 5376
C_SG = 5504
C_XQ = 6528
C_XG = 7552
C_MG = 8576

PV_GPRE, PV_GPOST, PV_GMEM, PV_MU = 0, 16, 32, 48
PV_DB, PV_IB, PV_KK, PV_KA, PV_RK, PV_GW, PV_GB, PV_SINK = 73, 81, 89, 97, 105, 113, 121, 129
NPV = 137
CC_ONES, CC_BLK, CC_ID, CC_MA, CC_ML, CC_II, CC_MS, CC_MS0, CC_SCAN = 0, 128, 256, 384, 512, 576, 640, 896, 1152
NCC = 1664


class Prog:
    COMPUTE = ("pe", "act", "dve", "pool")

    def __init__(self, nc, stack):
        self.nc, self.stack = nc, stack
        self.eng_names = ("pe", "act", "dve", "pool", "sp")
        self.ops = {e: [] for e in self.eng_names}
        self.cnt = {e: 0 for e in self.COMPUTE}
        self.sem_objs, self.sem_id, self.cur_sem = {}, 0, {}
        for e in self.COMPUTE:
            self.cur_sem[e] = self._new_sem()
        self.waited = {e: {} for e in self.eng_names}
        self.buf, self.dma_sems = {}, {}
        self.n_inst = 0
        self.E = {"pe": nc.tensor, "act": nc.scalar, "dve": nc.vector, "pool": nc.gpsimd, "sp": nc.sync}

    def _new_sem(self):
        s = self.stack.enter_context(self.nc.semaphore(f"s{self.sem_id}"))
        self.sem_objs[self.sem_id] = s
        self.sem_id += 1
        return self.sem_id - 1

    def _deps(self, eng, reads, writes):
        need = {}

        def add(tok):
            sidx, val, teng = tok
            if teng == "pe" and eng == "pe":
                return
            if need.get(sidx, 0) < val:
                need[sidx] = val
        for k in reads:
            st = self.buf.get(k)
            if st and st[0] is not None:
                add(st[0])
        for k in writes:
            st = self.buf.get(k)
            if st:
                if st[0] is not None:
                    add(st[0])
                for t in st[1]:
                    add(t)
        for sidx, val in need.items():
            if self.waited[eng].get(sidx, 0) >= val:
                continue
            self.waited[eng][sidx] = val
            self.E[eng].wait_ge(self.sem_objs[sidx], val)

    def _record(self, tok, reads, writes):
        for k in reads:
            self.buf.setdefault(k, [None, []])[1].append(tok)
        for k in writes:
            self.buf[k] = [tok, []]

    def op(self, eng, fn, rd=(), wr=()):
        self._deps(eng, rd, wr)
        if self.cnt[eng] >= EPOCH:
            self.cur_sem[eng] = self._new_sem()
            self.cnt[eng] = 0
        self.cnt[eng] += 1
        tok = (self.cur_sem[eng], self.cnt[eng], eng)
        fn(self.E[eng]).then_inc(self.sem_objs[self.cur_sem[eng]], 1)
        self._record(tok, rd, wr)
        self.n_inst += 1

    def dma(self, eng, fn, semkey, rd=(), wr=()):
        self._deps(eng, rd, wr)
        if semkey not in self.dma_sems:
            self.dma_sems[semkey] = [self._new_sem(), 0]
        ds = self.dma_sems[semkey]
        ds[1] += 16
        tok = (ds[0], ds[1], "dma")
        fn(self.E[eng]).then_inc(self.sem_objs[ds[0]], 16)
        self._record(tok, rd, wr)
        self.n_inst += 1

    def wait_all(self, eng, keys):
        self._deps(eng, keys, ())

    def emit(self):
        return

    def emit_old(self):
        engmap = {"pe": "tensor", "act": "scalar", "dve": "vector", "pool": "gpsimd", "sp": "sync"}
        with self.nc.Block() as block:
            for e in self.eng_names:
                ops = self.ops[e]
                if not ops:
                    continue

                def body(eng, ops=ops):
                    for o in ops:
                        if o[0] == "wait":
                            eng.wait_ge(self.sem_objs[o[1]], o[2])
                        else:
                            o[1](eng).then_inc(self.sem_objs[o[2]], o[3])
                getattr(block, engmap[e])(body)


def build(T, L):
    NT = T // TT
    nc = bass.Bass("TRN2", target_bir_lowering=False)
    dr = lambda name, shape, kind="ExternalInput": nc.dram_tensor(name, shape, F32, kind=kind).ap()
    xT = dr("xT", [D, T])
    memT = dr("memT", [D, MEM])
    w_in = dr("w_in", [L, D, DIN])
    w_mkv = dr("w_mkv", [L, D, 2048])
    w_up = [dr(f"w_up{i}", [L, 1024, D]) for i in range(3)]
    w_out = dr("w_out", [L, D, D])
    dwd = dr("dwd", [L, 128, 1024])
    pvd = dr("pvd", [128, L * NPV])
    ccd = dr("ccd", [128, NCC])
    outT = dr("outT", [D, T], kind="ExternalOutput")

    with contextlib.ExitStack() as st:
        P = Prog(nc, st)
        sb = lambda name, shape, dt: st.enter_context(nc.sbuf_tensor(name, shape, dt))
        x_res = sb("x_res", [128, 16, TT], F32)
        HY = sb("HY", [128, 10240], F32)
        hT = HY[:, 0:4096].bitcast(BF16).rearrange("p (a b) -> p a b", b=TT)
        ybr = HY[:, 4096:10240].bitcast(BF16).rearrange("p (a b) -> p a b", b=TT)
        o_f = HY[:, 0:8192].rearrange("p (a b) -> p a b", b=TT)

        def okeys(dc):
            return [("h", 2 * dc), ("h", 2 * dc + 1)] if dc < 8 else [("y", 2 * (dc - 8)), ("y", 2 * (dc - 8) + 1)]
        wbuf = [sb(f"wbuf{i}", [128, 16, 512], BF16) for i in range(2)]
        NF, NB = 14, 18
        ft = [sb(f"ft{i}", [128, 516], F32) for i in range(NF)]
        bt = [sb(f"bt{i}", [128, 512], BF16) for i in range(NB)]
        kmT = sb("kmT", [128, L * 8, MEM], BF16)
        vm = sb("vm", [128, L * 2, 1024], BF16)
        kd = sb("kd", [128, L * 2, 640], BF16)
        vt = sb("vt", [128, L * 5, 128], BF16)
        dw = sb("dw", [128, L, 1024], BF16)
        pv = sb("pv", [128, L * NPV], F32)
        omka = sb("omka", [128, L * 8], F32)
        esink = sb("esink", [128, L * 8], F32)
        ccf = sb("ccf", [128, NCC], F32)
        ccb = sb("ccb", [128, NCC], BF16)
        shp = sb("shp", [128, L * 25], F32)
        Hf = sb("Hf", [128, L * 8, 64], F32)
        Hb = sb("Hb", [128, L * 8, 64], BF16)
        ec = sb("ec", [128, 8], F32)
        sm = sb("sm", [128, 256], BF16)
        pp = [st.enter_context(nc.psum_tensor(f"pp{i}", [128, 512], F32)) for i in range(4)]
        pq = [st.enter_context(nc.psum_tensor(f"pq{i}", [128, 512], F32)) for i in range(4)]
        PPK = [("pp", i) for i in range(4)]
        PQK = [("pq", i) for i in range(4)]

        onesF = ccf[:, CC_ONES:CC_ONES + 128]
        onesB = ccb[:, CC_ONES:CC_ONES + 128]
        blkB = ccb[:, CC_BLK:CC_BLK + 128]
        identB = ccb[:, CC_ID:CC_ID + 128]
        maskA = ccf[:, CC_MA:CC_MA + 128]
        maskL = ccf[:, CC_ML:CC_ML + 64]
        identI = ccf[:, CC_II:CC_II + 64]
        maskS = ccb[:, CC_MS:CC_MS + 256]
        maskS0 = ccb[:, CC_MS0:CC_MS0 + 256]
        scanm = ccf[:, CC_SCAN:CC_SCAN + 512]

        P.dma("sp", lambda e: e.dma_start(out=pv[:], in_=pvd), "pv", wr=["pv"])
        P.dma("sp", lambda e: e.dma_start(out=ccf[:], in_=ccd), "ccf", wr=["ccf"])
        P.dma("pool", lambda e: e.dma_start(out=ccb[:], in_=ccd), "ccb", wr=["ccb"])
        for l in range(L):
            P.dma("pool", lambda e, l=l: e.dma_start(out=dw[:, l, :], in_=dwd[l]), "dw", wr=["dw"])
        P.op("dve", lambda e: e.memset(shp[:], 0.0), wr=["shp"])
        P.op("dve", lambda e: e.memset(Hf[:], 0.0), wr=["Hf"])
        P.op("dve", lambda e: e.memset(Hb[:], 0.0), wr=["Hb"])
        P.op("dve", lambda e: e.memset(kd[:], 0.0), wr=["kd"])
        P.op("dve", lambda e: e.memset(vt[:], 0.0), wr=["vt"])
        for l in range(L):
            b = l * NPV
            P.op("dve", lambda e, l=l, b=b: e.tensor_scalar(out=omka[:, l * 8:(l + 1) * 8], in0=pv[:, b + PV_KA:b + PV_KA + 8],
                                                             scalar1=-1.0, scalar2=1.0, op0=ALU.mult, op1=ALU.add), rd=["pv"], wr=["omka"])
            P.op("act", lambda e, l=l, b=b: e.activation(out=esink[:, l * 8:(l + 1) * 8], in_=pv[:, b + PV_SINK:b + PV_SINK + 8], func=AF.Exp),
                 rd=["pv"], wr=["esink"])

        wstate = {"slot": 0, "gid": None, "ti": 0}
        NG = 46
        wscr = nc.dram_tensor("wscr", [L * NG, 128, 8192], BF16, kind="Internal").ap()

        def load_w(src3, pieces, nk=16):
            s = wstate["slot"]
            wstate["slot"] = 1 - s
            gid = wstate["gid"]
            if gid is not None:
                wstate["gid"] = gid + 1
            if gid is not None and wstate["ti"] > 0:
                P.dma("sp", lambda e: e.dma_start(out=wbuf[s][:, 0:nk, :].rearrange("p a b -> p (a b)"), in_=wscr[gid][:, 0:nk * 512]),
                      f"w{s}", rd=[("scr", gid)], wr=[("w", s)])
                return s
            for (doff, c0, n) in pieces:
                P.dma("pool", lambda e, s=s, doff=doff, c0=c0, n=n: e.dma_start(
                    out=wbuf[s][:, 0:nk, doff:doff + n],
                    in_=src3[:, c0:c0 + n].rearrange("(kc p) c -> p kc c", p=128)), f"w{s}", wr=[("w", s)])
            if gid is not None and NT > 1:
                P.dma("sp", lambda e: e.dma_start(out=wscr[gid][:, 0:nk * 512], in_=wbuf[s][:, 0:nk, :].rearrange("p a b -> p (a b)")),
                      f"ws{s}", rd=[("w", s)], wr=[("scr", gid)])
            return s

        def proj(s, j, out_ps, out_key, rhs_fn, rhs_keys, nk=16, ncol=128):
            for kc in range(nk):
                P.op("pe", lambda e, kc=kc: e.matmul(out_ps, lhsT=wbuf[s][:, kc, j * 128:j * 128 + ncol], rhs=rhs_fn(kc),
                                                      start=(kc == 0), stop=(kc == nk - 1)),
                     rd=[("w", s)] + rhs_keys(kc), wr=[out_key])

        HK = [("h", i) for i in range(16)]

        def proj_h(s, j, bank):
            proj(s, j, pp[bank][:], ("pp", bank), lambda kc: hT[:, kc, :], lambda kc: [("h", kc)])

        def rms_stats(src_fn, src_keys, ncols, nchunks, out_rstd, out_key, tmpi):
            for dc in range(nchunks):
                t = ft[tmpi + (dc % 2)]
                P.op("act", lambda e, dc=dc, t=t: e.activation(out=t[:, 0:ncols], in_=src_fn(dc), func=AF.Square),
                     rd=src_keys(dc), wr=[("f", tmpi + (dc % 2))])
                P.op("pe", lambda e, dc=dc, t=t: e.matmul(pq[0][:, 0:ncols], lhsT=onesF, rhs=t[:, 0:ncols],
                                                           start=(dc == 0), stop=(dc == nchunks - 1)),
                     rd=[("f", tmpi + (dc % 2)), "ccf"], wr=[("pq", 0)])
            P.op("act", lambda e: e.activation(out=out_rstd, in_=pq[0][:, 0:ncols], func=AF.Sqrt, scale=1.0 / D, bias=1e-6),
                 rd=[("pq", 0)], wr=[out_key])
            P.op("dve", lambda e: e.reciprocal(out=out_rstd, in_=out_rstd), rd=[out_key], wr=[out_key])

        mT = x_res
        P.dma("sp", lambda e: e.dma_start(out=mT[:, :, 0:MEM], in_=memT.rearrange("(dc p) m -> p dc m", p=128)), "x0",
              wr=[("x", i) for i in range(16)])
        rstd_m = ft[2]
        rms_stats(lambda dc: mT[:, dc, 0:MEM], lambda dc: [("x", dc)], MEM, 16, rstd_m[:, 0:MEM], ("f", 2), 0)
        for l in range(L):
            b = l * NPV
            for dc in range(16):
                P.op("dve", lambda e, dc=dc, b=b: e.scalar_tensor_tensor(out=hT[:, dc, 0:MEM], in0=mT[:, dc, 0:MEM],
                                                                          scalar=pv[:, b + PV_GMEM + dc:b + PV_GMEM + dc + 1],
                                                                          in1=rstd_m[:, 0:MEM], op0=ALU.mult, op1=ALU.mult),
                     rd=[("x", dc), "pv", ("f", 2)], wr=[("h", dc)])
            for g in range(4):
                s = load_w(w_mkv[l], [(0, g * 512, 512)])
                if g < 2:
                    for j in range(4):
                        proj(s, j, pp[j][:, 0:MEM], ("pp", j), lambda kc: hT[:, kc, 0:MEM], lambda kc: [("h", kc)])
                        ci = l * 8 + g * 4 + j
                        P.op("act", lambda e, j=j, ci=ci: e.activation(out=kmT[:, ci, :], in_=pp[j][:, 0:MEM], func=AF.Copy),
                             rd=[("pp", j)], wr=["kmT"])
                else:
                    for mt in range(2):
                        for kc in range(16):
                            P.op("pe", lambda e, kc=kc, mt=mt, s=s: e.matmul(pp[mt][:], lhsT=hT[:, kc, mt * 128:(mt + 1) * 128],
                                                                                rhs=wbuf[s][:, kc, :], start=(kc == 0), stop=(kc == 15)),
                                 rd=[("w", s), ("h", kc)], wr=[("pp", mt)])
                        P.op("act", lambda e, mt=mt, l=l, g=g: e.activation(out=vm[:, l * 2 + mt, (g - 2) * 512:(g - 1) * 512], in_=pp[mt][:],
                                                                             func=AF.Copy), rd=[("pp", mt)], wr=["vm"])

        def block(ti, l):
            b = l * NPV
            wstate["gid"] = l * NG
            wstate["ti"] = ti
            pvc = lambda off, c: pv[:, b + off + c:b + off + c + 1]
            t0 = ti * TT
            if l == 0:
                for q in range(4):
                    P.dma("sp", lambda e, q=q: e.dma_start(out=x_res[:, q * 4:(q + 1) * 4, :],
                                                           in_=xT[q * 512:(q + 1) * 512, t0:t0 + TT].rearrange("(dc p) t -> p dc t", p=128)),
                          f"x{q}", wr=[("x", q * 4 + i) for i in range(4)])
            rstd = ft[2]
            rms_stats(lambda dc: x_res[:, dc, :], lambda dc: [("x", dc)], TT, 16, rstd[:, 0:TT], ("f", 2), 0)
            for dc in range(16):
                P.op("dve", lambda e, dc=dc: e.scalar_tensor_tensor(out=hT[:, dc, :], in0=x_res[:, dc, :], scalar=pvc(PV_GPRE, dc),
                                                                     in1=rstd[:, 0:TT], op0=ALU.mult, op1=ALU.mult),
                     rd=[("x", dc), "pv", ("f", 2)], wr=[("h", dc)])

            def shift(bank, fi_raw, fi_out, chunk_idx, rows=slice(0, 128)):
                raw = ft[fi_raw]
                si = l * 25 + chunk_idx
                P.op("act", lambda e: e.activation(out=raw[:, 1:513], in_=pp[bank][:], func=AF.Copy), rd=[("pp", bank)], wr=[("f", fi_raw)])
                P.op("dve", lambda e: e.tensor_copy(out=raw[:, 0:1], in_=shp[:, si:si + 1]), rd=["shp"], wr=[("f", fi_raw)])
                P.op("dve", lambda e: e.tensor_copy(out=shp[:, si:si + 1], in_=raw[:, 512:513]), rd=[("f", fi_raw)], wr=["shp"])
                P.op("dve", lambda e: e.tensor_tensor(out=ft[fi_out][:, 0:512], in0=raw[:, 0:512], in1=raw[:, 1:513], op=ALU.subtract),
                     rd=[("f", fi_raw)], wr=[("f", fi_out)])
                P.op("dve", lambda e: e.scalar_tensor_tensor(out=ft[fi_out][:, 0:512], in0=ft[fi_out][:, 0:512], scalar=pvc(PV_MU, chunk_idx),
                                                             in1=raw[:, 1:513], op0=ALU.mult, op1=ALU.add),
                     rd=[("f", fi_out), ("f", fi_raw), "pv"], wr=[("f", fi_out)])

            s = load_w(w_in[l], [(0, C_WD, 128)])
            proj_h(s, 0, 0)
            shift(0, 0, 1, 24)
            twd, adb = bt[16], bt[17]
            P.op("act", lambda e: e.activation(out=twd[0:64, :], in_=ft[1][0:64, 0:512], func=AF.Tanh), rd=[("f", 1)], wr=[("b", 16)])
            P.op("dve", lambda e: e.tensor_copy(out=adb[64:128, :], in_=ft[1][64:128, 0:512]), rd=[("f", 1)], wr=[("b", 17)])
            v3 = lambda ap: ap.rearrange("p (n t) -> p n t", t=64)
            for hp in range(8):
                s = load_w(w_in[l], [(0, C_R + hp * 128, 128), (128, C_K + hp * 128, 128), (256, C_V + hp * 128, 128),
                                     (384, C_RG + hp * 128, 128)])
                for j in range(4):
                    proj_h(s, j, j)
                Rs, Ks, Vs = ft[3], ft[4], ft[5]
                shift(0, 0, 3, hp)
                shift(1, 1, 4, 8 + hp)
                shift(2, 0, 5, 16 + hp)
                sg = ft[6]
                P.op("act", lambda e: e.activation(out=sg[:, 0:512], in_=pp[3][:], func=AF.Silu), rd=[("pp", 3)], wr=[("f", 6)])
                P.op("pe", lambda e, hp=hp: e.matmul(pq[0][:], lhsT=dw[0:64, l, hp * 128:(hp + 1) * 128], rhs=twd[0:64, :], start=True, stop=True),
                     rd=["dw", ("b", 16)], wr=[("pq", 0)])
                P.op("pe", lambda e, hp=hp: e.matmul(pq[1][:], lhsT=dw[64:128, l, hp * 128:(hp + 1) * 128], rhs=adb[64:128, :], start=True, stop=True),
                     rd=["dw", ("b", 17)], wr=[("pq", 1)])
                LW, A = ft[7], ft[8]
                P.op("act", lambda e, hp=hp: e.activation(out=LW[:, 0:512], in_=pq[0][:], func=AF.Sigmoid, bias=pvc(PV_DB, hp)),
                     rd=[("pq", 0), "pv"], wr=[("f", 7)])
                P.op("act", lambda e, hp=hp: e.activation(out=A[:, 0:512], in_=pq[1][:], func=AF.Sigmoid, bias=pvc(PV_IB, hp)),
                     rd=[("pq", 1), "pv"], wr=[("f", 8)])
                KK, TMP = ft[9], ft[0]
                P.op("dve", lambda e, hp=hp: e.tensor_scalar(out=KK[:, 0:512], in0=Ks[:, 0:512], scalar1=pvc(PV_KK, hp), scalar2=None, op0=ALU.mult),
                     rd=[("f", 4), "pv"], wr=[("f", 9)])
                P.op("dve", lambda e: e.tensor_tensor(out=bt[0][:], in0=KK[:, 0:512], in1=KK[:, 0:512], op=ALU.mult), rd=[("f", 9)], wr=[("b", 0)])
                P.op("pe", lambda e: e.matmul(pq[2][:], lhsT=blkB, rhs=bt[0][:], start=True, stop=True), rd=["ccb", ("b", 0)], wr=[("pq", 2)])
                P.op("act", lambda e: e.activation(out=TMP[:, 0:512], in_=pq[2][:], func=AF.Sqrt), rd=[("pq", 2)], wr=[("f", 0)])
                P.op("dve", lambda e: e.tensor_scalar(out=TMP[:, 0:512], in0=TMP[:, 0:512], scalar1=1e-12, scalar2=None, op0=ALU.max),
                     rd=[("f", 0)], wr=[("f", 0)])
                P.op("dve", lambda e: e.reciprocal(out=TMP[:, 0:512], in_=TMP[:, 0:512]), rd=[("f", 0)], wr=[("f", 0)])
                P.op("dve", lambda e: e.tensor_tensor(out=KK[:, 0:512], in0=KK[:, 0:512], in1=TMP[:, 0:512], op=ALU.mult),
                     rd=[("f", 9), ("f", 0)], wr=[("f", 9)])
                K2 = ft[10]
                P.op("dve", lambda e, hp=hp: e.tensor_scalar(out=K2[:, 0:512], in0=A[:, 0:512], scalar1=pvc(PV_KA, hp),
                                                             scalar2=omka[:, l * 8 + hp:l * 8 + hp + 1], op0=ALU.mult, op1=ALU.add),
                     rd=[("f", 8), "pv", "omka"], wr=[("f", 10)])
                P.op("dve", lambda e: e.tensor_tensor(out=K2[:, 0:512], in0=K2[:, 0:512], in1=Ks[:, 0:512], op=ALU.mult),
                     rd=[("f", 10), ("f", 4)], wr=[("f", 10)])
                P.op("dve", lambda e, hp=hp: e.scalar_tensor_tensor(out=bt[0][:], in0=Rs[:, 0:512], scalar=pvc(PV_RK, hp), in1=K2[:, 0:512],
                                                                    op0=ALU.mult, op1=ALU.mult), rd=[("f", 3), ("f", 10), "pv"], wr=[("b", 0)])
                P.op("pe", lambda e: e.matmul(pq[3][:], lhsT=blkB, rhs=bt[0][:], start=True, stop=True), rd=["ccb", ("b", 0)], wr=[("pq", 3)])
                BON = ft[11]
                P.op("dve", lambda e: e.tensor_tensor(out=BON[:, 0:512], in0=pq[3][:], in1=Vs[:, 0:512], op=ALU.mult),
                     rd=[("pq", 3), ("f", 5)], wr=[("f", 11)])
                CUM = ft[12]
                P.op("act", lambda e: e.mul(out=LW[:, 0:512], in_=LW[:, 0:512], mul=-0.6065306597126334), rd=[("f", 7)], wr=[("f", 7)])
                P.op("dve", lambda e: e.tensor_tensor_scan(out=CUM[:, 0:512], data0=scanm, data1=LW[:, 0:512], initial=0.0,
                                                           op0=ALU.mult, op1=ALU.add), rd=["ccf", ("f", 7)], wr=[("f", 12)])
                EP, EM = ft[13], ft[0]
                P.op("act", lambda e: e.activation(out=EP[:, 0:512], in_=CUM[:, 0:512], func=AF.Exp), rd=[("f", 12)], wr=[("f", 13)])
                P.op("dve", lambda e: e.tensor_copy(out=ec[:], in_=v3(EP[:, 0:512])[:, :, 63]), rd=[("f", 13)], wr=["ec"])
                RAh, KBh = [bt[1], bt[3]], [bt[2], bt[4]]
                RA4 = [x_[:].rearrange("p (n w t) -> p n w t", w=2, t=64) for x_ in RAh]
                KB4 = [x_[:].rearrange("p (n w t) -> p n w t", w=2, t=64) for x_ in KBh]
                RAK, KBK = [("b", 1), ("b", 3)], [("b", 2), ("b", 4)]
                for h in range(2):
                    P.op("dve", lambda e, h=h: e.tensor_tensor(out=RA4[h][:, :, 0, :], in0=v3(Rs[:, h * 256:(h + 1) * 256]),
                                                               in1=v3(EP[:, h * 256:(h + 1) * 256]), op=ALU.mult),
                         rd=[("f", 3), ("f", 13)], wr=[RAK[h]])
                P.op("dve", lambda e: e.tensor_tensor(out=EM[:, 0:512], in0=CUM[:, 0:512], in1=LW[:, 0:512], op=ALU.subtract),
                     rd=[("f", 12), ("f", 7)], wr=[("f", 0)])
                P.op("act", lambda e: e.activation(out=EM[:, 0:512], in_=EM[:, 0:512], func=AF.Exp), rd=[("f", 0)], wr=[("f", 0)])
                for h in range(2):
                    P.op("dve", lambda e, h=h: e.scalar_tensor_tensor(out=RA4[h][:, :, 1, :], in0=v3(KK[:, h * 256:(h + 1) * 256]), scalar=-1.0,
                                                                      in1=v3(EM[:, h * 256:(h + 1) * 256]), op0=ALU.mult, op1=ALU.mult),
                         rd=[("f", 9), ("f", 0)], wr=[RAK[h]])
                EN = ft[13]
                P.op("act", lambda e: e.activation(out=EN[:, 0:512], in_=CUM[:, 0:512], func=AF.Exp, scale=-1.0), rd=[("f", 12)], wr=[("f", 13)])
                BP = ft[7]
                P.op("dve", lambda e: e.tensor_tensor(out=BP[:, 0:512], in0=KK[:, 0:512], in1=A[:, 0:512], op=ALU.mult),
                     rd=[("f", 9), ("f", 8)], wr=[("f", 7)])
                for h in range(2):
                    sl = slice(h * 256, (h + 1) * 256)
                    P.op("dve", lambda e, h=h, sl=sl: e.tensor_tensor(out=KB4[h][:, :, 0, :], in0=v3(K2[:, sl]), in1=v3(EN[:, sl]), op=ALU.mult),
                         rd=[("f", 10), ("f", 13)], wr=[KBK[h]])
                    P.op("dve", lambda e, h=h, sl=sl: e.tensor_tensor(out=KB4[h][:, :, 1, :], in0=v3(BP[:, sl]), in1=v3(EN[:, sl]), op=ALU.mult),
                         rd=[("f", 7), ("f", 13)], wr=[KBK[h]])
                Vb = bt[5]
                P.op("act", lambda e: e.activation(out=Vb[:], in_=Vs[:, 0:512], func=AF.Copy), rd=[("f", 5)], wr=[("b", 5)])
                VT, KIT, BIT = bt[6], bt[7], bt[8]
                pqb = [pq[i][:].bitcast(BF16) for i in range(4)]
                for (dst, di, bank, srcfn, skeys) in (
                        (VT, 6, 0, lambda n, hs: Vb[hs, n * 64:(n + 1) * 64], lambda n: [("b", 5)]),
                        (KIT, 7, 1, lambda n, hs: KB4[n // 4][hs, n % 4, 0, :], lambda n: [KBK[n // 4]]),
                        (BIT, 8, 2, lambda n, hs: KB4[n // 4][hs, n % 4, 1, :], lambda n: [KBK[n // 4]])):
                    for n in range(8):
                        for h2 in range(2):
                            hs = slice(h2 * 64, (h2 + 1) * 64)
                            P.op("pe", lambda e, n=n, hs=hs, bank=bank, srcfn=srcfn: e.transpose(
                                out=pqb[bank][hs, n * 64:(n + 1) * 64], in_=srcfn(n, hs), identity=identB[hs, hs]),
                                rd=skeys(n) + ["ccb"], wr=[("pq", bank)])
                    P.op("act" if di != 7 else "dve",
                         (lambda e, dst=dst, bank=bank: e.activation(out=dst[:], in_=pqb[bank][:, 0:512], func=AF.Copy)) if di != 7 else
                         (lambda e, dst=dst, bank=bank: e.tensor_copy(out=dst[:], in_=pqb[bank][:, 0:512])),
                         rd=[("pq", bank)], wr=[("b", di)])
                ATk, ATb, X0 = [bt[9], bt[10]], [bt[11], bt[12]], bt[13]
                for h in range(2):
                    for n4 in range(4):
                        for h2 in range(2):
                            hs = slice(h2 * 64, (h2 + 1) * 64)
                            P.op("pe", lambda e, h=h, n4=n4, hs=hs: e.matmul(pq[0][hs, n4 * 128:(n4 + 1) * 128], lhsT=KB4[h][hs, n4, 0, :],
                                                                             rhs=RA4[h][hs, n4, :, :], start=True, stop=True),
                                 rd=[KBK[h], RAK[h]], wr=[("pq", 0)])
                            P.op("pe", lambda e, h=h, n4=n4, hs=hs: e.matmul(pq[1][hs, n4 * 128:(n4 + 1) * 128], lhsT=KB4[h][hs, n4, 1, :],
                                                                             rhs=RA4[h][hs, n4, :, :], start=True, stop=True),
                                 rd=[KBK[h], RAK[h]], wr=[("pq", 1)])
                            P.op("pe", lambda e, h=h, n4=n4, hs=hs: e.matmul(pq[2][hs, n4 * 64:(n4 + 1) * 64], lhsT=RA4[h][hs, n4, 1, :],
                                                                             rhs=KB4[h][hs, n4, 1, :], start=True, stop=True),
                                 rd=[KBK[h], RAK[h]], wr=[("pq", 2)])
                    mA = maskA.unsqueeze(1).broadcast_to([128, 4, 128])
                    mL = maskL.unsqueeze(1).broadcast_to([128, 4, 64])
                    P.op("dve", lambda e, h=h, mA=mA: e.tensor_tensor(out=ATk[h][:].rearrange("p (n c) -> p n c", c=128),
                                                                     in0=pq[0][:].rearrange("p (n c) -> p n c", c=128), in1=mA, op=ALU.mult),
                         rd=[("pq", 0), "ccf"], wr=[("b", 9 + h)])
                    P.op("dve", lambda e, h=h, mA=mA: e.tensor_tensor(out=ATb[h][:].rearrange("p (n c) -> p n c", c=128),
                                                                     in0=pq[1][:].rearrange("p (n c) -> p n c", c=128), in1=mA, op=ALU.mult),
                         rd=[("pq", 1), "ccf"], wr=[("b", 11 + h)])
                    P.op("dve", lambda e, h=h, mL=mL: e.tensor_tensor(out=X0[:, h * 256:(h + 1) * 256].rearrange("p (n c) -> p n c", c=64),
                                                                     in0=pq[2][:, 0:256].rearrange("p (n c) -> p n c", c=64), in1=mL, op=ALU.mult),
                         rd=[("pq", 2), "ccf"], wr=[("b", 13)])
                ATk3 = [a[:].rearrange("p (n c) -> p n c", c=128) for a in ATk]
                ATb3 = [a[:].rearrange("p (n c) -> p n c", c=128) for a in ATb]
                Xt0 = bt[14]
                for h in range(2):
                    P.op("act", lambda e, h=h: e.activation(out=Xt0[:, h * 256:(h + 1) * 256].rearrange("p (n c) -> p n c", c=64),
                                                            in_=ATb3[h][:, :, 64:128], func=AF.Copy), rd=[("b", 11 + h)], wr=[("b", 14)])
                Mt = bt[15]
                iI = identI.unsqueeze(1).broadcast_to([128, 8, 64])
                P.op("dve", lambda e: e.tensor_tensor(out=v3(Mt[:]), in0=v3(Xt0[:]), in1=iI, op=ALU.add), rd=[("b", 14), "ccf"], wr=[("b", 15)])
                Xc, Xtc, xi, xti = X0, Xt0, 13, 14
                alt = {13: 0, 14: 16 - 16}
                pingX, pingXt = [(bt[13], 13), (bt[0], 0)], [(bt[14], 14), (bt[5], 5)]
                for lvl in range(1, 6):
                    Xn, xni = pingX[lvl % 2]
                    Xtn, xtni = pingXt[lvl % 2]
                    for n in range(8):
                        for h2 in range(2):
                            hs = slice(h2 * 64, (h2 + 1) * 64)
                            cs = slice(n * 64, (n + 1) * 64)
                            P.op("pe", lambda e, hs=hs, cs=cs, Xc=Xc, Xtc=Xtc: e.matmul(pq[0][hs, cs], lhsT=Xc[hs, cs], rhs=Xtc[hs, cs], start=True, stop=True),
                                 rd=[("b", xi), ("b", xti)], wr=[("pq", 0)])
                            P.op("pe", lambda e, hs=hs, cs=cs, Xc=Xc, Xtc=Xtc: e.matmul(pq[1][hs, cs], lhsT=Xtc[hs, cs], rhs=Xc[hs, cs], start=True, stop=True),
                                 rd=[("b", xi), ("b", xti)], wr=[("pq", 1)])
                    P.op("act", lambda e, Xtn=Xtn: e.activation(out=Xtn[:], in_=pq[0][:], func=AF.Copy), rd=[("pq", 0)], wr=[("b", xtni)])
                    P.op("dve", lambda e, Xn=Xn: e.tensor_copy(out=Xn[:], in_=pq[1][:]), rd=[("pq", 1)], wr=[("b", xni)])
                    for n in range(8):
                        for h2 in range(2):
                            hs = slice(h2 * 64, (h2 + 1) * 64)
                            cs = slice(n * 64, (n + 1) * 64)
                            P.op("pe", lambda e, hs=hs, cs=cs, Xn=Xn: e.matmul(pq[2][hs, cs], lhsT=Xn[hs, cs], rhs=Mt[hs, cs], start=True, stop=True),
                                 rd=[("b", xni), ("b", 15)], wr=[("pq", 2)])
                    P.op("dve", lambda e: e.tensor_tensor(out=Mt[:], in0=pq[2][:], in1=Mt[:], op=ALU.add), rd=[("pq", 2), ("b", 15)], wr=[("b", 15)])
                    Xc, Xtc, xi, xti = Xn, Xtn, xni, xtni
                hh = l * 8 + hp
                R0b, Ub = sm[:, 0:64], sm[:, 64:128]
                TH = ft[0]
                for n in range(8):
                    h, n4 = n // 4, n % 4
                    cs = slice(n * 64, (n + 1) * 64)
                    for h2 in range(2):
                        hs = slice(h2 * 64, (h2 + 1) * 64)
                        P.op("pe", lambda e, hs=hs, h=h, n4=n4: e.matmul(pq[3][hs, 0:64], lhsT=RA4[h][hs, n4, 1, :], rhs=Hb[hs, hh, :], start=True, stop=False),
                             rd=[RAK[h], "Hb"], wr=[("pq", 3)])
                        P.op("pe", lambda e, hs=hs, h=h, n4=n4, cs=cs: e.matmul(pq[3][hs, 0:64], lhsT=ATk3[h][hs, n4, 64:128], rhs=VT[hs, cs], start=False, stop=True),
                             rd=[("b", 9 + h), ("b", 6)], wr=[("pq", 3)])
                    P.op("act", lambda e: e.activation(out=R0b, in_=pq[3][:, 0:64], func=AF.Copy), rd=[("pq", 3)], wr=["sm0"])
                    for h2 in range(2):
                        hs = slice(h2 * 64, (h2 + 1) * 64)
                        P.op("pe", lambda e, hs=hs, cs=cs: e.matmul(pq[3][hs, 64:128], lhsT=Mt[hs, cs], rhs=sm[hs, 0:64], start=True, stop=True),
                             rd=[("b", 15), "sm0"], wr=[("pq", 3)])
                    P.op("dve", lambda e: e.tensor_copy(out=Ub, in_=pq[3][:, 64:128]), rd=[("pq", 3)], wr=["sm1"])
                    for h2 in range(2):
                        hs = slice(h2 * 64, (h2 + 1) * 64)
                        P.op("pe", lambda e, hs=hs, h=h, n4=n4, cs=cs: e.matmul(pp[0][hs, cs], lhsT=Hb[hs, hh, :], rhs=RA4[h][hs, n4, 0, :], start=True, stop=False),
                             rd=["Hb", RAK[h]], wr=[("pp", 0)])
                        P.op("pe", lambda e, hs=hs, h=h, n4=n4, cs=cs: e.matmul(pp[0][hs, cs], lhsT=sm[hs, 64:128], rhs=ATb3[h][hs, n4, 0:64], start=False, stop=False),
                             rd=["sm1", ("b", 11 + h)], wr=[("pp", 0)])
                        P.op("pe", lambda e, hs=hs, h=h, n4=n4, cs=cs: e.matmul(pp[0][hs, cs], lhsT=VT[hs, cs], rhs=ATk3[h][hs, n4, 0:64], start=False, stop=True),
                             rd=[("b", 6), ("b", 9 + h)], wr=[("pp", 0)])
                        P.op("pe", lambda e, hs=hs, cs=cs: e.matmul(pq[3][hs, 128:192], lhsT=KIT[hs, cs], rhs=VT[hs, cs], start=True, stop=False),
                             rd=[("b", 7), ("b", 6)], wr=[("pq", 3)])
                        P.op("pe", lambda e, hs=hs, cs=cs: e.matmul(pq[3][hs, 128:192], lhsT=BIT[hs, cs], rhs=sm[hs, 64:128], start=False, stop=True),
                             rd=[("b", 8), "sm1"], wr=[("pq", 3)])
                    P.op("dve", lambda e: e.tensor_tensor(out=TH[:, 0:64], in0=pq[3][:, 128:192], in1=Hf[:, hh, :], op=ALU.add),
                         rd=[("pq", 3), "Hf"], wr=[("f", 0)])
                    P.op("dve", lambda e, n=n: e.tensor_scalar(out=Hf[:, hh, :], in0=TH[:, 0:64], scalar1=ec[:, n:n + 1], scalar2=None, op0=ALU.mult),
                         rd=[("f", 0), "ec"], wr=["Hf"])
                    P.op("act", lambda e: e.activation(out=Hb[:, hh, :], in_=Hf[:, hh, :], func=AF.Copy), rd=["Hf"], wr=["Hb"])
                Yf, Yb, Y2 = ft[1], bt[0], bt[5]
                P.op("act", lambda e: e.activation(out=Yf[:, 0:512], in_=pp[0][:], func=AF.Copy), rd=[("pp", 0)], wr=[("f", 1)])
                P.op("dve", lambda e: e.tensor_copy(out=Yb[:], in_=Yf[:, 0:512]), rd=[("f", 1)], wr=[("b", 0)])
                P.op("dve", lambda e: e.tensor_tensor(out=Y2[:], in0=Yf[:, 0:512], in1=Yf[:, 0:512], op=ALU.mult), rd=[("f", 1)], wr=[("b", 5)])
                P.op("pe", lambda e: e.matmul(pq[0][:], lhsT=blkB, rhs=Yb[:], start=True, stop=True), rd=["ccb", ("b", 0)], wr=[("pq", 0)])
                P.op("pe", lambda e: e.matmul(pq[1][:], lhsT=blkB, rhs=Y2[:], start=True, stop=True), rd=["ccb", ("b", 5)], wr=[("pq", 1)])
                MEAN, VAR = ft[0], ft[9]
                P.op("dve", lambda e: e.tensor_scalar(out=MEAN[:, 0:512], in0=pq[0][:], scalar1=1.0 / 64, scalar2=None, op0=ALU.mult),
                     rd=[("pq", 0)], wr=[("f", 0)])
                P.op("dve", lambda e: e.tensor_tensor(out=VAR[:, 0:512], in0=MEAN[:, 0:512], in1=MEAN[:, 0:512], op=ALU.mult), rd=[("f", 0)], wr=[("f", 9)])
                P.op("dve", lambda e: e.scalar_tensor_tensor(out=VAR[:, 0:512], in0=pq[1][:], scalar=1.0 / 64, in1=VAR[:, 0:512],
                                                             op0=ALU.mult, op1=ALU.subtract), rd=[("pq", 1), ("f", 9)], wr=[("f", 9)])
                P.op("act", lambda e: e.activation(out=VAR[:, 0:512], in_=VAR[:, 0:512], func=AF.Sqrt, bias=64e-5), rd=[("f", 9)], wr=[("f", 9)])
                P.op("dve", lambda e: e.reciprocal(out=VAR[:, 0:512], in_=VAR[:, 0:512]), rd=[("f", 9)], wr=[("f", 9)])
                P.op("dve", lambda e: e.tensor_tensor(out=Yf[:, 0:512], in0=Yf[:, 0:512], in1=MEAN[:, 0:512], op=ALU.subtract),
                     rd=[("f", 1), ("f", 0)], wr=[("f", 1)])
                P.op("dve", lambda e: e.tensor_tensor(out=Yf[:, 0:512], in0=Yf[:, 0:512], in1=VAR[:, 0:512], op=ALU.mult),
                     rd=[("f", 1), ("f", 9)], wr=[("f", 1)])
                P.op("dve", lambda e, hp=hp: e.tensor_scalar(out=Yf[:, 0:512], in0=Yf[:, 0:512], scalar1=pvc(PV_GW, hp), scalar2=pvc(PV_GB, hp),
                                                             op0=ALU.mult, op1=ALU.add), rd=[("f", 1), "pv"], wr=[("f", 1)])
                P.op("dve", lambda e: e.tensor_tensor(out=Yf[:, 0:512], in0=Yf[:, 0:512], in1=BON[:, 0:512], op=ALU.add),
                     rd=[("f", 1), ("f", 11)], wr=[("f", 1)])
                P.op("dve", lambda e, hp=hp: e.tensor_tensor(out=ybr[:, hp, :], in0=Yf[:, 0:512], in1=sg[:, 0:512], op=ALU.mult),
                     rd=[("f", 1), ("f", 6)], wr=[("y", hp)])

            s = load_w(w_in[l], [(0, C_SK, 64), (64, C_SK, 64), (128, C_SK + 64, 64), (192, C_SK + 64, 64), (256, C_SV, 128)])
            for g in range(2):
                proj_h(s, g, g)
                P.op("act", lambda e, g=g: e.activation(out=kd[:, l * 2 + g, 128:640], in_=pp[g][:], func=AF.Copy), rd=[("pp", g)], wr=[("kd", g)])
            for blk in range(4):
                for kc in range(16):
                    P.op("pe", lambda e, kc=kc, blk=blk, s=s: e.matmul(pp[2][:, blk * 128:(blk + 1) * 128], lhsT=hT[:, kc, blk * 128:(blk + 1) * 128],
                                                                       rhs=wbuf[s][:, kc, 256:384], start=(kc == 0), stop=(kc == 15)),
                         rd=[("w", s), ("h", kc)], wr=[("pp", 2)])
            P.op("act", lambda e: e.activation(out=vt[:, l * 5 + 1:l * 5 + 5, :], in_=pp[2][:].rearrange("p (a b) -> p a b", b=128), func=AF.Copy),
                 rd=[("pp", 2)], wr=["vt"])
            for cp in range(4):
                c0 = 2 * cp
                s = load_w(w_in[l], [(0, C_SQ + c0 * 128, 128), (128, C_SG + c0 * 128, 128), (256, C_SQ + (c0 + 1) * 128, 128),
                                     (384, C_SG + (c0 + 1) * 128, 128)])
                for j in range(4):
                    proj_h(s, j, j)
                for ci in range(2):
                    c = c0 + ci
                    g = c // 4
                    qTb, sgs = bt[0], ft[3]
                    P.op("act", lambda e, ci=ci: e.activation(out=qTb[:], in_=pp[2 * ci][:], func=AF.Copy), rd=[("pp", 2 * ci)], wr=[("b", 0)])
                    P.op("act", lambda e, ci=ci: e.activation(out=sgs[:, 0:512], in_=pp[2 * ci + 1][:], func=AF.Silu), rd=[("pp", 2 * ci + 1)], wr=[("f", 3)])
                    for blk in range(4):
                        bs = slice(blk * 128, (blk + 1) * 128)
                        ex, exm = bt[1], bt[2]
                        for h2 in range(2):
                            hs = slice(h2 * 64, (h2 + 1) * 64)
                            for w_ in range(2):
                                P.op("pe", lambda e, hs=hs, h2=h2, w_=w_, blk=blk, bs=bs, g=g: e.matmul(
                                    pq[h2][:, w_ * 128:(w_ + 1) * 128], lhsT=kd[hs, l * 2 + g, (blk + w_) * 128:(blk + w_ + 1) * 128],
                                    rhs=qTb[hs, bs], start=True, stop=True), rd=[("kd", g), ("b", 0)], wr=[("pq", h2)])
                            P.op("act", lambda e, h2=h2: e.activation(out=ex[:, h2 * 256:(h2 + 1) * 256], in_=pq[h2][:, 0:256], func=AF.Exp, scale=0.125),
                                 rd=[("pq", h2)], wr=[("b", 1)])
                        mk = (maskS0 if (ti == 0 and blk == 0) else maskS).unsqueeze(1).broadcast_to([128, 2, 256])
                        P.op("dve", lambda e, mk=mk: e.tensor_tensor(out=exm[:].rearrange("p (a b) -> p a b", b=256),
                                                                   in0=ex[:].rearrange("p (a b) -> p a b", b=256), in1=mk, op=ALU.mult),
                             rd=[("b", 1), "ccb"], wr=[("b", 2)])
                        for h2 in range(2):
                            hs = slice(h2 * 64, (h2 + 1) * 64)
                            for w_ in range(2):
                                P.op("pe", lambda e, hs=hs, h2=h2, w_=w_, blk=blk, bs=bs, g=g: e.matmul(
                                    pq[2][hs, bs], lhsT=vt[:, l * 5 + blk + w_, g * 64:(g + 1) * 64],
                                    rhs=exm[:, h2 * 256 + w_ * 128:h2 * 256 + (w_ + 1) * 128], start=(w_ == 0), stop=(w_ == 1)),
                                    rd=["vt", ("b", 2)], wr=[("pq", 2)])
                            for w_ in range(2):
                                P.op("pe", lambda e, hs=hs, h2=h2, w_=w_, bs=bs: e.matmul(
                                    pq[3][hs, bs], lhsT=onesB[:, 0:64], rhs=exm[:, h2 * 256 + w_ * 128:h2 * 256 + (w_ + 1) * 128],
                                    start=(w_ == 0), stop=(w_ == 1)), rd=["ccb", ("b", 2)], wr=[("pq", 3)])
                    DEN = ft[4]
                    P.op("dve", lambda e, c=c: e.tensor_scalar(out=DEN[:, 0:512], in0=pq[3][:], scalar1=esink[:, l * 8 + c:l * 8 + c + 1], scalar2=None,
                                                               op0=ALU.add), rd=[("pq", 3), "esink"], wr=[("f", 4)])
                    P.op("dve", lambda e: e.reciprocal(out=DEN[:, 0:512], in_=DEN[:, 0:512]), rd=[("f", 4)], wr=[("f", 4)])
                    P.op("dve", lambda e: e.tensor_tensor(out=DEN[:, 0:512], in0=pq[2][:], in1=DEN[:, 0:512], op=ALU.mult),
                         rd=[("pq", 2), ("f", 4)], wr=[("f", 4)])
                    P.op("dve", lambda e, c=c: e.tensor_tensor(out=ybr[:, 8 + c, :], in0=DEN[:, 0:512], in1=sgs[:, 0:512], op=ALU.mult),
                         rd=[("f", 4), ("f", 3)], wr=[("y", 8 + c)])
            for g in range(2):
                P.op("dve", lambda e, g=g: e.tensor_copy(out=kd[:, l * 2 + g, 0:128], in_=kd[:, l * 2 + g, 512:640]), rd=[("kd", g)], wr=[("kd", g)])
            P.op("dve", lambda e: e.tensor_copy(out=vt[:, l * 5, :], in_=vt[:, l * 5 + 4, :]), rd=["vt"], wr=["vt"])

            for h in range(4):
                s = load_w(w_in[l], [(0, C_XQ + h * 256, 256), (256, C_XG + h * 256, 256)])
                for j in range(4):
                    proj_h(s, j, j)
                qx, sgx = [bt[0], bt[1]], [ft[3], ft[4]]
                for j in range(2):
                    P.op("act", lambda e, j=j: e.activation(out=qx[j][:], in_=pp[j][:], func=AF.Copy), rd=[("pp", j)], wr=[("b", j)])
                    P.op("act", lambda e, j=j: e.activation(out=sgx[j][:, 0:512], in_=pp[2 + j][:], func=AF.Silu), rd=[("pp", 2 + j)], wr=[("f", 3 + j)])
                exx = [bt[2], bt[3]]
                for mt in range(2):
                    for j in range(2):
                        P.op("pe", lambda e, mt=mt, j=j, h=h: e.matmul(pq[mt][:], lhsT=kmT[:, l * 8 + h * 2 + j, mt * 128:(mt + 1) * 128], rhs=qx[j][:],
                                                                       start=(j == 0), stop=(j == 1)), rd=["kmT", ("b", j)], wr=[("pq", mt)])
                    P.op("act", lambda e, mt=mt: e.activation(out=exx[mt][:], in_=pq[mt][:], func=AF.Exp, scale=1.0 / 16), rd=[("pq", mt)], wr=[("b", 2 + mt)])
                for j in range(2):
                    for mt in range(2):
                        P.op("pe", lambda e, mt=mt, j=j, h=h: e.matmul(pp[j][:], lhsT=vm[:, l * 2 + mt, h * 256 + j * 128:h * 256 + (j + 1) * 128],
                                                                       rhs=exx[mt][:], start=(mt == 0), stop=(mt == 1)),
                             rd=["vm", ("b", 2 + mt)], wr=[("pp", j)])
                for mt in range(2):
                    P.op("pe", lambda e, mt=mt: e.matmul(pq[2][:], lhsT=onesB, rhs=exx[mt][:], start=(mt == 0), stop=(mt == 1)),
                         rd=["ccb", ("b", 2 + mt)], wr=[("pq", 2)])
                REC = ft[5]
                P.op("dve", lambda e: e.reciprocal(out=REC[:, 0:512], in_=pq[2][:]), rd=[("pq", 2)], wr=[("f", 5)])
                for j in range(2):
                    P.op("dve", lambda e, j=j: e.tensor_tensor(out=sgx[j][:, 0:512], in0=sgx[j][:, 0:512], in1=REC[:, 0:512], op=ALU.mult),
                         rd=[("f", 3 + j), ("f", 5)], wr=[("f", 3 + j)])
                    P.op("dve", lambda e, j=j, h=h: e.tensor_tensor(out=ybr[:, 16 + h * 2 + j, :], in0=pp[j][:], in1=sgx[j][:, 0:512], op=ALU.mult),
                         rd=[("pp", j), ("f", 3 + j)], wr=[("y", 16 + h * 2 + j)])

            for dg in range(4):
                for br in range(3):
                    s = load_w(w_in[l], [(0, C_MG + br * 2048 + dg * 512, 512)])
                    for j in range(4):
                        proj_h(s, j, j)
                        P.op("act", lambda e, j=j: e.activation(out=ft[3 + j][:, 0:512], in_=pp[j][:], func=AF.Sigmoid), rd=[("pp", j)], wr=[("f", 3 + j)])
                    s = load_w(w_up[br][l], [(0, dg * 512, 512)], nk=8)
                    for j in range(4):
                        proj(s, j, pq[j][:], ("pq", j), lambda kc, br=br: ybr[:, br * 8 + kc, :], lambda kc, br=br: [("y", br * 8 + kc)], nk=8)
                        acc = ft[7 + j]
                        if br == 0:
                            P.op("dve", lambda e, j=j, acc=acc: e.tensor_tensor(out=acc[:, 0:512], in0=pq[j][:], in1=ft[3 + j][:, 0:512], op=ALU.mult),
                                 rd=[("pq", j), ("f", 3 + j)], wr=[("f", 7 + j)])
                        else:
                            P.op("dve", lambda e, j=j: e.tensor_tensor(out=ft[3 + j][:, 0:512], in0=pq[j][:], in1=ft[3 + j][:, 0:512], op=ALU.mult),
                                 rd=[("pq", j), ("f", 3 + j)], wr=[("f", 3 + j)])
                            if br == 1:
                                P.op("dve", lambda e, j=j, acc=acc: e.tensor_tensor(out=acc[:, 0:512], in0=acc[:, 0:512], in1=ft[3 + j][:, 0:512], op=ALU.add),
                                     rd=[("f", 7 + j), ("f", 3 + j)], wr=[("f", 7 + j)])
                            else:
                                dc = dg * 4 + j
                                P.op("dve", lambda e, j=j, acc=acc, dc=dc: e.tensor_tensor(out=bt[dc][:], in0=acc[:, 0:512], in1=ft[3 + j][:, 0:512], op=ALU.add),
                                     rd=[("f", 7 + j), ("f", 3 + j)], wr=[("b", dc)])
            for dg in range(4):
                s = load_w(w_out[l], [(0, dg * 512, 512)])
                for j in range(4):
                    dc = dg * 4 + j
                    proj(s, j, pp[j][:], ("pp", j), lambda kc: bt[kc][:], lambda kc: [("b", kc)])
                    P.op("act", lambda e, j=j, dc=dc: e.activation(out=o_f[:, dc, :], in_=pp[j][:], func=AF.Copy), rd=[("pp", j)], wr=okeys(dc))
                    t = ft[dc % 2]
                    P.op("act", lambda e, dc=dc, t=t: e.activation(out=t[:, 0:512], in_=o_f[:, dc, :], func=AF.Square), rd=okeys(dc), wr=[("f", dc % 2)])
                    P.op("pe", lambda e, dc=dc, t=t: e.matmul(pq[0][:], lhsT=onesF, rhs=t[:, 0:512], start=(dc == 0), stop=(dc == 15)),
                         rd=[("f", dc % 2), "ccf"], wr=[("pq", 0)])
            rs2 = ft[2]
            P.op("act", lambda e: e.activation(out=rs2[:, 0:512], in_=pq[0][:], func=AF.Sqrt, scale=1.0 / D, bias=1e-6), rd=[("pq", 0)], wr=[("f", 2)])
            P.op("dve", lambda e: e.reciprocal(out=rs2[:, 0:512], in_=rs2[:, 0:512]), rd=[("f", 2)], wr=[("f", 2)])
            for dc in range(16):
                t = ft[3 + (dc % 2)]
                P.op("dve", lambda e, dc=dc, t=t: e.scalar_tensor_tensor(out=t[:, 0:512], in0=o_f[:, dc, :], scalar=pvc(PV_GPOST, dc), in1=rs2[:, 0:512],
                                                                         op0=ALU.mult, op1=ALU.mult), rd=okeys(dc) + ["pv", ("f", 2)], wr=[("f", 3 + dc % 2)])
                P.op("dve", lambda e, dc=dc, t=t: e.tensor_tensor(out=x_res[:, dc, :], in0=x_res[:, dc, :], in1=t[:, 0:512], op=ALU.add),
                     rd=[("x", dc), ("f", 3 + dc % 2)], wr=[("x", dc)])
            if l == L - 1:
                for q in range(4):
                    P.dma("sp", lambda e, q=q: e.dma_start(out=outT[q * 512:(q + 1) * 512, t0:t0 + TT].rearrange("(dc p) t -> p dc t", p=128),
                                                           in_=x_res[:, q * 4:(q + 1) * 4, :]),
                          f"o{q}", rd=[("x", q * 4 + i) for i in range(4)], wr=[("out", q)])

        for ti in range(NT):
            for l in range(L):
                block(ti, l)
        P.wait_all("sp", [("out", q) for q in range(4)])
        P.emit()
        print("instructions:", P.n_inst, "sems:", P.sem_id)
    return nc


def _consts():
    cc = np.zeros((128, NCC), np.float32)
    p = np.arange(128)[:, None]
    c = np.arange(128)[None, :]
    cc[:, CC_ONES:CC_ONES + 128] = 1.0
    cc[:, CC_BLK:CC_BLK + 128] = (p // 64 == c // 64)
    cc[:, CC_ID:CC_ID + 128] = (p == c)
    j = p % 64
    t = np.arange(64)[None, :]
    cc[:, CC_MA:CC_MA + 64] = (j <= t)
    cc[:, CC_MA + 64:CC_MA + 128] = (j < t)
    cc[:, CC_ML:CC_ML + 64] = (t < j)
    cc[:, CC_II:CC_II + 64] = (j == t)
    q = np.arange(128)[None, :]
    cc[:, CC_MS:CC_MS + 128] = (p > q)
    cc[:, CC_MS + 128:CC_MS + 256] = (q >= p)
    cc[:, CC_MS0 + 128:CC_MS0 + 256] = (q >= p)
    sc = np.ones((128, 512), np.float32)
    sc[:, 0::64] = 0.0
    cc[:, CC_SCAN:CC_SCAN + 512] = sc
    return cc


def _layout(inputs, L):
    col = lambda v, n: np.ascontiguousarray(v.reshape(n, 128).T)
    pvs = []
    for l in range(L):
        sk = np.repeat(inputs["attn_sinks"][l], 64)
        pvs += [col(inputs["g_pre"][l], 16), col(inputs["g_post"][l], 16), col(inputs["g_mem"][l], 16),
                col(inputs["mu_shift"][l], 25), col(inputs["decay_base"][l], 8), col(inputs["iclr_base"][l], 8),
                col(inputs["k_k"][l], 8), col(inputs["k_a"][l], 8), col(inputs["r_k"][l].reshape(-1), 8),
                col(inputs["gn_w"][l], 8), col(inputs["gn_b"][l], 8), col(sk, 8)]
    pvd = np.ascontiguousarray(np.concatenate(pvs, axis=1), dtype=np.float32)
    dwd = np.ascontiguousarray(np.concatenate([inputs["decay_up"][:L], inputs["iclr_up"][:L]], axis=1), dtype=np.float32)
    return pvd, dwd


def run(inputs, T, L, B, trace=False):
    inputs = {k: np.asarray(v, dtype=np.float32) for k, v in inputs.items()}
    nc = build(T, L)
    pvd, dwd = _layout(inputs, L)
    cc = _consts()
    shared = {
        "w_in": np.ascontiguousarray(inputs["w_in"][:L]), "w_mkv": np.ascontiguousarray(inputs["w_mem_kv"][:L]),
        "w_up0": np.ascontiguousarray(inputs["w_up_rwkv"][:L]), "w_up1": np.ascontiguousarray(inputs["w_up_swa"][:L]),
        "w_up2": np.ascontiguousarray(inputs["w_up_xattn"][:L]), "w_out": np.ascontiguousarray(inputs["w_out"][:L]),
        "dwd": dwd, "pvd": pvd, "ccd": cc,
    }
    in_maps = []
    for b in range(B):
        m = dict(shared)
        m["xT"] = np.ascontiguousarray(inputs["x"][b].T)
        m["memT"] = np.ascontiguousarray(inputs["mem"][b].T)
        in_maps.append(m)
    res = run_bass_kernel_spmd(nc, in_maps, core_ids=list(range(B)), trace=trace)
    out = np.stack([np.ascontiguousarray(r["outT"].T) for r in res.results], axis=0)
    return out.astype(np.float32), res


def kernel(**inputs):
    out, _ = run(inputs, 2048, 2, 8)
    return out
```

```python
import contextlib
import numpy as np
import concourse.bass as bass
import concourse.mybir as mybir
from concourse.bass_utils import run_bass_kernel_spmd

F32 = mybir.dt.float32
BF16 = mybir.dt.bfloat16
AF = mybir.ActivationFunctionType
ALU = mybir.AluOpType

D = 2048
DIN = 14720
MEM = 256
TT = 512
EPOCH = 30000

C_R, C_K, C_V, C_WD = 0, 1024, 2048, 3072
C_RG = 3200
C_SQ = 4224
C_SK = 5248
C_SV = 5376
C_SG = 5504
C_XQ = 6528
C_XG = 7552
C_MG = 8576

PV_GPRE, PV_GPOST, PV_GMEM, PV_MU = 0, 16, 32, 48
PV_DB, PV_IB, PV_KK, PV_KA, PV_RK, PV_GW, PV_GB, PV_SINK = 73, 81, 89, 97, 105, 113, 121, 129
NPV = 137
CF_ONES, CF_MA, CF_ML, CF_II, CF_SCAN = 0, 128, 256, 320, 384
CB_ONES, CB_BLK, CB_ID, CB_MS, CB_MS0 = 0, 128, 256, 384, 640
NCC = 896


class Prog:
    COMPUTE = ("pe", "act", "dve", "pool")

    def __init__(self, nc, stack):
        self.nc, self.stack = nc, stack
        self.eng_names = ("pe", "act", "dve", "pool", "sp")
        self.ops = {e: [] for e in self.eng_names}
        self.cnt = {e: 0 for e in self.COMPUTE}
        self.sem_objs, self.sem_id, self.cur_sem = {}, 0, {}
        for e in self.COMPUTE:
            self.cur_sem[e] = self._new_sem()
        self.waited = {e: {} for e in self.eng_names}
        self.buf, self.dma_sems = {}, {}
        self.n_inst = 0
        self.E = {"pe": nc.tensor, "act": nc.scalar, "dve": nc.vector, "pool": nc.gpsimd, "sp": nc.sync}

    def _new_sem(self):
        s = self.stack.enter_context(self.nc.semaphore(f"s{self.sem_id}"))
        self.sem_objs[self.sem_id] = s
        self.sem_id += 1
        return self.sem_id - 1

    def _deps(self, eng, reads, writes):
        need = {}

        def add(tok):
            sidx, val, teng = tok
            if teng == "pe" and eng == "pe":
                return
            if need.get(sidx, 0) < val:
                need[sidx] = val
        for k in reads:
            st = self.buf.get(k)
            if st and st[0] is not None:
                add(st[0])
        for k in writes:
            st = self.buf.get(k)
            if st:
                if st[0] is not None:
                    add(st[0])
                for t in st[1]:
                    add(t)
        for sidx, val in need.items():
            if self.waited[eng].get(sidx, 0) >= val:
                continue
            self.waited[eng][sidx] = val
            self.E[eng].wait_ge(self.sem_objs[sidx], val)

    def _record(self, tok, reads, writes):
        for k in reads:
            self.buf.setdefault(k, [None, []])[1].append(tok)
        for k in writes:
            self.buf[k] = [tok, []]

    def op(self, eng, fn, rd=(), wr=()):
        self._deps(eng, rd, wr)
        if self.cnt[eng] >= EPOCH:
            self.cur_sem[eng] = self._new_sem()
            self.cnt[eng] = 0
        self.cnt[eng] += 1
        tok = (self.cur_sem[eng], self.cnt[eng], eng)
        fn(self.E[eng]).then_inc(self.sem_objs[self.cur_sem[eng]], 1)
        self._record(tok, rd, wr)
        self.n_inst += 1

    def dma(self, eng, fn, semkey, rd=(), wr=()):
        self._deps(eng, rd, wr)
        if semkey not in self.dma_sems:
            self.dma_sems[semkey] = [self._new_sem(), 0]
        ds = self.dma_sems[semkey]
        ds[1] += 16
        tok = (ds[0], ds[1], "dma")
        fn(self.E[eng]).then_inc(self.sem_objs[ds[0]], 16)
        self._record(tok, rd, wr)
        self.n_inst += 1

    def wait_all(self, eng, keys):
        self._deps(eng, keys, ())

    def emit(self):
        return

    def emit_old(self):
        engmap = {"pe": "tensor", "act": "scalar", "dve": "vector", "pool": "gpsimd", "sp": "sync"}
        with self.nc.Block() as block:
            for e in self.eng_names:
                ops = self.ops[e]
                if not ops:
                    continue

                def body(eng, ops=ops):
                    for o in ops:
                        if o[0] == "wait":
                            eng.wait_ge(self.sem_objs[o[1]], o[2])
                        else:
                            o[1](eng).then_inc(self.sem_objs[o[2]], o[3])
                getattr(block, engmap[e])(body)


def build(T, L):
    NT = T // TT
    nc = bass.Bass("TRN2", target_bir_lowering=False)
    dr = lambda name, shape, kind="ExternalInput": nc.dram_tensor(name, shape, F32, kind=kind).ap()
    xT = dr("xT", [D, T])
    memT = dr("memT", [D, MEM])
    w_in = dr("w_in", [L, D, DIN])
    w_mkv = dr("w_mkv", [L, D, 2048])
    w_up = [dr(f"w_up{i}", [L, 1024, D]) for i in range(3)]
    w_out = dr("w_out", [L, D, D])
    dwd = dr("dwd", [L, 128, 1024])
    pvd = dr("pvd", [128, L * NPV])
    ccdf = dr("ccdf", [128, NCC])
    ccdb = dr("ccdb", [128, NCC])
    outT = dr("outT", [D, T], kind="ExternalOutput")

    with contextlib.ExitStack() as st:
        P = Prog(nc, st)
        sb = lambda name, shape, dt: st.enter_context(nc.sbuf_tensor(name, shape, dt))
        x_res = sb("x_res", [128, 16, TT], F32)
        HY = sb("HY", [128, 10240], F32)
        hT = HY[:, 0:4096].bitcast(BF16).rearrange("p (a b) -> p a b", b=TT)
        ybr = HY[:, 4096:10240].bitcast(BF16).rearrange("p (a b) -> p a b", b=TT)
        o_f = HY[:, 0:8192].rearrange("p (a b) -> p a b", b=TT)

        def okeys(dc):
            return [("h", 2 * dc), ("h", 2 * dc + 1)] if dc < 8 else [("y", 2 * (dc - 8)), ("y", 2 * (dc - 8) + 1)]
        wbuf = [sb(f"wbuf{i}", [128, 16, 512], BF16) for i in range(2)]
        NF, NB = 16, 24
        ft = [sb(f"ft{i}", [128, 514], F32) for i in range(NF)]
        bt_all = sb("bt_all", [128, NB, 512], BF16)
        bt = [bt_all[:, i, :] for i in range(NB)]
        f32v = lambda i: bt_all[:, i:i + 2, :].rearrange("p a b -> p (a b)").bitcast(F32)
        kmT = sb("kmT", [128, L * 8, MEM], BF16)
        vm = sb("vm", [128, L * 2, 1024], BF16)
        kd = sb("kd", [128, L * 2, 640], BF16)
        vt = sb("vt", [128, L * 5, 128], BF16)
        dw = sb("dw", [128, L, 1024], BF16)
        pv = sb("pv", [128, L * NPV], F32)
        omka = sb("omka", [128, L * 8], F32)
        esink = sb("esink", [128, L * 8], F32)
        ccf = sb("ccf", [128, NCC], F32)
        ccb = sb("ccb", [128, NCC], BF16)
        gs = sb("gs", [128, 64], F32)
        shp = sb("shp", [128, L * 25], F32)
        Hf = sb("Hf", [128, L * 8, 64], F32)
        Hb = sb("Hb", [128, L * 8, 64], BF16)
        ec = sb("ec", [128, 2, 8], F32)
        sm = sb("sm", [128, 256], BF16)
        pp = [st.enter_context(nc.psum_tensor(f"pp{i}", [128, 512], F32)) for i in range(4)]
        pq = [st.enter_context(nc.psum_tensor(f"pq{i}", [128, 512], F32)) for i in range(4)]
        PPK = [("pp", i) for i in range(4)]
        PQK = [("pq", i) for i in range(4)]

        onesF = ccf[:, CF_ONES:CF_ONES + 128]
        onesB = ccb[:, CB_ONES:CB_ONES + 128]
        blkB = ccb[:, CB_BLK:CB_BLK + 128]
        identB = ccb[:, CB_ID:CB_ID + 128]
        maskA = ccf[:, CF_MA:CF_MA + 128]
        maskL = ccf[:, CF_ML:CF_ML + 64]
        identI = ccf[:, CF_II:CF_II + 64]
        maskS = ccb[:, CB_MS:CB_MS + 256]
        maskS0 = ccb[:, CB_MS0:CB_MS0 + 256]
        scanm = ccf[:, CF_SCAN:CF_SCAN + 512]

        P.dma("sp", lambda e: e.dma_start(out=pv[:], in_=pvd), "pv", wr=["pv"])
        P.dma("sp", lambda e: e.dma_start(out=ccf[:], in_=ccdf), "ccf", wr=["ccf"])
        P.dma("pool", lambda e: e.dma_start(out=ccb[:], in_=ccdb), "ccb", wr=["ccb"])
        for l in range(L):
            P.dma("pool", lambda e, l=l: e.dma_start(out=dw[:, l, :], in_=dwd[l]), "dw", wr=["dw"])
        P.op("dve", lambda e: e.memset(shp[:], 0.0), wr=["shp"])
        P.op("dve", lambda e: e.memset(Hf[:], 0.0), wr=["Hf"])
        P.op("dve", lambda e: e.memset(Hb[:], 0.0), wr=["Hb"])
        P.op("dve", lambda e: e.memset(kd[:], 0.0), wr=["kd"])
        P.op("dve", lambda e: e.memset(vt[:], 0.0), wr=["vt"])
        for l in range(L):
            b = l * NPV
            P.op("dve", lambda e, l=l, b=b: e.tensor_scalar(out=omka[:, l * 8:(l + 1) * 8], in0=pv[:, b + PV_KA:b + PV_KA + 8],
                                                             scalar1=-1.0, scalar2=1.0, op0=ALU.mult, op1=ALU.add), rd=["pv"], wr=["omka"])
            P.op("act", lambda e, l=l, b=b: e.activation(out=esink[:, l * 8:(l + 1) * 8], in_=pv[:, b + PV_SINK:b + PV_SINK + 8], func=AF.Exp),
                 rd=["pv"], wr=["esink"])

        wstate = {"slot": 0, "gid": None, "ti": 0}
        NG = 46
        wscr = nc.dram_tensor("wscr", [L * NG, 128, 8192], BF16, kind="Internal").ap()

        def load_w(src3, pieces, nk=16):
            s = wstate["slot"]
            wstate["slot"] = 1 - s
            gid = wstate["gid"]
            if gid is not None:
                wstate["gid"] = gid + 1
            if gid is not None and wstate["ti"] > 0:
                P.dma("sp", lambda e: e.dma_start(out=wbuf[s][:, 0:nk, :].rearrange("p a b -> p (a b)"), in_=wscr[gid][:, 0:nk * 512]),
                      f"w{s}", rd=[("scr", gid)], wr=[("w", s)])
                return s
            for (doff, c0, n) in pieces:
                P.dma("pool", lambda e, s=s, doff=doff, c0=c0, n=n: e.dma_start(
                    out=wbuf[s][:, 0:nk, doff:doff + n],
                    in_=src3[:, c0:c0 + n].rearrange("(kc p) c -> p kc c", p=128)), f"w{s}", wr=[("w", s)])
            if gid is not None and NT > 1:
                P.dma("sp", lambda e: e.dma_start(out=wscr[gid][:, 0:nk * 512], in_=wbuf[s][:, 0:nk, :].rearrange("p a b -> p (a b)")),
                      f"ws{s}", rd=[("w", s)], wr=[("scr", gid)])
            return s

        def proj(s, j, out_ps, out_key, rhs_fn, rhs_keys, nk=16, ncol=128):
            for kc in range(nk):
                P.op("pe", lambda e, kc=kc: e.matmul(out_ps, lhsT=wbuf[s][:, kc, j * 128:j * 128 + ncol], rhs=rhs_fn(kc),
                                                      start=(kc == 0), stop=(kc == nk - 1)),
                     rd=[("w", s)] + rhs_keys(kc), wr=[out_key])

        HK = [("h", i) for i in range(16)]

        def proj_h(s, j, bank):
            proj(s, j, pp[bank][:], ("pp", bank), lambda kc: hT[:, kc, :], lambda kc: [("h", kc)])

        def rms_stats(src_fn, src_keys, ncols, nchunks, out_rstd, out_key, tmpi):
            for dc in range(nchunks):
                t = ft[tmpi + (dc % 2)]
                P.op("act", lambda e, dc=dc, t=t: e.activation(out=t[:, 0:ncols], in_=src_fn(dc), func=AF.Square),
                     rd=src_keys(dc), wr=[("f", tmpi + (dc % 2))])
                P.op("pe", lambda e, dc=dc, t=t: e.matmul(pq[0][:, 0:ncols], lhsT=onesF, rhs=t[:, 0:ncols],
                                                           start=(dc == 0), stop=(dc == nchunks - 1)),
                     rd=[("f", tmpi + (dc % 2)), "ccf"], wr=[("pq", 0)])
            P.op("act", lambda e: e.activation(out=out_rstd, in_=pq[0][:, 0:ncols], func=AF.Sqrt, scale=1.0 / D, bias=1e-6),
                 rd=[("pq", 0)], wr=[out_key])
            P.op("dve", lambda e: e.reciprocal(out=out_rstd, in_=out_rstd), rd=[out_key], wr=[out_key])

        mT = x_res
        P.dma("sp", lambda e: e.dma_start(out=mT[:, :, 0:MEM], in_=memT.rearrange("(dc p) m -> p dc m", p=128)), "x0",
              wr=[("x", i) for i in range(16)])
        rstd_m = ft[2]
        rms_stats(lambda dc: mT[:, dc, 0:MEM], lambda dc: [("x", dc)], MEM, 16, rstd_m[:, 0:MEM], ("f", 2), 0)
        for l in range(L):
            b = l * NPV
            for dc in range(16):
                P.op("dve", lambda e, dc=dc, b=b: e.scalar_tensor_tensor(out=hT[:, dc, 0:MEM], in0=mT[:, dc, 0:MEM],
                                                                          scalar=pv[:, b + PV_GMEM + dc:b + PV_GMEM + dc + 1],
                                                                          in1=rstd_m[:, 0:MEM], op0=ALU.mult, op1=ALU.mult),
                     rd=[("x", dc), "pv", ("f", 2)], wr=[("h", dc)])
            for g in range(4):
                s = load_w(w_mkv[l], [(0, g * 512, 512)])
                if g < 2:
                    for j in range(4):
                        proj(s, j, pp[j][:, 0:MEM], ("pp", j), lambda kc: hT[:, kc, 0:MEM], lambda kc: [("h", kc)])
                        ci = l * 8 + g * 4 + j
                        P.op("act", lambda e, j=j, ci=ci: e.activation(out=kmT[:, ci, :], in_=pp[j][:, 0:MEM], func=AF.Copy),
                             rd=[("pp", j)], wr=["kmT"])
                else:
                    for mt in range(2):
                        for kc in range(16):
                            P.op("pe", lambda e, kc=kc, mt=mt, s=s: e.matmul(pp[mt][:], lhsT=hT[:, kc, mt * 128:(mt + 1) * 128],
                                                                                rhs=wbuf[s][:, kc, :], start=(kc == 0), stop=(kc == 15)),
                                 rd=[("w", s), ("h", kc)], wr=[("pp", mt)])
                        P.op("act", lambda e, mt=mt, l=l, g=g: e.activation(out=vm[:, l * 2 + mt, (g - 2) * 512:(g - 1) * 512], in_=pp[mt][:],
                                                                             func=AF.Copy), rd=[("pp", mt)], wr=["vm"])

        def block(ti, l):
            b = l * NPV
            wstate["gid"] = l * NG
            wstate["ti"] = ti
            pvc = lambda off, c: pv[:, b + off + c:b + off + c + 1]
            t0 = ti * TT
            if l == 0:
                for q in range(4):
                    P.dma("sp", lambda e, q=q: e.dma_start(out=x_res[:, q * 4:(q + 1) * 4, :],
                                                           in_=xT[q * 512:(q + 1) * 512, t0:t0 + TT].rearrange("(dc p) t -> p dc t", p=128)),
                          f"x{q}", wr=[("x", q * 4 + i) for i in range(4)])
            rstd = ft[2]
            rms_stats(lambda dc: x_res[:, dc, :], lambda dc: [("x", dc)], TT, 16, rstd[:, 0:TT], ("f", 2), 0)
            for dc in range(16):
                P.op("dve", lambda e, dc=dc: e.scalar_tensor_tensor(out=hT[:, dc, :], in0=x_res[:, dc, :], scalar=pvc(PV_GPRE, dc),
                                                                     in1=rstd[:, 0:TT], op0=ALU.mult, op1=ALU.mult),
                     rd=[("x", dc), "pv", ("f", 2)], wr=[("h", dc)])

            def shift(bank, fi_raw, fi_out, chunk_idx, rows=slice(0, 128)):
                raw = ft[fi_raw]
                si = l * 25 + chunk_idx
                P.op("act", lambda e: e.activation(out=raw[:, 1:513], in_=pp[bank][:], func=AF.Copy), rd=[("pp", bank)], wr=[("f", fi_raw)])
                P.op("dve", lambda e: e.tensor_copy(out=raw[:, 0:1], in_=shp[:, si:si + 1]), rd=["shp"], wr=[("f", fi_raw)])
                P.op("dve", lambda e: e.tensor_copy(out=shp[:, si:si + 1], in_=raw[:, 512:513]), rd=[("f", fi_raw)], wr=["shp"])
                P.op("dve", lambda e: e.tensor_tensor(out=ft[fi_out][:, 0:512], in0=raw[:, 0:512], in1=raw[:, 1:513], op=ALU.subtract),
                     rd=[("f", fi_raw)], wr=[("f", fi_out)])
                P.op("dve", lambda e: e.scalar_tensor_tensor(out=ft[fi_out][:, 0:512], in0=ft[fi_out][:, 0:512], scalar=pvc(PV_MU, chunk_idx),
                                                             in1=raw[:, 1:513], op0=ALU.mult, op1=ALU.add),
                     rd=[("f", fi_out), ("f", fi_raw), "pv"], wr=[("f", fi_out)])

            s = load_w(w_in[l], [(0, C_WD, 128)])
            proj_h(s, 0, 0)
            shift(0, 0, 1, 24)
            twd, adb = bt[16], bt[17]
            P.op("act", lambda e: e.activation(out=twd[0:64, :], in_=ft[1][0:64, 0:512], func=AF.Tanh), rd=[("f", 1)], wr=[("b", 16)])
            P.op("dve", lambda e: e.tensor_copy(out=adb[64:128, :], in_=ft[1][64:128, 0:512]), rd=[("f", 1)], wr=[("b", 17)])
            v3 = lambda ap: ap.rearrange("p (n t) -> p n t", t=64)
            RAH = [[(bt[1], 1), (bt[3], 3)], [(bt[19], 19), (bt[20], 20)]]
            KBH = [[(bt[2], 2), (bt[4], 4)], [(bt[21], 21), (bt[22], 22)]]
            VBS = [(bt[5], 5), (bt[23], 23)]
            BONS = [(ft[11], 11), (ft[14], 14)]
            SGS = [(ft[6], 6), (ft[15], 15)]
            r4 = lambda x_: x_.rearrange("p (n w t) -> p n w t", w=2, t=64)

            def rwkv_A(hp, par):
                RA4 = [r4(RAH[par][h][0]) for h in range(2)]
                KB4 = [r4(KBH[par][h][0]) for h in range(2)]
                RAK = [("b", RAH[par][h][1]) for h in range(2)]
                KBK = [("b", KBH[par][h][1]) for h in range(2)]
                Vb, vbi = VBS[par]
                BON, boni = BONS[par]
                sg, sgi = SGS[par]
                ecp, eck = ec[:, par, :], ("ec", par)
                s = load_w(w_in[l], [(0, C_R + hp * 128, 128), (128, C_K + hp * 128, 128), (256, C_V + hp * 128, 128),
                                     (384, C_RG + hp * 128, 128)])
                for j in range(4):
                    for k4 in range(4):
                        for kc in range(k4 * 4, k4 * 4 + 4):
                            P.op("pe", lambda e, kc=kc, j=j: e.matmul(pp[j][:], lhsT=wbuf[s][:, kc, j * 128:(j + 1) * 128], rhs=hT[:, kc, :],
                                                                      start=(kc == 0), stop=(kc == 15)), rd=[("w", s), ("h", kc)], wr=[("pp", j)])
                        yield
                Rs, Ks, Vs = ft[3], ft[4], ft[5]
                shift(0, 0, 3, hp)
                yield
                shift(1, 1, 4, 8 + hp)
                yield
                shift(2, 0, 5, 16 + hp)
                yield
                P.op("act", lambda e: e.activation(out=sg[:, 0:512], in_=pp[3][:], func=AF.Silu), rd=[("pp", 3)], wr=[("f", sgi)])
                P.op("pe", lambda e: e.matmul(pp[0][:], lhsT=dw[0:64, l, hp * 128:(hp + 1) * 128], rhs=twd[0:64, :], start=True, stop=True),
                     rd=["dw", ("b", 16)], wr=[("pp", 0)])
                P.op("pe", lambda e: e.matmul(pp[1][:], lhsT=dw[64:128, l, hp * 128:(hp + 1) * 128], rhs=adb[64:128, :], start=True, stop=True),
                     rd=["dw", ("b", 17)], wr=[("pp", 1)])
                yield
                LW, A = ft[7], ft[8]
                P.op("act", lambda e: e.activation(out=LW[:, 0:512], in_=pp[0][:], func=AF.Sigmoid, bias=pvc(PV_DB, hp)),
                     rd=[("pp", 0), "pv"], wr=[("f", 7)])
                P.op("act", lambda e: e.activation(out=A[:, 0:512], in_=pp[1][:], func=AF.Sigmoid, bias=pvc(PV_IB, hp)),
                     rd=[("pp", 1), "pv"], wr=[("f", 8)])
                yield
                KK, TMP = ft[9], ft[0]
                P.op("act", lambda e: e.activation(out=KK[:, 0:512], in_=Ks[:, 0:512], func=AF.Copy, scale=pvc(PV_KK, hp)),
                     rd=[("f", 4), "pv"], wr=[("f", 9)])
                P.op("dve", lambda e: e.tensor_tensor(out=bt[18][:], in0=KK[:, 0:512], in1=KK[:, 0:512], op=ALU.mult), rd=[("f", 9)], wr=[("b", 18)])
                P.op("pe", lambda e: e.matmul(pp[2][:], lhsT=blkB, rhs=bt[18][:], start=True, stop=True), rd=["ccb", ("b", 18)], wr=[("pp", 2)])
                yield
                K2 = ft[10]
                P.op("act", lambda e: e.activation(out=K2[:, 0:512], in_=A[:, 0:512], func=AF.Identity, scale=pvc(PV_KA, hp),
                                                   bias=omka[:, l * 8 + hp:l * 8 + hp + 1]), rd=[("f", 8), "pv", "omka"], wr=[("f", 10)])
                P.op("act", lambda e: e.activation(out=TMP[:, 0:512], in_=pp[2][:], func=AF.Sqrt), rd=[("pp", 2)], wr=[("f", 0)])
                yield
                P.op("dve", lambda e: e.tensor_scalar(out=TMP[:, 0:512], in0=TMP[:, 0:512], scalar1=1e-12, scalar2=None, op0=ALU.max),
                     rd=[("f", 0)], wr=[("f", 0)])
                yield
                P.op("dve", lambda e: e.reciprocal(out=TMP[:, 0:512], in_=TMP[:, 0:512]), rd=[("f", 0)], wr=[("f", 0)])
                yield
                P.op("dve", lambda e: e.tensor_tensor(out=KK[:, 0:512], in0=KK[:, 0:512], in1=TMP[:, 0:512], op=ALU.mult),
                     rd=[("f", 9), ("f", 0)], wr=[("f", 9)])
                yield
                P.op("dve", lambda e: e.tensor_tensor(out=K2[:, 0:512], in0=K2[:, 0:512], in1=Ks[:, 0:512], op=ALU.mult),
                     rd=[("f", 10), ("f", 4)], wr=[("f", 10)])
                yield
                P.op("dve", lambda e: e.scalar_tensor_tensor(out=bt[18][:], in0=Rs[:, 0:512], scalar=pvc(PV_RK, hp), in1=K2[:, 0:512],
                                                             op0=ALU.mult, op1=ALU.mult), rd=[("f", 3), ("f", 10), "pv"], wr=[("b", 18)])
                P.op("pe", lambda e: e.matmul(pp[3][:], lhsT=blkB, rhs=bt[18][:], start=True, stop=True), rd=["ccb", ("b", 18)], wr=[("pp", 3)])
                yield
                CUM = ft[12]
                P.op("act", lambda e: e.mul(out=LW[:, 0:512], in_=LW[:, 0:512], mul=-0.6065306597126334), rd=[("f", 7)], wr=[("f", 7)])
                P.op("dve", lambda e: e.tensor_tensor_scan(out=CUM[:, 0:512], data0=scanm, data1=LW[:, 0:512], initial=0.0,
                                                           op0=ALU.mult, op1=ALU.add), rd=["ccf", ("f", 7)], wr=[("f", 12)])
                yield
                P.op("dve", lambda e: e.tensor_tensor(out=BON[:, 0:512], in0=pp[3][:], in1=Vs[:, 0:512], op=ALU.mult),
                     rd=[("pp", 3), ("f", 5)], wr=[("f", boni)])
                EP, EM = ft[13], ft[0]
                P.op("act", lambda e: e.activation(out=EP[:, 0:512], in_=CUM[:, 0:512], func=AF.Exp), rd=[("f", 12)], wr=[("f", 13)])
                yield
                P.op("dve", lambda e: e.tensor_tensor(out=EM[:, 0:512], in0=CUM[:, 0:512], in1=LW[:, 0:512], op=ALU.subtract),
                     rd=[("f", 12), ("f", 7)], wr=[("f", 0)])
                P.op("act", lambda e: e.activation(out=EM[:, 0:512], in_=EM[:, 0:512], func=AF.Exp), rd=[("f", 0)], wr=[("f", 0)])
                yield
                P.op("dve", lambda e: e.tensor_copy(out=ecp, in_=v3(EP[:, 0:512])[:, :, 63]), rd=[("f", 13)], wr=[eck])
                for h in range(2):
                    P.op("dve", lambda e, h=h: e.tensor_tensor(out=RA4[h][:, :, 0, :], in0=v3(Rs[:, h * 256:(h + 1) * 256]),
                                                               in1=v3(EP[:, h * 256:(h + 1) * 256]), op=ALU.mult),
                         rd=[("f", 3), ("f", 13)], wr=[RAK[h]])
                    yield
                for h in range(2):
                    P.op("dve", lambda e, h=h: e.scalar_tensor_tensor(out=RA4[h][:, :, 1, :], in0=v3(KK[:, h * 256:(h + 1) * 256]), scalar=-1.0,
                                                                      in1=v3(EM[:, h * 256:(h + 1) * 256]), op0=ALU.mult, op1=ALU.mult),
                         rd=[("f", 9), ("f", 0)], wr=[RAK[h]])
                    yield
                EN = ft[13]
                P.op("act", lambda e: e.activation(out=EN[:, 0:512], in_=CUM[:, 0:512], func=AF.Exp, scale=-1.0), rd=[("f", 12)], wr=[("f", 13)])
                BP = ft[7]
                P.op("dve", lambda e: e.tensor_tensor(out=BP[:, 0:512], in0=KK[:, 0:512], in1=A[:, 0:512], op=ALU.mult),
                     rd=[("f", 9), ("f", 8)], wr=[("f", 7)])
                yield
                for h in range(2):
                    sl = slice(h * 256, (h + 1) * 256)
                    P.op("dve", lambda e, h=h, sl=sl: e.tensor_tensor(out=KB4[h][:, :, 0, :], in0=v3(K2[:, sl]), in1=v3(EN[:, sl]), op=ALU.mult),
                         rd=[("f", 10), ("f", 13)], wr=[KBK[h]])
                    yield
                    P.op("dve", lambda e, h=h, sl=sl: e.tensor_tensor(out=KB4[h][:, :, 1, :], in0=v3(BP[:, sl]), in1=v3(EN[:, sl]), op=ALU.mult),
                         rd=[("f", 7), ("f", 13)], wr=[KBK[h]])
                    yield
                P.op("act", lambda e: e.activation(out=Vb[:], in_=Vs[:, 0:512], func=AF.Copy), rd=[("f", 5)], wr=[("b", vbi)])
                yield

            def rwkv_B(hp, par):
                RA4 = [r4(RAH[par][h][0]) for h in range(2)]
                KB4 = [r4(KBH[par][h][0]) for h in range(2)]
                RAK = [("b", RAH[par][h][1]) for h in range(2)]
                KBK = [("b", KBH[par][h][1]) for h in range(2)]
                Vb, vbi = VBS[par]
                BON, boni = BONS[par]
                sg, sgi = SGS[par]
                ecp, eck = ec[:, par, :], ("ec", par)
                VT, KIT, BIT = bt[6], bt[7], bt[8]
                pqb = [pq[i][:].bitcast(BF16) for i in range(4)]
                for (dst, di, bank, srcfn, skeys) in (
                        (VT, 6, 0, lambda n, hs: Vb[hs, n * 64:(n + 1) * 64], lambda n: [("b", vbi)]),
                        (KIT, 7, 1, lambda n, hs: KB4[n // 4][hs, n % 4, 0, :], lambda n: [KBK[n // 4]]),
                        (BIT, 8, 2, lambda n, hs: KB4[n // 4][hs, n % 4, 1, :], lambda n: [KBK[n // 4]])):
                    for n in range(8):
                        for h2 in range(2):
                            hs = slice(h2 * 64, (h2 + 1) * 64)
                            P.op("pe", lambda e, n=n, hs=hs, bank=bank, srcfn=srcfn: e.transpose(
                                out=pqb[bank][hs, n * 64:(n + 1) * 64], in_=srcfn(n, hs), identity=identB[hs, hs]),
                                rd=skeys(n) + ["ccb"], wr=[("pq", bank)])
                    if di != 7:
                        P.op("act", lambda e, dst=dst, bank=bank: e.activation(out=dst[:], in_=pqb[bank][:, 0:512], func=AF.Copy),
                             rd=[("pq", bank)], wr=[("b", di)])
                    else:
                        P.op("dve", lambda e, dst=dst, bank=bank: e.tensor_copy(out=dst[:], in_=pqb[bank][:, 0:512]),
                             rd=[("pq", bank)], wr=[("b", di)])
                    yield
                ATk, ATb, X0 = [bt[9], bt[10]], [bt[11], bt[12]], bt[13]
                mA = maskA.unsqueeze(1).broadcast_to([128, 4, 128])
                mL = maskL.unsqueeze(1).broadcast_to([128, 4, 64])
                for h in range(2):
                    for n4 in range(4):
                        for h2 in range(2):
                            hs = slice(h2 * 64, (h2 + 1) * 64)
                            P.op("pe", lambda e, h=h, n4=n4, hs=hs: e.matmul(pq[0][hs, n4 * 128:(n4 + 1) * 128], lhsT=KB4[h][hs, n4, 0, :],
                                                                             rhs=RA4[h][hs, n4, :, :], start=True, stop=True),
                                 rd=[KBK[h], RAK[h]], wr=[("pq", 0)])
                            P.op("pe", lambda e, h=h, n4=n4, hs=hs: e.matmul(pq[1][hs, n4 * 128:(n4 + 1) * 128], lhsT=KB4[h][hs, n4, 1, :],
                                                                             rhs=RA4[h][hs, n4, :, :], start=True, stop=True),
                                 rd=[KBK[h], RAK[h]], wr=[("pq", 1)])
                            P.op("pe", lambda e, h=h, n4=n4, hs=hs: e.matmul(pq[2][hs, n4 * 64:(n4 + 1) * 64], lhsT=RA4[h][hs, n4, 1, :],
                                                                             rhs=KB4[h][hs, n4, 1, :], start=True, stop=True),
                                 rd=[KBK[h], RAK[h]], wr=[("pq", 2)])
                    yield
                    P.op("dve", lambda e, h=h: e.tensor_tensor(out=ATk[h][:].rearrange("p (n c) -> p n c", c=128),
                                                               in0=pq[0][:].rearrange("p (n c) -> p n c", c=128), in1=mA, op=ALU.mult),
                         rd=[("pq", 0), "ccf"], wr=[("b", 9 + h)])
                    P.op("dve", lambda e, h=h: e.tensor_tensor(out=ATb[h][:].rearrange("p (n c) -> p n c", c=128),
                                                               in0=pq[1][:].rearrange("p (n c) -> p n c", c=128), in1=mA, op=ALU.mult),
                         rd=[("pq", 1), "ccf"], wr=[("b", 11 + h)])
                    P.op("dve", lambda e, h=h: e.tensor_tensor(out=X0[:, h * 256:(h + 1) * 256].rearrange("p (n c) -> p n c", c=64),
                                                               in0=pq[2][:, 0:256].rearrange("p (n c) -> p n c", c=64), in1=mL, op=ALU.mult),
                         rd=[("pq", 2), "ccf"], wr=[("b", 13)])
                    yield
                ATk3 = [a[:].rearrange("p (n c) -> p n c", c=128) for a in ATk]
                ATb3 = [a[:].rearrange("p (n c) -> p n c", c=128) for a in ATb]
                Xt0 = bt[14]
                for h in range(2):
                    P.op("act", lambda e, h=h: e.activation(out=Xt0[:, h * 256:(h + 1) * 256].rearrange("p (n c) -> p n c", c=64),
                                                            in_=ATb3[h][:, :, 64:128], func=AF.Copy), rd=[("b", 11 + h)], wr=[("b", 14)])
                Mt = bt[15]
                iI = identI.unsqueeze(1).broadcast_to([128, 8, 64])
                P.op("dve", lambda e: e.tensor_tensor(out=v3(Mt[:]), in0=v3(Xt0[:]), in1=iI, op=ALU.add), rd=[("b", 14), "ccf"], wr=[("b", 15)])
                yield
                Xc, Xtc, xi, xti = X0, Xt0, 13, 14
                pingX, pingXt = [(bt[13], 13), (bt[0], 0)], [(bt[14], 14), (Vb, vbi)]
                for lvl in range(1, 6):
                    Xn, xni = pingX[lvl % 2]
                    Xtn, xtni = pingXt[lvl % 2]
                    for n in range(8):
                        for h2 in range(2):
                            hs = slice(h2 * 64, (h2 + 1) * 64)
                            cs = slice(n * 64, (n + 1) * 64)
                            P.op("pe", lambda e, hs=hs, cs=cs, Xc=Xc, Xtc=Xtc: e.matmul(pq[0][hs, cs], lhsT=Xc[hs, cs], rhs=Xtc[hs, cs], start=True, stop=True),
                                 rd=[("b", xi), ("b", xti)], wr=[("pq", 0)])
                            P.op("pe", lambda e, hs=hs, cs=cs, Xc=Xc, Xtc=Xtc: e.matmul(pq[1][hs, cs], lhsT=Xtc[hs, cs], rhs=Xc[hs, cs], start=True, stop=True),
                                 rd=[("b", xi), ("b", xti)], wr=[("pq", 1)])
                    yield
                    P.op("act", lambda e, Xtn=Xtn: e.activation(out=Xtn[:], in_=pq[0][:], func=AF.Copy), rd=[("pq", 0)], wr=[("b", xtni)])
                    P.op("dve", lambda e, Xn=Xn: e.tensor_copy(out=Xn[:], in_=pq[1][:]), rd=[("pq", 1)], wr=[("b", xni)])
                    yield
                    for n in range(8):
                        for h2 in range(2):
                            hs = slice(h2 * 64, (h2 + 1) * 64)
                            cs = slice(n * 64, (n + 1) * 64)
                            P.op("pe", lambda e, hs=hs, cs=cs, Xn=Xn: e.matmul(pq[2][hs, cs], lhsT=Xn[hs, cs], rhs=Mt[hs, cs], start=True, stop=True),
                                 rd=[("b", xni), ("b", 15)], wr=[("pq", 2)])
                    yield
                    P.op("dve", lambda e: e.tensor_tensor(out=Mt[:], in0=pq[2][:], in1=Mt[:], op=ALU.add), rd=[("pq", 2), ("b", 15)], wr=[("b", 15)])
                    yield
                    Xc, Xtc, xi, xti = Xn, Xtn, xni, xtni
                hh = l * 8 + hp
                R0b, Ub = sm[:, 0:64], sm[:, 64:128]
                for n in range(8):
                    h, n4 = n // 4, n % 4
                    cs = slice(n * 64, (n + 1) * 64)
                    P.op("act", lambda e, n=n: e.activation(out=gs[:, 0:64], in_=Hf[:, hh, :], func=AF.Copy, scale=ecp[:, n:n + 1]),
                         rd=["Hf", eck], wr=["gs"])
                    for h2 in range(2):
                        hs = slice(h2 * 64, (h2 + 1) * 64)
                        P.op("pe", lambda e, hs=hs, h=h, n4=n4: e.matmul(pq[3][hs, 0:64], lhsT=RA4[h][hs, n4, 1, :], rhs=Hb[hs, hh, :], start=True, stop=False),
                             rd=[RAK[h], "Hb"], wr=[("pq", 3)])
                        P.op("pe", lambda e, hs=hs, h=h, n4=n4, cs=cs: e.matmul(pq[3][hs, 0:64], lhsT=ATk3[h][hs, n4, 64:128], rhs=VT[hs, cs], start=False, stop=True),
                             rd=[("b", 9 + h), ("b", 6)], wr=[("pq", 3)])
                    yield
                    P.op("act", lambda e: e.activation(out=R0b, in_=pq[3][:, 0:64], func=AF.Copy), rd=[("pq", 3)], wr=["sm0"])
                    for h2 in range(2):
                        hs = slice(h2 * 64, (h2 + 1) * 64)
                        P.op("pe", lambda e, hs=hs, cs=cs: e.matmul(pq[3][hs, 64:128], lhsT=Mt[hs, cs], rhs=sm[hs, 0:64], start=True, stop=True),
                             rd=[("b", 15), "sm0"], wr=[("pq", 3)])
                    yield
                    P.op("dve", lambda e: e.tensor_copy(out=Ub, in_=pq[3][:, 64:128]), rd=[("pq", 3)], wr=["sm1"])
                    for h2 in range(2):
                        hs = slice(h2 * 64, (h2 + 1) * 64)
                        P.op("pe", lambda e, hs=hs, cs=cs: e.matmul(pq[3][hs, 128:192], lhsT=KIT[hs, cs], rhs=VT[hs, cs], start=True, stop=False),
                             rd=[("b", 7), ("b", 6)], wr=[("pq", 3)])
                        P.op("pe", lambda e, hs=hs, cs=cs: e.matmul(pq[3][hs, 128:192], lhsT=BIT[hs, cs], rhs=sm[hs, 64:128], start=False, stop=True),
                             rd=[("b", 8), "sm1"], wr=[("pq", 3)])
                    for h2 in range(2):
                        hs = slice(h2 * 64, (h2 + 1) * 64)
                        P.op("pe", lambda e, hs=hs, h=h, n4=n4, cs=cs: e.matmul(pq[0][hs, cs], lhsT=Hb[hs, hh, :], rhs=RA4[h][hs, n4, 0, :], start=True, stop=False),
                             rd=["Hb", RAK[h]], wr=[("pq", 0)])
                        P.op("pe", lambda e, hs=hs, h=h, n4=n4, cs=cs: e.matmul(pq[0][hs, cs], lhsT=sm[hs, 64:128], rhs=ATb3[h][hs, n4, 0:64], start=False, stop=False),
                             rd=["sm1", ("b", 11 + h)], wr=[("pq", 0)])
                        P.op("pe", lambda e, hs=hs, h=h, n4=n4, cs=cs: e.matmul(pq[0][hs, cs], lhsT=VT[hs, cs], rhs=ATk3[h][hs, n4, 0:64], start=False, stop=True),
                             rd=[("b", 6), ("b", 9 + h)], wr=[("pq", 0)])
                    yield
                    P.op("dve", lambda e, n=n: e.scalar_tensor_tensor(out=Hb[:, hh, :], in0=pq[3][:, 128:192], scalar=ecp[:, n:n + 1], in1=gs[:, 0:64],
                                                                      op0=ALU.mult, op1=ALU.add), rd=[("pq", 3), eck, "gs"], wr=["Hb"])
                    P.op("dve", lambda e, n=n: e.scalar_tensor_tensor(out=Hf[:, hh, :], in0=pq[3][:, 128:192], scalar=ecp[:, n:n + 1], in1=gs[:, 0:64],
                                                                      op0=ALU.mult, op1=ALU.add), rd=[("pq", 3), eck, "gs"], wr=["Hf"])
                    yield
                Yf, MEAN, VAR = f32v(6), f32v(9), f32v(11)
                YFK, MK, VK = [("b", 6), ("b", 7)], [("b", 9), ("b", 10)], [("b", 11), ("b", 12)]
                Yb, Y2 = bt[13], bt[14]
                P.op("act", lambda e: e.activation(out=Yf, in_=pq[0][:], func=AF.Copy), rd=[("pq", 0)], wr=YFK)
                yield
                P.op("dve", lambda e: e.tensor_copy(out=Yb[:], in_=Yf), rd=YFK, wr=[("b", 13)])
                P.op("act", lambda e: e.activation(out=Y2[:], in_=Yf, func=AF.Square), rd=YFK, wr=[("b", 14)])
                P.op("pe", lambda e: e.matmul(pq[1][:], lhsT=blkB, rhs=Yb[:], start=True, stop=True), rd=["ccb", ("b", 13)], wr=[("pq", 1)])
                P.op("pe", lambda e: e.matmul(pq[2][:], lhsT=blkB, rhs=Y2[:], start=True, stop=True), rd=["ccb", ("b", 14)], wr=[("pq", 2)])
                yield
                P.op("act", lambda e: e.activation(out=MEAN, in_=pq[1][:], func=AF.Copy, scale=1.0 / 64), rd=[("pq", 1)], wr=MK)
                yield
                P.op("dve", lambda e: e.tensor_tensor(out=VAR, in0=MEAN, in1=MEAN, op=ALU.mult), rd=MK, wr=VK)
                yield
                P.op("dve", lambda e: e.scalar_tensor_tensor(out=VAR, in0=pq[2][:], scalar=1.0 / 64, in1=VAR,
                                                             op0=ALU.mult, op1=ALU.subtract), rd=[("pq", 2)] + VK, wr=VK)
                yield
                P.op("act", lambda e: e.activation(out=VAR, in_=VAR, func=AF.Sqrt, bias=64e-5), rd=VK, wr=VK)
                P.op("dve", lambda e: e.tensor_tensor(out=Yf, in0=Yf, in1=MEAN, op=ALU.subtract), rd=YFK + MK, wr=YFK)
                yield
                P.op("dve", lambda e: e.reciprocal(out=VAR, in_=VAR), rd=VK, wr=VK)
                yield
                P.op("dve", lambda e: e.tensor_tensor(out=Yf, in0=Yf, in1=VAR, op=ALU.mult), rd=YFK + VK, wr=YFK)
                yield
                P.op("act", lambda e: e.activation(out=Yf, in_=Yf, func=AF.Identity, scale=pvc(PV_GW, hp), bias=pvc(PV_GB, hp)),
                     rd=YFK + ["pv"], wr=YFK)
                yield
                P.op("dve", lambda e: e.tensor_tensor(out=Yf, in0=Yf, in1=BON[:, 0:512], op=ALU.add), rd=YFK + [("f", boni)], wr=YFK)
                yield
                P.op("dve", lambda e: e.tensor_tensor(out=ybr[:, hp, :], in0=Yf, in1=sg[:, 0:512], op=ALU.mult),
                     rd=YFK + [("f", sgi)], wr=[("y", hp)])
                yield

            def drive(gb, ga, ratio=0.6):
                acc, a_done, b_done = 0.0, ga is None, gb is None
                while not (a_done and b_done):
                    if not b_done:
                        try:
                            next(gb)
                        except StopIteration:
                            b_done = True
                    if not a_done:
                        acc += ratio if not b_done else 1e9
                        while acc >= 1.0 and not a_done:
                            acc -= 1.0
                            try:
                                next(ga)
                            except StopIteration:
                                a_done = True

            drive(None, rwkv_A(0, 0))
            for hp in range(8):
                drive(rwkv_B(hp, hp % 2), rwkv_A(hp + 1, (hp + 1) % 2) if hp < 7 else None)

            s = load_w(w_in[l], [(0, C_SK, 64), (64, C_SK, 64), (128, C_SK + 64, 64), (192, C_SK + 64, 64), (256, C_SV, 128)])
            for g in range(2):
                proj_h(s, g, g)
                P.op("act", lambda e, g=g: e.activation(out=kd[:, l * 2 + g, 128:640], in_=pp[g][:], func=AF.Copy), rd=[("pp", g)], wr=[("kd", g)])
            for blk in range(4):
                for kc in range(16):
                    P.op("pe", lambda e, kc=kc, blk=blk, s=s: e.matmul(pp[2][:, blk * 128:(blk + 1) * 128], lhsT=hT[:, kc, blk * 128:(blk + 1) * 128],
                                                                       rhs=wbuf[s][:, kc, 256:384], start=(kc == 0), stop=(kc == 15)),
                         rd=[("w", s), ("h", kc)], wr=[("pp", 2)])
            P.op("act", lambda e: e.activation(out=vt[:, l * 5 + 1:l * 5 + 5, :], in_=pp[2][:].rearrange("p (a b) -> p a b", b=128), func=AF.Copy),
                 rd=[("pp", 2)], wr=["vt"])
            for cp in range(4):
                c0 = 2 * cp
                s = load_w(w_in[l], [(0, C_SQ + c0 * 128, 128), (128, C_SG + c0 * 128, 128), (256, C_SQ + (c0 + 1) * 128, 128),
                                     (384, C_SG + (c0 + 1) * 128, 128)])
                for j in range(4):
                    proj_h(s, j, j)
                for ci in range(2):
                    c = c0 + ci
                    g = c // 4
                    qTb, sgs = bt[0], ft[3]
                    P.op("act", lambda e, ci=ci: e.activation(out=qTb[:], in_=pp[2 * ci][:], func=AF.Copy), rd=[("pp", 2 * ci)], wr=[("b", 0)])
                    P.op("act", lambda e, ci=ci: e.activation(out=sgs[:, 0:512], in_=pp[2 * ci + 1][:], func=AF.Silu), rd=[("pp", 2 * ci + 1)], wr=[("f", 3)])
                    for blk in range(4):
                        bs = slice(blk * 128, (blk + 1) * 128)
                        ex, exm = bt[1], bt[2]
                        for h2 in range(2):
                            hs = slice(h2 * 64, (h2 + 1) * 64)
                            for w_ in range(2):
                                P.op("pe", lambda e, hs=hs, h2=h2, w_=w_, blk=blk, bs=bs, g=g: e.matmul(
                                    pq[h2][:, w_ * 128:(w_ + 1) * 128], lhsT=kd[hs, l * 2 + g, (blk + w_) * 128:(blk + w_ + 1) * 128],
                                    rhs=qTb[hs, bs], start=True, stop=True), rd=[("kd", g), ("b", 0)], wr=[("pq", h2)])
                            P.op("act", lambda e, h2=h2: e.activation(out=ex[:, h2 * 256:(h2 + 1) * 256], in_=pq[h2][:, 0:256], func=AF.Exp, scale=0.125),
                                 rd=[("pq", h2)], wr=[("b", 1)])
                        mk = (maskS0 if (ti == 0 and blk == 0) else maskS).unsqueeze(1).broadcast_to([128, 2, 256])
                        P.op("dve", lambda e, mk=mk: e.tensor_tensor(out=exm[:].rearrange("p (a b) -> p a b", b=256),
                                                                   in0=ex[:].rearrange("p (a b) -> p a b", b=256), in1=mk, op=ALU.mult),
                             rd=[("b", 1), "ccb"], wr=[("b", 2)])
                        for h2 in range(2):
                            hs = slice(h2 * 64, (h2 + 1) * 64)
                            for w_ in range(2):
                                P.op("pe", lambda e, hs=hs, h2=h2, w_=w_, blk=blk, bs=bs, g=g: e.matmul(
                                    pq[2][hs, bs], lhsT=vt[:, l * 5 + blk + w_, g * 64:(g + 1) * 64],
                                    rhs=exm[:, h2 * 256 + w_ * 128:h2 * 256 + (w_ + 1) * 128], start=(w_ == 0), stop=(w_ == 1)),
                                    rd=["vt", ("b", 2)], wr=[("pq", 2)])
                            for w_ in range(2):
                                P.op("pe", lambda e, hs=hs, h2=h2, w_=w_, bs=bs: e.matmul(
                                    pq[3][hs, bs], lhsT=onesB[:, 0:64], rhs=exm[:, h2 * 256 + w_ * 128:h2 * 256 + (w_ + 1) * 128],
                                    start=(w_ == 0), stop=(w_ == 1)), rd=["ccb", ("b", 2)], wr=[("pq", 3)])
                    DEN = ft[4]
                    P.op("dve", lambda e, c=c: e.tensor_scalar(out=DEN[:, 0:512], in0=pq[3][:], scalar1=esink[:, l * 8 + c:l * 8 + c + 1], scalar2=None,
                                                               op0=ALU.add), rd=[("pq", 3), "esink"], wr=[("f", 4)])
                    P.op("dve", lambda e: e.reciprocal(out=DEN[:, 0:512], in_=DEN[:, 0:512]), rd=[("f", 4)], wr=[("f", 4)])
                    P.op("dve", lambda e: e.tensor_tensor(out=DEN[:, 0:512], in0=pq[2][:], in1=DEN[:, 0:512], op=ALU.mult),
                         rd=[("pq", 2), ("f", 4)], wr=[("f", 4)])
                    P.op("dve", lambda e, c=c: e.tensor_tensor(out=ybr[:, 8 + c, :], in0=DEN[:, 0:512], in1=sgs[:, 0:512], op=ALU.mult),
                         rd=[("f", 4), ("f", 3)], wr=[("y", 8 + c)])
            for g in range(2):
                P.op("dve", lambda e, g=g: e.tensor_copy(out=kd[:, l * 2 + g, 0:128], in_=kd[:, l * 2 + g, 512:640]), rd=[("kd", g)], wr=[("kd", g)])
            P.op("dve", lambda e: e.tensor_copy(out=vt[:, l * 5, :], in_=vt[:, l * 5 + 4, :]), rd=["vt"], wr=["vt"])

            for h in range(4):
                s = load_w(w_in[l], [(0, C_XQ + h * 256, 256), (256, C_XG + h * 256, 256)])
                for j in range(4):
                    proj_h(s, j, j)
                qx, sgx = [bt[0], bt[1]], [ft[3], ft[4]]
                for j in range(2):
                    P.op("act", lambda e, j=j: e.activation(out=qx[j][:], in_=pp[j][:], func=AF.Copy), rd=[("pp", j)], wr=[("b", j)])
                    P.op("act", lambda e, j=j: e.activation(out=sgx[j][:, 0:512], in_=pp[2 + j][:], func=AF.Silu), rd=[("pp", 2 + j)], wr=[("f", 3 + j)])
                exx = [bt[2], bt[3]]
                for mt in range(2):
                    for j in range(2):
                        P.op("pe", lambda e, mt=mt, j=j, h=h: e.matmul(pq[mt][:], lhsT=kmT[:, l * 8 + h * 2 + j, mt * 128:(mt + 1) * 128], rhs=qx[j][:],
                                                                       start=(j == 0), stop=(j == 1)), rd=["kmT", ("b", j)], wr=[("pq", mt)])
                    P.op("act", lambda e, mt=mt: e.activation(out=exx[mt][:], in_=pq[mt][:], func=AF.Exp, scale=1.0 / 16), rd=[("pq", mt)], wr=[("b", 2 + mt)])
                for j in range(2):
                    for mt in range(2):
                        P.op("pe", lambda e, mt=mt, j=j, h=h: e.matmul(pp[j][:], lhsT=vm[:, l * 2 + mt, h * 256 + j * 128:h * 256 + (j + 1) * 128],
                                                                       rhs=exx[mt][:], start=(mt == 0), stop=(mt == 1)),
                             rd=["vm", ("b", 2 + mt)], wr=[("pp", j)])
                for mt in range(2):
                    P.op("pe", lambda e, mt=mt: e.matmul(pq[2][:], lhsT=onesB, rhs=exx[mt][:], start=(mt == 0), stop=(mt == 1)),
                         rd=["ccb", ("b", 2 + mt)], wr=[("pq", 2)])
                REC = ft[5]
                P.op("dve", lambda e: e.reciprocal(out=REC[:, 0:512], in_=pq[2][:]), rd=[("pq", 2)], wr=[("f", 5)])
                for j in range(2):
                    P.op("dve", lambda e, j=j: e.tensor_tensor(out=sgx[j][:, 0:512], in0=sgx[j][:, 0:512], in1=REC[:, 0:512], op=ALU.mult),
                         rd=[("f", 3 + j), ("f", 5)], wr=[("f", 3 + j)])
                    P.op("dve", lambda e, j=j, h=h: e.tensor_tensor(out=ybr[:, 16 + h * 2 + j, :], in0=pp[j][:], in1=sgx[j][:, 0:512], op=ALU.mult),
                         rd=[("pp", j), ("f", 3 + j)], wr=[("y", 16 + h * 2 + j)])

            for dg in range(4):
                for br in range(3):
                    s = load_w(w_in[l], [(0, C_MG + br * 2048 + dg * 512, 512)])
                    for j in range(4):
                        proj_h(s, j, j)
                        P.op("act", lambda e, j=j: e.activation(out=ft[3 + j][:, 0:512], in_=pp[j][:], func=AF.Sigmoid), rd=[("pp", j)], wr=[("f", 3 + j)])
                    s = load_w(w_up[br][l], [(0, dg * 512, 512)], nk=8)
                    for j in range(4):
                        proj(s, j, pq[j][:], ("pq", j), lambda kc, br=br: ybr[:, br * 8 + kc, :], lambda kc, br=br: [("y", br * 8 + kc)], nk=8)
                        acc = ft[7 + j]
                        if br == 0:
                            P.op("dve", lambda e, j=j, acc=acc: e.tensor_tensor(out=acc[:, 0:512], in0=pq[j][:], in1=ft[3 + j][:, 0:512], op=ALU.mult),
                                 rd=[("pq", j), ("f", 3 + j)], wr=[("f", 7 + j)])
                        else:
                            P.op("dve", lambda e, j=j: e.tensor_tensor(out=ft[3 + j][:, 0:512], in0=pq[j][:], in1=ft[3 + j][:, 0:512], op=ALU.mult),
                                 rd=[("pq", j), ("f", 3 + j)], wr=[("f", 3 + j)])
                            if br == 1:
                                P.op("dve", lambda e, j=j, acc=acc: e.tensor_tensor(out=acc[:, 0:512], in0=acc[:, 0:512], in1=ft[3 + j][:, 0:512], op=ALU.add),
                                     rd=[("f", 7 + j), ("f", 3 + j)], wr=[("f", 7 + j)])
                            else:
                                dc = dg * 4 + j
                                P.op("dve", lambda e, j=j, acc=acc, dc=dc: e.tensor_tensor(out=bt[dc][:], in0=acc[:, 0:512], in1=ft[3 + j][:, 0:512], op=ALU.add),
                                     rd=[("f", 7 + j), ("f", 3 + j)], wr=[("b", dc)])
            for dg in range(4):
                s = load_w(w_out[l], [(0, dg * 512, 512)])
                for j in range(4):
                    dc = dg * 4 + j
                    proj(s, j, pp[j][:], ("pp", j), lambda kc: bt[kc][:], lambda kc: [("b", kc)])
                    P.op("act", lambda e, j=j, dc=dc: e.activation(out=o_f[:, dc, :], in_=pp[j][:], func=AF.Copy), rd=[("pp", j)], wr=okeys(dc))
                    t = ft[dc % 2]
                    P.op("act", lambda e, dc=dc, t=t: e.activation(out=t[:, 0:512], in_=o_f[:, dc, :], func=AF.Square), rd=okeys(dc), wr=[("f", dc % 2)])
                    P.op("pe", lambda e, dc=dc, t=t: e.matmul(pq[0][:], lhsT=onesF, rhs=t[:, 0:512], start=(dc == 0), stop=(dc == 15)),
                         rd=[("f", dc % 2), "ccf"], wr=[("pq", 0)])
            rs2 = ft[2]
            P.op("act", lambda e: e.activation(out=rs2[:, 0:512], in_=pq[0][:], func=AF.Sqrt, scale=1.0 / D, bias=1e-6), rd=[("pq", 0)], wr=[("f", 2)])
            P.op("dve", lambda e: e.reciprocal(out=rs2[:, 0:512], in_=rs2[:, 0:512]), rd=[("f", 2)], wr=[("f", 2)])
            for dc in range(16):
                t = ft[3 + (dc % 2)]
                P.op("dve", lambda e, dc=dc, t=t: e.scalar_tensor_tensor(out=t[:, 0:512], in0=o_f[:, dc, :], scalar=pvc(PV_GPOST, dc), in1=rs2[:, 0:512],
                                                                         op0=ALU.mult, op1=ALU.mult), rd=okeys(dc) + ["pv", ("f", 2)], wr=[("f", 3 + dc % 2)])
                P.op("dve", lambda e, dc=dc, t=t: e.tensor_tensor(out=x_res[:, dc, :], in0=x_res[:, dc, :], in1=t[:, 0:512], op=ALU.add),
                     rd=[("x", dc), ("f", 3 + dc % 2)], wr=[("x", dc)])
            if l == L - 1:
                for q in range(4):
                    P.dma("sp", lambda e, q=q: e.dma_start(out=outT[q * 512:(q + 1) * 512, t0:t0 + TT].rearrange("(dc p) t -> p dc t", p=128),
                                                           in_=x_res[:, q * 4:(q + 1) * 4, :]),
                          f"o{q}", rd=[("x", q * 4 + i) for i in range(4)], wr=[("out", q)])

        for ti in range(NT):
            for l in range(L):
                block(ti, l)
        P.wait_all("sp", [("out", q) for q in range(4)])
        P.emit()
        print("instructions:", P.n_inst, "sems:", P.sem_id)
    return nc


def _consts():
    cf = np.zeros((128, NCC), np.float32)
    cb = np.zeros((128, NCC), np.float32)
    p = np.arange(128)[:, None]
    c = np.arange(128)[None, :]
    cf[:, CF_ONES:CF_ONES + 128] = 1.0
    cb[:, CB_ONES:CB_ONES + 128] = 1.0
    cb[:, CB_BLK:CB_BLK + 128] = (p // 64 == c // 64)
    cb[:, CB_ID:CB_ID + 128] = (p == c)
    j = p % 64
    t = np.arange(64)[None, :]
    cf[:, CF_MA:CF_MA + 64] = (j <= t)
    cf[:, CF_MA + 64:CF_MA + 128] = (j < t)
    cf[:, CF_ML:CF_ML + 64] = (t < j)
    cf[:, CF_II:CF_II + 64] = (j == t)
    q = np.arange(128)[None, :]
    cb[:, CB_MS:CB_MS + 128] = (p > q)
    cb[:, CB_MS + 128:CB_MS + 256] = (q >= p)
    cb[:, CB_MS0 + 128:CB_MS0 + 256] = (q >= p)
    sc = np.ones((128, 512), np.float32)
    sc[:, 0::64] = 0.0
    cf[:, CF_SCAN:CF_SCAN + 512] = sc
    return cf, cb


def _layout(inputs, L):
    col = lambda v, n: np.ascontiguousarray(v.reshape(n, 128).T)
    pvs = []
    for l in range(L):
        sk = np.repeat(inputs["attn_sinks"][l], 64)
        pvs += [col(inputs["g_pre"][l], 16), col(inputs["g_post"][l], 16), col(inputs["g_mem"][l], 16),
                col(inputs["mu_shift"][l], 25), col(inputs["decay_base"][l], 8), col(inputs["iclr_base"][l], 8),
                col(inputs["k_k"][l], 8), col(inputs["k_a"][l], 8), col(inputs["r_k"][l].reshape(-1), 8),
                col(inputs["gn_w"][l], 8), col(inputs["gn_b"][l], 8), col(sk, 8)]
    pvd = np.ascontiguousarray(np.concatenate(pvs, axis=1), dtype=np.float32)
    dwd = np.ascontiguousarray(np.concatenate([inputs["decay_up"][:L], inputs["iclr_up"][:L]], axis=1), dtype=np.float32)
    return pvd, dwd


def run(inputs, T, L, B, trace=False):
    inputs = {k: np.asarray(v, dtype=np.float32) for k, v in inputs.items()}
    nc = build(T, L)
    pvd, dwd = _layout(inputs, L)
    cf, cb = _consts()
    shared = {
        "w_in": np.ascontiguousarray(inputs["w_in"][:L]), "w_mkv": np.ascontiguousarray(inputs["w_mem_kv"][:L]),
        "w_up0": np.ascontiguousarray(inputs["w_up_rwkv"][:L]), "w_up1": np.ascontiguousarray(inputs["w_up_swa"][:L]),
        "w_up2": np.ascontiguousarray(inputs["w_up_xattn"][:L]), "w_out": np.ascontiguousarray(inputs["w_out"][:L]),
        "dwd": dwd, "pvd": pvd, "ccdf": cf, "ccdb": cb,
    }
    in_maps = []
    for b in range(B):
        m = dict(shared)
        m["xT"] = np.ascontiguousarray(inputs["x"][b].T)
        m["memT"] = np.ascontiguousarray(inputs["mem"][b].T)
        in_maps.append(m)
    res = run_bass_kernel_spmd(nc, in_maps, core_ids=list(range(B)), trace=trace)
    out = np.stack([np.ascontiguousarray(r["outT"].T) for r in res.results], axis=0)
    return out.astype(np.float32), res


def kernel(**inputs):
    out, _ = run(inputs, 2048, 2, 8)
    return out
```

```python
import contextlib
import numpy as np
import concourse.bass as bass
import concourse.mybir as mybir
from concourse.bass_utils import run_bass_kernel_spmd

F32 = mybir.dt.float32
BF16 = mybir.dt.bfloat16
AF = mybir.ActivationFunctionType
ALU = mybir.AluOpType

D = 2048
DIN = 14720
MEM = 256
TT = 512
EPOCH = 30000

C_R, C_K, C_V, C_WD = 0, 1024, 2048, 3072
C_RG = 3200
C_SQ = 4224
C_SK = 5248
C_SV = 5376
C_SG = 5504
C_XQ = 6528
C_XG = 7552
C_MG = 8576

PV_GPRE, PV_GPOST, PV_GMEM, PV_MU = 0, 16, 32, 48
PV_DB, PV_IB, PV_KK, PV_KA, PV_RK, PV_GW, PV_GB, PV_SINK = 73, 81, 89, 97, 105, 113, 121, 129
NPV = 137
CF_ONES, CF_MA, CF_ML, CF_II, CF_SCAN = 0, 128, 256, 320, 384
CB_ONES, CB_BLK, CB_ID, CB_MS, CB_MS0 = 0, 128, 256, 384, 640
NCC = 896


class Prog:
    COMPUTE = ("pe", "act", "dve", "pool")

    def __init__(self, nc, stack):
        self.nc, self.stack = nc, stack
        self.eng_names = ("pe", "act", "dve", "pool", "sp")
        self.ops = {e: [] for e in self.eng_names}
        self.cnt = {e: 0 for e in self.COMPUTE}
        self.sem_objs, self.sem_id, self.cur_sem = {}, 0, {}
        for e in self.COMPUTE:
            self.cur_sem[e] = self._new_sem()
        self.waited = {e: {} for e in self.eng_names}
        self.buf, self.dma_sems = {}, {}
        self.n_inst = 0
        self.E = {"pe": nc.tensor, "act": nc.scalar, "dve": nc.vector, "pool": nc.gpsimd, "sp": nc.sync}

    def _new_sem(self):
        s = self.stack.enter_context(self.nc.semaphore(f"s{self.sem_id}"))
        self.sem_objs[self.sem_id] = s
        self.sem_id += 1
        return self.sem_id - 1

    def _deps(self, eng, reads, writes):
        need = {}

        def add(tok):
            sidx, val, teng = tok
            if teng == "pe" and eng == "pe":
                return
            if need.get(sidx, 0) < val:
                need[sidx] = val
        for k in reads:
            st = self.buf.get(k)
            if st and st[0] is not None:
                add(st[0])
        for k in writes:
            st = self.buf.get(k)
            if st:
                if st[0] is not None:
                    add(st[0])
                for t in st[1]:
                    add(t)
        for sidx, val in need.items():
            if self.waited[eng].get(sidx, 0) >= val:
                continue
            self.waited[eng][sidx] = val
            self.E[eng].wait_ge(self.sem_objs[sidx], val)

    def _record(self, tok, reads, writes):
        for k in reads:
            self.buf.setdefault(k, [None, []])[1].append(tok)
        for k in writes:
            self.buf[k] = [tok, []]

    def op(self, eng, fn, rd=(), wr=()):
        self._deps(eng, rd, wr)
        if self.cnt[eng] >= EPOCH:
            self.cur_sem[eng] = self._new_sem()
            self.cnt[eng] = 0
        self.cnt[eng] += 1
        tok = (self.cur_sem[eng], self.cnt[eng], eng)
        fn(self.E[eng]).then_inc(self.sem_objs[self.cur_sem[eng]], 1)
        self._record(tok, rd, wr)
        self.n_inst += 1

    def dma(self, eng, fn, semkey, rd=(), wr=()):
        self._deps(eng, rd, wr)
        if semkey not in self.dma_sems:
            self.dma_sems[semkey] = [self._new_sem(), 0]
        ds = self.dma_sems[semkey]
        ds[1] += 16
        tok = (ds[0], ds[1], "dma")
        fn(self.E[eng]).then_inc(self.sem_objs[ds[0]], 16)
        self._record(tok, rd, wr)
        self.n_inst += 1

    def wait_all(self, eng, keys):
        self._deps(eng, keys, ())

    def emit(self):
        return

    def emit_old(self):
        engmap = {"pe": "tensor", "act": "scalar", "dve": "vector", "pool": "gpsimd", "sp": "sync"}
        with self.nc.Block() as block:
            for e in self.eng_names:
                ops = self.ops[e]
                if not ops:
                    continue

                def body(eng, ops=ops):
                    for o in ops:
                        if o[0] == "wait":
                            eng.wait_ge(self.sem_objs[o[1]], o[2])
                        else:
                            o[1](eng).then_inc(self.sem_objs[o[2]], o[3])
                getattr(block, engmap[e])(body)


def build(T, L):
    NT = T // TT
    nc = bass.Bass("TRN2", target_bir_lowering=False)
    dr = lambda name, shape, kind="ExternalInput": nc.dram_tensor(name, shape, F32, kind=kind).ap()
    xT = dr("xT", [D, T])
    memT = dr("memT", [D, MEM])
    w_in = dr("w_in", [L, D, DIN])
    w_mkv = dr("w_mkv", [L, D, 2048])
    w_up = [dr(f"w_up{i}", [L, 1024, D]) for i in range(3)]
    w_out = dr("w_out", [L, D, D])
    dwd = dr("dwd", [L, 128, 1024])
    pvd = dr("pvd", [128, L * NPV])
    ccdf = dr("ccdf", [128, NCC])
    ccdb = dr("ccdb", [128, NCC])
    outT = dr("outT", [D, T], kind="ExternalOutput")

    with contextlib.ExitStack() as st:
        P = Prog(nc, st)
        sb = lambda name, shape, dt: st.enter_context(nc.sbuf_tensor(name, shape, dt))
        x_res = sb("x_res", [128, 16, TT], F32)
        HY = sb("HY", [128, 10240], F32)
        hT = HY[:, 0:4096].bitcast(BF16).rearrange("p (a b) -> p a b", b=TT)
        ybr = HY[:, 4096:10240].bitcast(BF16).rearrange("p (a b) -> p a b", b=TT)
        o_f = HY[:, 0:8192].rearrange("p (a b) -> p a b", b=TT)

        def okeys(dc):
            return [("h", 2 * dc), ("h", 2 * dc + 1)] if dc < 8 else [("y", 2 * (dc - 8)), ("y", 2 * (dc - 8) + 1)]
        wbuf = [sb(f"wbuf{i}", [128, 16, 512], BF16) for i in range(2)]
        NF, NB = 15, 29
        ft = [sb(f"ft{i}", [128, 514], F32) for i in range(NF)]
        bt_all = sb("bt_all", [128, NB, 512], BF16)
        bt = [bt_all[:, i, :] for i in range(NB)]
        f32v = lambda i: bt_all[:, i:i + 2, :].rearrange("p a b -> p (a b)").bitcast(F32)
        kmT = sb("kmT", [128, L * 8, MEM], BF16)
        vm = sb("vm", [128, L * 2, 1024], BF16)
        kd = sb("kd", [128, L * 2, 640], BF16)
        vt = sb("vt", [128, L * 5, 128], BF16)
        dw = sb("dw", [128, L, 1024], BF16)
        pv = sb("pv", [128, L * NPV], F32)
        omka = sb("omka", [128, L * 8], F32)
        nb = sb("nb", [128, L * 16], F32)
        esink = sb("esink", [128, L * 8], F32)
        ccf = sb("ccf", [128, NCC], F32)
        ccb = sb("ccb", [128, NCC], BF16)
        gs = sb("gs", [128, 64], F32)
        shp = sb("shp", [128, L * 25], F32)
        Hf = sb("Hf", [128, L * 8, 64], F32)
        Hb = sb("Hb", [128, L * 8, 64], BF16)
        ec = sb("ec", [128, 2, 8], F32)
        sm = sb("sm", [128, 256], BF16)
        pp = [st.enter_context(nc.psum_tensor(f"pp{i}", [128, 512], F32)) for i in range(4)]
        pq = [st.enter_context(nc.psum_tensor(f"pq{i}", [128, 512], F32)) for i in range(4)]
        PPK = [("pp", i) for i in range(4)]
        PQK = [("pq", i) for i in range(4)]

        onesF = ccf[:, CF_ONES:CF_ONES + 128]
        onesB = ccb[:, CB_ONES:CB_ONES + 128]
        blkB = ccb[:, CB_BLK:CB_BLK + 128]
        identB = ccb[:, CB_ID:CB_ID + 128]
        maskA = ccf[:, CF_MA:CF_MA + 128]
        maskL = ccf[:, CF_ML:CF_ML + 64]
        identI = ccf[:, CF_II:CF_II + 64]
        maskS = ccb[:, CB_MS:CB_MS + 256]
        maskS0 = ccb[:, CB_MS0:CB_MS0 + 256]
        scanm = ccf[:, CF_SCAN:CF_SCAN + 512]

        P.dma("sp", lambda e: e.dma_start(out=pv[:], in_=pvd), "pv", wr=["pv"])
        P.dma("sp", lambda e: e.dma_start(out=ccf[:], in_=ccdf), "ccf", wr=["ccf"])
        P.dma("pool", lambda e: e.dma_start(out=ccb[:], in_=ccdb), "ccb", wr=["ccb"])
        for l in range(L):
            P.dma("pool", lambda e, l=l: e.dma_start(out=dw[:, l, :], in_=dwd[l]), "dw", wr=["dw"])
        P.op("dve", lambda e: e.memset(shp[:], 0.0), wr=["shp"])
        P.op("dve", lambda e: e.memset(Hf[:], 0.0), wr=["Hf"])
        P.op("dve", lambda e: e.memset(Hb[:], 0.0), wr=["Hb"])
        P.op("dve", lambda e: e.memset(kd[:], 0.0), wr=["kd"])
        P.op("dve", lambda e: e.memset(vt[:], 0.0), wr=["vt"])
        for l in range(L):
            b = l * NPV
            P.op("dve", lambda e, l=l, b=b: e.tensor_scalar(out=omka[:, l * 8:(l + 1) * 8], in0=pv[:, b + PV_KA:b + PV_KA + 8],
                                                             scalar1=-1.0, scalar2=1.0, op0=ALU.mult, op1=ALU.add), rd=["pv"], wr=["omka"])
            P.op("act", lambda e, l=l, b=b: e.activation(out=esink[:, l * 8:(l + 1) * 8], in_=pv[:, b + PV_SINK:b + PV_SINK + 8], func=AF.Exp),
                 rd=["pv"], wr=["esink"])
            P.op("dve", lambda e, l=l, b=b: e.tensor_scalar(out=nb[:, l * 16:(l + 1) * 16], in0=pv[:, b + PV_DB:b + PV_DB + 16],
                                                             scalar1=-1.0, scalar2=None, op0=ALU.mult), rd=["pv"], wr=["nb"])

        wstate = {"slot": 0, "gid": None, "ti": 0}
        NG = 46
        wscr = nc.dram_tensor("wscr", [L * NG, 128, 8192], BF16, kind="Internal").ap()

        def load_w(src3, pieces, nk=16):
            s = wstate["slot"]
            wstate["slot"] = 1 - s
            gid = wstate["gid"]
            if gid is not None:
                wstate["gid"] = gid + 1
            if gid is not None and wstate["ti"] > 0:
                P.dma("sp", lambda e: e.dma_start(out=wbuf[s][:, 0:nk, :].rearrange("p a b -> p (a b)"), in_=wscr[gid][:, 0:nk * 512]),
                      f"w{s}", rd=[("scr", gid)], wr=[("w", s)])
                return s
            for (doff, c0, n) in pieces:
                P.dma("pool", lambda e, s=s, doff=doff, c0=c0, n=n: e.dma_start(
                    out=wbuf[s][:, 0:nk, doff:doff + n],
                    in_=src3[:, c0:c0 + n].rearrange("(kc p) c -> p kc c", p=128)), f"w{s}", wr=[("w", s)])
            if gid is not None and NT > 1:
                P.dma("sp", lambda e: e.dma_start(out=wscr[gid][:, 0:nk * 512], in_=wbuf[s][:, 0:nk, :].rearrange("p a b -> p (a b)")),
                      f"ws{s}", rd=[("w", s)], wr=[("scr", gid)])
            return s

        def proj(s, j, out_ps, out_key, rhs_fn, rhs_keys, nk=16, ncol=128):
            for kc in range(nk):
                P.op("pe", lambda e, kc=kc: e.matmul(out_ps, lhsT=wbuf[s][:, kc, j * 128:j * 128 + ncol], rhs=rhs_fn(kc),
                                                      start=(kc == 0), stop=(kc == nk - 1)),
                     rd=[("w", s)] + rhs_keys(kc), wr=[out_key])

        HK = [("h", i) for i in range(16)]

        def proj_h(s, j, bank):
            proj(s, j, pp[bank][:], ("pp", bank), lambda kc: hT[:, kc, :], lambda kc: [("h", kc)])

        def act_sigmoid(out, in_, rd, wrk, nbias=None, final_bias=None):
            kw = {"bias": nbias} if nbias is not None else {}
            P.op("act", lambda e: e.activation(out=out, in_=in_, func=AF.Exp, scale=-1.0, **kw), rd=rd, wr=wrk)
            P.op("act", lambda e: e.activation(out=out, in_=out, func=AF.Ln, bias=1.0), rd=wrk, wr=wrk)
            kw2 = {"bias": final_bias} if final_bias is not None else {}
            P.op("act", lambda e: e.activation(out=out, in_=out, func=AF.Exp, scale=-1.0, **kw2), rd=wrk, wr=wrk)

        def act_silu(out, in_ps, rd, wrk):
            act_sigmoid(out, in_ps, rd, wrk)
            P.op("dve", lambda e: e.tensor_tensor(out=out, in0=in_ps, in1=out, op=ALU.mult), rd=rd + wrk, wr=wrk)

        def act_rpow(out, in_, rd, wrk, p, scale=1.0, bias=None):
            kw = {"bias": bias} if bias is not None else {}
            P.op("act", lambda e: e.activation(out=out, in_=in_, func=AF.Ln, scale=scale, **kw), rd=rd, wr=wrk)
            P.op("act", lambda e: e.activation(out=out, in_=out, func=AF.Exp, scale=-p), rd=wrk, wr=wrk)

        def rms_stats(src_fn, src_keys, ncols, nchunks, out_rstd, out_key, tmpi):
            for dc in range(nchunks):
                bi = 27 + (dc % 2)
                t = bt[bi]
                P.op("act", lambda e, dc=dc, t=t: e.activation(out=t[:, 0:ncols], in_=src_fn(dc), func=AF.Square),
                     rd=src_keys(dc), wr=[("b", bi)])
                P.op("pe", lambda e, dc=dc, t=t: e.matmul(pq[0][:, 0:ncols], lhsT=onesB, rhs=t[:, 0:ncols],
                                                           start=(dc == 0), stop=(dc == nchunks - 1)),
                     rd=[("b", bi), "ccb"], wr=[("pq", 0)])
            act_rpow(out_rstd, pq[0][:, 0:ncols], [("pq", 0)], [out_key], 0.5, scale=1.0 / D, bias=1e-6)

        mT = x_res
        P.dma("sp", lambda e: e.dma_start(out=mT[:, :, 0:MEM], in_=memT.rearrange("(dc p) m -> p dc m", p=128)), "x0",
              wr=[("x", i) for i in range(16)])
        rstd_m = ft[2]
        rms_stats(lambda dc: mT[:, dc, 0:MEM], lambda dc: [("x", dc)], MEM, 16, rstd_m[:, 0:MEM], ("f", 2), 0)
        for l in range(L):
            b = l * NPV
            for dc in range(16):
                P.op("dve", lambda e, dc=dc, b=b: e.scalar_tensor_tensor(out=hT[:, dc, 0:MEM], in0=mT[:, dc, 0:MEM],
                                                                          scalar=pv[:, b + PV_GMEM + dc:b + PV_GMEM + dc + 1],
                                                                          in1=rstd_m[:, 0:MEM], op0=ALU.mult, op1=ALU.mult),
                     rd=[("x", dc), "pv", ("f", 2)], wr=[("h", dc)])
            for g in range(4):
                s = load_w(w_mkv[l], [(0, g * 512, 512)])
                if g < 2:
                    for j in range(4):
                        proj(s, j, pp[j][:, 0:MEM], ("pp", j), lambda kc: hT[:, kc, 0:MEM], lambda kc: [("h", kc)])
                        ci = l * 8 + g * 4 + j
                        P.op("act", lambda e, j=j, ci=ci: e.activation(out=kmT[:, ci, :], in_=pp[j][:, 0:MEM], func=AF.Copy),
                             rd=[("pp", j)], wr=["kmT"])
                else:
                    for mt in range(2):
                        for kc in range(16):
                            P.op("pe", lambda e, kc=kc, mt=mt, s=s: e.matmul(pp[mt][:], lhsT=hT[:, kc, mt * 128:(mt + 1) * 128],
                                                                                rhs=wbuf[s][:, kc, :], start=(kc == 0), stop=(kc == 15)),
                                 rd=[("w", s), ("h", kc)], wr=[("pp", mt)])
                        P.op("act", lambda e, mt=mt, l=l, g=g: e.activation(out=vm[:, l * 2 + mt, (g - 2) * 512:(g - 1) * 512], in_=pp[mt][:],
                                                                             func=AF.Copy), rd=[("pp", mt)], wr=["vm"])

        def block(ti, l):
            b = l * NPV
            wstate["gid"] = l * NG
            wstate["ti"] = ti
            pvc = lambda off, c: pv[:, b + off + c:b + off + c + 1]
            t0 = ti * TT
            if l == 0:
                for q in range(4):
                    P.dma("sp", lambda e, q=q: e.dma_start(out=x_res[:, q * 4:(q + 1) * 4, :],
                                                           in_=xT[q * 512:(q + 1) * 512, t0:t0 + TT].rearrange("(dc p) t -> p dc t", p=128)),
                          f"x{q}", wr=[("x", q * 4 + i) for i in range(4)])
            rstd = ft[2]
            rms_stats(lambda dc: x_res[:, dc, :], lambda dc: [("x", dc)], TT, 16, rstd[:, 0:TT], ("f", 2), 0)
            for dc in range(16):
                P.op("dve", lambda e, dc=dc: e.scalar_tensor_tensor(out=hT[:, dc, :], in0=x_res[:, dc, :], scalar=pvc(PV_GPRE, dc),
                                                                     in1=rstd[:, 0:TT], op0=ALU.mult, op1=ALU.mult),
                     rd=[("x", dc), "pv", ("f", 2)], wr=[("h", dc)])

            def shift(bank, fi_raw, fi_out, chunk_idx, rows=slice(0, 128)):
                raw = ft[fi_raw]
                si = l * 25 + chunk_idx
                P.op("act", lambda e: e.activation(out=raw[:, 1:513], in_=pp[bank][:], func=AF.Copy), rd=[("pp", bank)], wr=[("f", fi_raw)])
                P.op("dve", lambda e: e.tensor_copy(out=raw[:, 0:1], in_=shp[:, si:si + 1]), rd=["shp"], wr=[("f", fi_raw)])
                P.op("dve", lambda e: e.tensor_copy(out=shp[:, si:si + 1], in_=raw[:, 512:513]), rd=[("f", fi_raw)], wr=["shp"])
                P.op("dve", lambda e: e.tensor_tensor(out=ft[fi_out][:, 0:512], in0=raw[:, 0:512], in1=raw[:, 1:513], op=ALU.subtract),
                     rd=[("f", fi_raw)], wr=[("f", fi_out)])
                P.op("dve", lambda e: e.scalar_tensor_tensor(out=ft[fi_out][:, 0:512], in0=ft[fi_out][:, 0:512], scalar=pvc(PV_MU, chunk_idx),
                                                             in1=raw[:, 1:513], op0=ALU.mult, op1=ALU.add),
                     rd=[("f", fi_out), ("f", fi_raw), "pv"], wr=[("f", fi_out)])

            s = load_w(w_in[l], [(0, C_WD, 128)])
            proj_h(s, 0, 0)
            shift(0, 0, 1, 24)
            twd, adb = bt[7], bt[8]
            P.op("act", lambda e: e.activation(out=twd[0:64, :], in_=ft[1][0:64, 0:512], func=AF.Tanh), rd=[("f", 1)], wr=[("b", 7)])
            P.op("dve", lambda e: e.tensor_copy(out=adb[64:128, :], in_=ft[1][64:128, 0:512]), rd=[("f", 1)], wr=[("b", 8)])
            v3 = lambda ap: ap.rearrange("p (n t) -> p n t", t=64)
            r4 = lambda x_: x_.rearrange("p (n w t) -> p n w t", w=2, t=64)
            c3 = lambda x_: x_.rearrange("p (n c) -> p n c", c=128)
            BONS = [(ft[11], 11), (ft[13], 13)]
            SGS = [(ft[6], 6), (ft[14], 14)]
            KB4 = [r4(bt[1]), r4(bt[2])]
            KBK = [("b", 1), ("b", 2)]
            Vb, vbi = bt[3], 3

            def pset(par):
                b0 = 9 + par * 10
                d = {"VT": (bt[b0], b0), "KIT": (bt[b0 + 1], b0 + 1), "BIT": (bt[b0 + 2], b0 + 2),
                     "ATk": [(bt[b0 + 3], b0 + 3), (bt[b0 + 4], b0 + 4)], "ATb": [(bt[b0 + 5], b0 + 5), (bt[b0 + 6], b0 + 6)],
                     "Mt": (bt[b0 + 7], b0 + 7), "RA": [(bt[b0 + 8], b0 + 8), (bt[b0 + 9], b0 + 9)], "b0": b0}
                return d

            def stage1(hp, par):
                S = pset(par)
                RA4 = [r4(S["RA"][h][0]) for h in range(2)]
                RAK = [("b", S["RA"][h][1]) for h in range(2)]
                BON, boni = BONS[par]
                sg, sgi = SGS[par]
                ecp, eck = ec[:, par, :], ("ec", par)
                s = load_w(w_in[l], [(0, C_R + hp * 128, 128), (128, C_K + hp * 128, 128), (256, C_V + hp * 128, 128),
                                     (384, C_RG + hp * 128, 128)])
                for j in range(4):
                    for k4 in range(4):
                        for kc in range(k4 * 4, k4 * 4 + 4):
                            P.op("pe", lambda e, kc=kc, j=j: e.matmul(pp[j][:], lhsT=wbuf[s][:, kc, j * 128:(j + 1) * 128], rhs=hT[:, kc, :],
                                                                      start=(kc == 0), stop=(kc == 15)), rd=[("w", s), ("h", kc)], wr=[("pp", j)])
                        yield
                Rs, Ks, Vs = ft[3], ft[4], ft[5]
                shift(0, 0, 3, hp)
                yield
                shift(1, 1, 4, 8 + hp)
                yield
                shift(2, 0, 5, 16 + hp)
                yield
                act_silu(sg[:, 0:512], pp[3][:], [("pp", 3)], [("f", sgi)])
                P.op("pe", lambda e: e.matmul(pp[0][:], lhsT=dw[0:64, l, hp * 128:(hp + 1) * 128], rhs=twd[0:64, :], start=True, stop=True),
                     rd=["dw", ("b", 7)], wr=[("pp", 0)])
                P.op("pe", lambda e: e.matmul(pp[1][:], lhsT=dw[64:128, l, hp * 128:(hp + 1) * 128], rhs=adb[64:128, :], start=True, stop=True),
                     rd=["dw", ("b", 8)], wr=[("pp", 1)])
                yield
                LW, A = ft[7], ft[8]
                act_sigmoid(LW[:, 0:512], pp[0][:], [("pp", 0), "nb"], [("f", 7)], nbias=nb[:, l * 16 + hp:l * 16 + hp + 1], final_bias=-0.5)
                yield
                act_sigmoid(A[:, 0:512], pp[1][:], [("pp", 1), "nb"], [("f", 8)], nbias=nb[:, l * 16 + 8 + hp:l * 16 + 8 + hp + 1])
                yield
                KK, TMP = ft[9], ft[0]
                P.op("act", lambda e: e.activation(out=KK[:, 0:512], in_=Ks[:, 0:512], func=AF.Copy, scale=pvc(PV_KK, hp)),
                     rd=[("f", 4), "pv"], wr=[("f", 9)])
                P.op("dve", lambda e: e.tensor_tensor(out=bt[6][:], in0=KK[:, 0:512], in1=KK[:, 0:512], op=ALU.mult), rd=[("f", 9)], wr=[("b", 6)])
                P.op("pe", lambda e: e.matmul(pp[2][:], lhsT=blkB, rhs=bt[6][:], start=True, stop=True), rd=["ccb", ("b", 6)], wr=[("pp", 2)])
                yield
                K2 = ft[10]
                P.op("act", lambda e: e.activation(out=K2[:, 0:512], in_=A[:, 0:512], func=AF.Identity, scale=pvc(PV_KA, hp),
                                                   bias=omka[:, l * 8 + hp:l * 8 + hp + 1]), rd=[("f", 8), "pv", "omka"], wr=[("f", 10)])
                P.op("dve", lambda e: e.tensor_scalar(out=TMP[:, 0:512], in0=pp[2][:], scalar1=1e-24, scalar2=None, op0=ALU.max),
                     rd=[("pp", 2)], wr=[("f", 0)])
                yield
                act_rpow(TMP[:, 0:512], TMP[:, 0:512], [("f", 0)], [("f", 0)], 0.5)
                yield
                P.op("dve", lambda e: e.tensor_tensor(out=KK[:, 0:512], in0=KK[:, 0:512], in1=TMP[:, 0:512], op=ALU.mult),
                     rd=[("f", 9), ("f", 0)], wr=[("f", 9)])
                yield
                P.op("dve", lambda e: e.tensor_tensor(out=K2[:, 0:512], in0=K2[:, 0:512], in1=Ks[:, 0:512], op=ALU.mult),
                     rd=[("f", 10), ("f", 4)], wr=[("f", 10)])
                yield
                P.op("dve", lambda e: e.scalar_tensor_tensor(out=bt[6][:], in0=Rs[:, 0:512], scalar=pvc(PV_RK, hp), in1=K2[:, 0:512],
                                                             op0=ALU.mult, op1=ALU.mult), rd=[("f", 3), ("f", 10), "pv"], wr=[("b", 6)])
                P.op("pe", lambda e: e.matmul(pp[3][:], lhsT=blkB, rhs=bt[6][:], start=True, stop=True), rd=["ccb", ("b", 6)], wr=[("pp", 3)])
                yield
                CUM = ft[1]
                P.op("dve", lambda e: e.tensor_tensor_scan(out=CUM[:, 0:512], data0=scanm, data1=LW[:, 0:512], initial=0.0,
                                                           op0=ALU.mult, op1=ALU.subtract), rd=["ccf", ("f", 7)], wr=[("f", 1)])
                yield
                P.op("dve", lambda e: e.tensor_tensor(out=BON[:, 0:512], in0=pp[3][:], in1=Vs[:, 0:512], op=ALU.mult),
                     rd=[("pp", 3), ("f", 5)], wr=[("f", boni)])
                EP, EM = ft[12], ft[0]
                P.op("act", lambda e: e.activation(out=EP[:, 0:512], in_=CUM[:, 0:512], func=AF.Exp), rd=[("f", 1)], wr=[("f", 12)])
                yield
                P.op("dve", lambda e: e.tensor_tensor(out=EM[:, 0:512], in0=CUM[:, 0:512], in1=LW[:, 0:512], op=ALU.add),
                     rd=[("f", 1), ("f", 7)], wr=[("f", 0)])
                P.op("act", lambda e: e.activation(out=EM[:, 0:512], in_=EM[:, 0:512], func=AF.Exp), rd=[("f", 0)], wr=[("f", 0)])
                yield
                P.op("dve", lambda e: e.tensor_copy(out=ecp, in_=v3(EP[:, 0:512])[:, :, 63]), rd=[("f", 12)], wr=[eck])
                for h in range(2):
                    P.op("dve", lambda e, h=h: e.tensor_tensor(out=RA4[h][:, :, 0, :], in0=v3(Rs[:, h * 256:(h + 1) * 256]),
                                                               in1=v3(EP[:, h * 256:(h + 1) * 256]), op=ALU.mult),
                         rd=[("f", 3), ("f", 12)], wr=[RAK[h]])
                    yield
                for h in range(2):
                    P.op("dve", lambda e, h=h: e.scalar_tensor_tensor(out=RA4[h][:, :, 1, :], in0=v3(KK[:, h * 256:(h + 1) * 256]), scalar=-1.0,
                                                                      in1=v3(EM[:, h * 256:(h + 1) * 256]), op0=ALU.mult, op1=ALU.mult),
                         rd=[("f", 9), ("f", 0)], wr=[RAK[h]])
                    yield
                EN = ft[12]
                P.op("act", lambda e: e.activation(out=EN[:, 0:512], in_=CUM[:, 0:512], func=AF.Exp, scale=-1.0), rd=[("f", 1)], wr=[("f", 12)])
                BP = ft[7]
                P.op("dve", lambda e: e.tensor_tensor(out=BP[:, 0:512], in0=KK[:, 0:512], in1=A[:, 0:512], op=ALU.mult),
                     rd=[("f", 9), ("f", 8)], wr=[("f", 7)])
                yield
                for h in range(2):
                    sl = slice(h * 256, (h + 1) * 256)
                    P.op("dve", lambda e, h=h, sl=sl: e.tensor_tensor(out=KB4[h][:, :, 0, :], in0=v3(K2[:, sl]), in1=v3(EN[:, sl]), op=ALU.mult),
                         rd=[("f", 10), ("f", 12)], wr=[KBK[h]])
                    yield
                    P.op("dve", lambda e, h=h, sl=sl: e.tensor_tensor(out=KB4[h][:, :, 1, :], in0=v3(BP[:, sl]), in1=v3(EN[:, sl]), op=ALU.mult),
                         rd=[("f", 7), ("f", 12)], wr=[KBK[h]])
                    yield
                P.op("act", lambda e: e.activation(out=Vb[:], in_=Vs[:, 0:512], func=AF.Copy), rd=[("f", 5)], wr=[("b", vbi)])
                yield
                ppb = [pp[i][:].bitcast(BF16) for i in range(4)]
                for (nm, bank, srcfn, skeys, eng) in (
                        ("VT", 0, lambda n, hs: Vb[hs, n * 64:(n + 1) * 64], lambda n: [("b", vbi)], "act"),
                        ("KIT", 1, lambda n, hs: KB4[n // 4][hs, n % 4, 0, :], lambda n: [KBK[n // 4]], "dve"),
                        ("BIT", 2, lambda n, hs: KB4[n // 4][hs, n % 4, 1, :], lambda n: [KBK[n // 4]], "act")):
                    dst, di = S[nm]
                    for n in range(8):
                        for h2 in range(2):
                            hs = slice(h2 * 64, (h2 + 1) * 64)
                            P.op("pe", lambda e, n=n, hs=hs, bank=bank, srcfn=srcfn: e.transpose(
                                out=ppb[bank][hs, n * 64:(n + 1) * 64], in_=srcfn(n, hs), identity=identB[hs, hs]),
                                rd=skeys(n) + ["ccb"], wr=[("pp", bank)])
                    if eng == "act":
                        P.op("act", lambda e, dst=dst, bank=bank: e.activation(out=dst[:], in_=ppb[bank][:, 0:512], func=AF.Copy),
                             rd=[("pp", bank)], wr=[("b", di)])
                    else:
                        P.op("dve", lambda e, dst=dst, bank=bank: e.tensor_copy(out=dst[:], in_=ppb[bank][:, 0:512]),
                             rd=[("pp", bank)], wr=[("b", di)])
                    yield
                X0, Xt0 = bt[4], bt[5]
                mA = maskA.unsqueeze(1).broadcast_to([128, 4, 128])
                mL = maskL.unsqueeze(1).broadcast_to([128, 4, 64])
                for h in range(2):
                    ATk, atki = S["ATk"][h]
                    ATb, atbi = S["ATb"][h]
                    for n4 in range(4):
                        for h2 in range(2):
                            hs = slice(h2 * 64, (h2 + 1) * 64)
                            P.op("pe", lambda e, h=h, n4=n4, hs=hs: e.matmul(pp[0][hs, n4 * 128:(n4 + 1) * 128], lhsT=KB4[h][hs, n4, 0, :],
                                                                             rhs=RA4[h][hs, n4, :, :], start=True, stop=True),
                                 rd=[KBK[h], RAK[h]], wr=[("pp", 0)])
                            P.op("pe", lambda e, h=h, n4=n4, hs=hs: e.matmul(pp[1][hs, n4 * 128:(n4 + 1) * 128], lhsT=KB4[h][hs, n4, 1, :],
                                                                             rhs=RA4[h][hs, n4, :, :], start=True, stop=True),
                                 rd=[KBK[h], RAK[h]], wr=[("pp", 1)])
                            P.op("pe", lambda e, h=h, n4=n4, hs=hs: e.matmul(pp[2][hs, n4 * 64:(n4 + 1) * 64], lhsT=RA4[h][hs, n4, 1, :],
                                                                             rhs=KB4[h][hs, n4, 1, :], start=True, stop=True),
                                 rd=[KBK[h], RAK[h]], wr=[("pp", 2)])
                    yield
                    P.op("dve", lambda e, ATk=ATk: e.tensor_tensor(out=c3(ATk[:]), in0=c3(pp[0][:]), in1=mA, op=ALU.mult),
                         rd=[("pp", 0), "ccf"], wr=[("b", atki)])
                    P.op("dve", lambda e, ATb=ATb: e.tensor_tensor(out=c3(ATb[:]), in0=c3(pp[1][:]), in1=mA, op=ALU.mult),
                         rd=[("pp", 1), "ccf"], wr=[("b", atbi)])
                    P.op("dve", lambda e, h=h: e.tensor_tensor(out=X0[:, h * 256:(h + 1) * 256].rearrange("p (n c) -> p n c", c=64),
                                                               in0=pp[2][:, 0:256].rearrange("p (n c) -> p n c", c=64), in1=mL, op=ALU.mult),
                         rd=[("pp", 2), "ccf"], wr=[("b", 4)])
                    P.op("act", lambda e, h=h, ATb=ATb: e.activation(out=Xt0[:, h * 256:(h + 1) * 256].rearrange("p (n c) -> p n c", c=64),
                                                                     in_=c3(ATb[:])[:, :, 64:128], func=AF.Copy), rd=[("b", atbi)], wr=[("b", 5)])
                    yield
                Mt, mti = S["Mt"]
                iI = identI.unsqueeze(1).broadcast_to([128, 8, 64])
                P.op("dve", lambda e: e.tensor_tensor(out=v3(Mt[:]), in0=v3(Xt0[:]), in1=iI, op=ALU.add), rd=[("b", 5), "ccf"], wr=[("b", mti)])
                yield
                pingX, pingXt = [(bt[4], 4), (bt[0], 0)], [(bt[5], 5), (Vb, vbi)]

                def sq(Xc, Xtc, xi, xti):
                    for n in range(8):
                        for h2 in range(2):
                            hs = slice(h2 * 64, (h2 + 1) * 64)
                            cs = slice(n * 64, (n + 1) * 64)
                            P.op("pe", lambda e, hs=hs, cs=cs: e.matmul(pp[0][hs, cs], lhsT=Xc[hs, cs], rhs=Xtc[hs, cs], start=True, stop=True),
                                 rd=[("b", xi), ("b", xti)], wr=[("pp", 0)])
                            P.op("pe", lambda e, hs=hs, cs=cs: e.matmul(pp[1][hs, cs], lhsT=Xtc[hs, cs], rhs=Xc[hs, cs], start=True, stop=True),
                                 rd=[("b", xi), ("b", xti)], wr=[("pp", 1)])

                def ev(lvl):
                    Xn, xni = pingX[lvl % 2]
                    Xtn, xtni = pingXt[lvl % 2]
                    P.op("act", lambda e: e.activation(out=Xtn[:], in_=pp[0][:], func=AF.Copy), rd=[("pp", 0)], wr=[("b", xtni)])
                    P.op("dve", lambda e: e.tensor_copy(out=Xn[:], in_=pp[1][:]), rd=[("pp", 1)], wr=[("b", xni)])
                    return Xn, Xtn, xni, xtni

                def mtmm(Xn, xni):
                    for n in range(8):
                        for h2 in range(2):
                            hs = slice(h2 * 64, (h2 + 1) * 64)
                            cs = slice(n * 64, (n + 1) * 64)
                            P.op("pe", lambda e, hs=hs, cs=cs: e.matmul(pp[2][hs, cs], lhsT=Xn[hs, cs], rhs=Mt[hs, cs], start=True, stop=True),
                                 rd=[("b", xni), ("b", mti)], wr=[("pp", 2)])

                def madd():
                    P.op("dve", lambda e: e.tensor_tensor(out=Mt[:], in0=pp[2][:], in1=Mt[:], op=ALU.add), rd=[("pp", 2), ("b", mti)], wr=[("b", mti)])

                sq(X0, Xt0, 4, 5)
                yield
                Xc, Xtc, xi, xti = ev(1)
                yield
                for lvl in range(2, 6):
                    sq(Xc, Xtc, xi, xti)
                    yield
                    mtmm(Xc, xi)
                    yield
                    Xc, Xtc, xi, xti = ev(lvl)
                    yield
                    madd()
                    yield
                mtmm(Xc, xi)
                yield
                madd()
                yield

            def stage2(hp, par):
                S = pset(par)
                RA4 = [r4(S["RA"][h][0]) for h in range(2)]
                RAK = [("b", S["RA"][h][1]) for h in range(2)]
                ATk3 = [c3(S["ATk"][h][0][:]) for h in range(2)]
                ATb3 = [c3(S["ATb"][h][0][:]) for h in range(2)]
                ATKK = [("b", S["ATk"][h][1]) for h in range(2)]
                ATBK = [("b", S["ATb"][h][1]) for h in range(2)]
                VT, vti = S["VT"]
                KIT, kiti = S["KIT"]
                BIT, biti = S["BIT"]
                Mt, mti = S["Mt"]
                BON, boni = BONS[par]
                sg, sgi = SGS[par]
                ecp, eck = ec[:, par, :], ("ec", par)
                hh = l * 8 + hp
                R0b, Ub = sm[:, 0:64], sm[:, 64:128]
                for n in range(8):
                    h, n4 = n // 4, n % 4
                    cs = slice(n * 64, (n + 1) * 64)
                    P.op("act", lambda e, n=n: e.activation(out=gs[:, 0:64], in_=Hf[:, hh, :], func=AF.Copy, scale=ecp[:, n:n + 1]),
                         rd=["Hf", eck], wr=["gs"])
                    for h2 in range(2):
                        hs = slice(h2 * 64, (h2 + 1) * 64)
                        P.op("pe", lambda e, hs=hs, h=h, n4=n4: e.matmul(pq[3][hs, 0:64], lhsT=RA4[h][hs, n4, 1, :], rhs=Hb[hs, hh, :], start=True, stop=False),
                             rd=[RAK[h], "Hb"], wr=[("pq", 3)])
                        P.op("pe", lambda e, hs=hs, h=h, n4=n4, cs=cs: e.matmul(pq[3][hs, 0:64], lhsT=ATk3[h][hs, n4, 64:128], rhs=VT[hs, cs], start=False, stop=True),
                             rd=[ATKK[h], ("b", vti)], wr=[("pq", 3)])
                    yield
                    P.op("act", lambda e: e.activation(out=R0b, in_=pq[3][:, 0:64], func=AF.Copy), rd=[("pq", 3)], wr=["sm0"])
                    for h2 in range(2):
                        hs = slice(h2 * 64, (h2 + 1) * 64)
                        P.op("pe", lambda e, hs=hs, cs=cs: e.matmul(pq[3][hs, 64:128], lhsT=Mt[hs, cs], rhs=sm[hs, 0:64], start=True, stop=True),
                             rd=[("b", mti), "sm0"], wr=[("pq", 3)])
                    yield
                    P.op("dve", lambda e: e.tensor_copy(out=Ub, in_=pq[3][:, 64:128]), rd=[("pq", 3)], wr=["sm1"])
                    for h2 in range(2):
                        hs = slice(h2 * 64, (h2 + 1) * 64)
                        P.op("pe", lambda e, hs=hs, cs=cs: e.matmul(pq[3][hs, 128:192], lhsT=KIT[hs, cs], rhs=VT[hs, cs], start=True, stop=False),
                             rd=[("b", kiti), ("b", vti)], wr=[("pq", 3)])
                        P.op("pe", lambda e, hs=hs, cs=cs: e.matmul(pq[3][hs, 128:192], lhsT=BIT[hs, cs], rhs=sm[hs, 64:128], start=False, stop=True),
                             rd=[("b", biti), "sm1"], wr=[("pq", 3)])
                    for h2 in range(2):
                        hs = slice(h2 * 64, (h2 + 1) * 64)
                        P.op("pe", lambda e, hs=hs, h=h, n4=n4, cs=cs: e.matmul(pq[0][hs, cs], lhsT=Hb[hs, hh, :], rhs=RA4[h][hs, n4, 0, :], start=True, stop=False),
                             rd=["Hb", RAK[h]], wr=[("pq", 0)])
                        P.op("pe", lambda e, hs=hs, h=h, n4=n4, cs=cs: e.matmul(pq[0][hs, cs], lhsT=sm[hs, 64:128], rhs=ATb3[h][hs, n4, 0:64], start=False, stop=False),
                             rd=["sm1", ATBK[h]], wr=[("pq", 0)])
                        P.op("pe", lambda e, hs=hs, h=h, n4=n4, cs=cs: e.matmul(pq[0][hs, cs], lhsT=VT[hs, cs], rhs=ATk3[h][hs, n4, 0:64], start=False, stop=True),
                             rd=[("b", vti), ATKK[h]], wr=[("pq", 0)])
                    yield
                    P.op("dve", lambda e, n=n: e.scalar_tensor_tensor(out=Hb[:, hh, :], in0=pq[3][:, 128:192], scalar=ecp[:, n:n + 1], in1=gs[:, 0:64],
                                                                      op0=ALU.mult, op1=ALU.add), rd=[("pq", 3), eck, "gs"], wr=["Hb"])
                    P.op("dve", lambda e, n=n: e.scalar_tensor_tensor(out=Hf[:, hh, :], in0=pq[3][:, 128:192], scalar=ecp[:, n:n + 1], in1=gs[:, 0:64],
                                                                      op0=ALU.mult, op1=ALU.add), rd=[("pq", 3), eck, "gs"], wr=["Hf"])
                    yield
                b0 = S["b0"]
                Yf, MEAN, VAR = f32v(b0), f32v(b0 + 3), f32v(b0 + 5)
                YFK, MK, VK = [("b", b0), ("b", b0 + 1)], [("b", b0 + 3), ("b", b0 + 4)], [("b", b0 + 5), ("b", b0 + 6)]
                Yb, ybi, Y2, y2i = bt[b0 + 2], b0 + 2, bt[b0 + 7], b0 + 7
                P.op("act", lambda e: e.activation(out=Yf, in_=pq[0][:], func=AF.Copy), rd=[("pq", 0)], wr=YFK)
                yield
                P.op("dve", lambda e: e.tensor_copy(out=Yb[:], in_=Yf), rd=YFK, wr=[("b", ybi)])
                P.op("act", lambda e: e.activation(out=Y2[:], in_=Yf, func=AF.Square), rd=YFK, wr=[("b", y2i)])
                P.op("pe", lambda e: e.matmul(pq[1][:], lhsT=blkB, rhs=Yb[:], start=True, stop=True), rd=["ccb", ("b", ybi)], wr=[("pq", 1)])
                P.op("pe", lambda e: e.matmul(pq[2][:], lhsT=blkB, rhs=Y2[:], start=True, stop=True), rd=["ccb", ("b", y2i)], wr=[("pq", 2)])
                yield
                P.op("act", lambda e: e.activation(out=MEAN, in_=pq[1][:], func=AF.Copy, scale=1.0 / 64), rd=[("pq", 1)], wr=MK)
                yield
                P.op("dve", lambda e: e.tensor_tensor(out=VAR, in0=MEAN, in1=MEAN, op=ALU.mult), rd=MK, wr=VK)
                yield
                P.op("dve", lambda e: e.scalar_tensor_tensor(out=VAR, in0=pq[2][:], scalar=1.0 / 64, in1=VAR,
                                                             op0=ALU.mult, op1=ALU.subtract), rd=[("pq", 2)] + VK, wr=VK)
                yield
                act_rpow(VAR, VAR, VK, VK, 0.5, bias=64e-5)
                P.op("dve", lambda e: e.tensor_tensor(out=Yf, in0=Yf, in1=MEAN, op=ALU.subtract), rd=YFK + MK, wr=YFK)
                yield
                P.op("dve", lambda e: e.tensor_tensor(out=Yf, in0=Yf, in1=VAR, op=ALU.mult), rd=YFK + VK, wr=YFK)
                yield
                P.op("act", lambda e: e.activation(out=Yf, in_=Yf, func=AF.Identity, scale=pvc(PV_GW, hp), bias=pvc(PV_GB, hp)),
                     rd=YFK + ["pv"], wr=YFK)
                yield
                P.op("dve", lambda e: e.tensor_tensor(out=Yf, in0=Yf, in1=BON[:, 0:512], op=ALU.add), rd=YFK + [("f", boni)], wr=YFK)
                yield
                P.op("dve", lambda e: e.tensor_tensor(out=ybr[:, hp, :], in0=Yf, in1=sg[:, 0:512], op=ALU.mult),
                     rd=YFK + [("f", sgi)], wr=[("y", hp)])
                yield

            def drive(gb, ga, ratio):
                acc, a_done, b_done = 0.0, ga is None, gb is None
                while not (a_done and b_done):
                    if not b_done:
                        try:
                            next(gb)
                        except StopIteration:
                            b_done = True
                    if not a_done:
                        acc += ratio if not b_done else 1e9
                        while acc >= 1.0 and not a_done:
                            acc -= 1.0
                            try:
                                next(ga)
                            except StopIteration:
                                a_done = True

            drive(None, stage1(0, 0), 1.0)
            for hp in range(8):
                drive(stage2(hp, hp % 2), stage1(hp + 1, (hp + 1) % 2) if hp < 7 else None, 2.2)

            s = load_w(w_in[l], [(0, C_SK, 64), (64, C_SK, 64), (128, C_SK + 64, 64), (192, C_SK + 64, 64), (256, C_SV, 128)])
            for g in range(2):
                proj_h(s, g, g)
                P.op("act", lambda e, g=g: e.activation(out=kd[:, l * 2 + g, 128:640], in_=pp[g][:], func=AF.Copy), rd=[("pp", g)], wr=[("kd", g)])
            for blk in range(4):
                for kc in range(16):
                    P.op("pe", lambda e, kc=kc, blk=blk, s=s: e.matmul(pp[2][:, blk * 128:(blk + 1) * 128], lhsT=hT[:, kc, blk * 128:(blk + 1) * 128],
                                                                       rhs=wbuf[s][:, kc, 256:384], start=(kc == 0), stop=(kc == 15)),
                         rd=[("w", s), ("h", kc)], wr=[("pp", 2)])
            P.op("act", lambda e: e.activation(out=vt[:, l * 5 + 1:l * 5 + 5, :], in_=pp[2][:].rearrange("p (a b) -> p a b", b=128), func=AF.Copy),
                 rd=[("pp", 2)], wr=["vt"])
            for cp in range(4):
                c0 = 2 * cp
                s = load_w(w_in[l], [(0, C_SQ + c0 * 128, 128), (128, C_SG + c0 * 128, 128), (256, C_SQ + (c0 + 1) * 128, 128),
                                     (384, C_SG + (c0 + 1) * 128, 128)])
                for j in range(4):
                    proj_h(s, j, j)
                for ci in range(2):
                    c = c0 + ci
                    g = c // 4
                    qTb, sgs = bt[0], ft[3]
                    P.op("act", lambda e, ci=ci: e.activation(out=qTb[:], in_=pp[2 * ci][:], func=AF.Copy), rd=[("pp", 2 * ci)], wr=[("b", 0)])
                    act_silu(sgs[:, 0:512], pp[2 * ci + 1][:], [("pp", 2 * ci + 1)], [("f", 3)])
                    for blk in range(4):
                        bs = slice(blk * 128, (blk + 1) * 128)
                        ex, exm = bt[1], bt[2]
                        for h2 in range(2):
                            hs = slice(h2 * 64, (h2 + 1) * 64)
                            for w_ in range(2):
                                P.op("pe", lambda e, hs=hs, h2=h2, w_=w_, blk=blk, bs=bs, g=g: e.matmul(
                                    pq[h2][:, w_ * 128:(w_ + 1) * 128], lhsT=kd[hs, l * 2 + g, (blk + w_) * 128:(blk + w_ + 1) * 128],
                                    rhs=qTb[hs, bs], start=True, stop=True), rd=[("kd", g), ("b", 0)], wr=[("pq", h2)])
                            P.op("act", lambda e, h2=h2: e.activation(out=ex[:, h2 * 256:(h2 + 1) * 256], in_=pq[h2][:, 0:256], func=AF.Exp, scale=0.125),
                                 rd=[("pq", h2)], wr=[("b", 1)])
                        mk = (maskS0 if (ti == 0 and blk == 0) else maskS).unsqueeze(1).broadcast_to([128, 2, 256])
                        P.op("dve", lambda e, mk=mk: e.tensor_tensor(out=exm[:].rearrange("p (a b) -> p a b", b=256),
                                                                   in0=ex[:].rearrange("p (a b) -> p a b", b=256), in1=mk, op=ALU.mult),
                             rd=[("b", 1), "ccb"], wr=[("b", 2)])
                        for h2 in range(2):
                            hs = slice(h2 * 64, (h2 + 1) * 64)
                            for w_ in range(2):
                                P.op("pe", lambda e, hs=hs, h2=h2, w_=w_, blk=blk, bs=bs, g=g: e.matmul(
                                    pq[2][hs, bs], lhsT=vt[:, l * 5 + blk + w_, g * 64:(g + 1) * 64],
                                    rhs=exm[:, h2 * 256 + w_ * 128:h2 * 256 + (w_ + 1) * 128], start=(w_ == 0), stop=(w_ == 1)),
                                    rd=["vt", ("b", 2)], wr=[("pq", 2)])
                            for w_ in range(2):
                                P.op("pe", lambda e, hs=hs, h2=h2, w_=w_, bs=bs: e.matmul(
                                    pq[3][hs, bs], lhsT=onesB[:, 0:64], rhs=exm[:, h2 * 256 + w_ * 128:h2 * 256 + (w_ + 1) * 128],
                                    start=(w_ == 0), stop=(w_ == 1)), rd=["ccb", ("b", 2)], wr=[("pq", 3)])
                    DEN = ft[4]
                    act_rpow(DEN[:, 0:512], pq[3][:], [("pq", 3), "esink"], [("f", 4)], 1.0, bias=esink[:, l * 8 + c:l * 8 + c + 1])
                    P.op("dve", lambda e: e.tensor_tensor(out=DEN[:, 0:512], in0=pq[2][:], in1=DEN[:, 0:512], op=ALU.mult),
                         rd=[("pq", 2), ("f", 4)], wr=[("f", 4)])
                    P.op("dve", lambda e, c=c: e.tensor_tensor(out=ybr[:, 8 + c, :], in0=DEN[:, 0:512], in1=sgs[:, 0:512], op=ALU.mult),
                         rd=[("f", 4), ("f", 3)], wr=[("y", 8 + c)])
            for g in range(2):
                P.op("dve", lambda e, g=g: e.tensor_copy(out=kd[:, l * 2 + g, 0:128], in_=kd[:, l * 2 + g, 512:640]), rd=[("kd", g)], wr=[("kd", g)])
            P.op("dve", lambda e: e.tensor_copy(out=vt[:, l * 5, :], in_=vt[:, l * 5 + 4, :]), rd=["vt"], wr=["vt"])

            for h in range(4):
                s = load_w(w_in[l], [(0, C_XQ + h * 256, 256), (256, C_XG + h * 256, 256)])
                for j in range(4):
                    proj_h(s, j, j)
                qx, sgx = [bt[0], bt[1]], [ft[3], ft[4]]
                for j in range(2):
                    P.op("act", lambda e, j=j: e.activation(out=qx[j][:], in_=pp[j][:], func=AF.Copy), rd=[("pp", j)], wr=[("b", j)])
                    act_silu(sgx[j][:, 0:512], pp[2 + j][:], [("pp", 2 + j)], [("f", 3 + j)])
                exx = [bt[2], bt[3]]
                for mt in range(2):
                    for j in range(2):
                        P.op("pe", lambda e, mt=mt, j=j, h=h: e.matmul(pq[mt][:], lhsT=kmT[:, l * 8 + h * 2 + j, mt * 128:(mt + 1) * 128], rhs=qx[j][:],
                                                                       start=(j == 0), stop=(j == 1)), rd=["kmT", ("b", j)], wr=[("pq", mt)])
                    P.op("act", lambda e, mt=mt: e.activation(out=exx[mt][:], in_=pq[mt][:], func=AF.Exp, scale=1.0 / 16), rd=[("pq", mt)], wr=[("b", 2 + mt)])
                for j in range(2):
                    for mt in range(2):
                        P.op("pe", lambda e, mt=mt, j=j, h=h: e.matmul(pp[j][:], lhsT=vm[:, l * 2 + mt, h * 256 + j * 128:h * 256 + (j + 1) * 128],
                                                                       rhs=exx[mt][:], start=(mt == 0), stop=(mt == 1)),
                             rd=["vm", ("b", 2 + mt)], wr=[("pp", j)])
                for mt in range(2):
                    P.op("pe", lambda e, mt=mt: e.matmul(pq[2][:], lhsT=onesB, rhs=exx[mt][:], start=(mt == 0), stop=(mt == 1)),
                         rd=["ccb", ("b", 2 + mt)], wr=[("pq", 2)])
                REC = ft[5]
                act_rpow(REC[:, 0:512], pq[2][:], [("pq", 2)], [("f", 5)], 1.0)
                for j in range(2):
                    P.op("dve", lambda e, j=j: e.tensor_tensor(out=sgx[j][:, 0:512], in0=sgx[j][:, 0:512], in1=REC[:, 0:512], op=ALU.mult),
                         rd=[("f", 3 + j), ("f", 5)], wr=[("f", 3 + j)])
                    P.op("dve", lambda e, j=j, h=h: e.tensor_tensor(out=ybr[:, 16 + h * 2 + j, :], in0=pp[j][:], in1=sgx[j][:, 0:512], op=ALU.mult),
                         rd=[("pp", j), ("f", 3 + j)], wr=[("y", 16 + h * 2 + j)])

            for dg in range(4):
                for br in range(3):
                    s = load_w(w_in[l], [(0, C_MG + br * 2048 + dg * 512, 512)])
                    for j in range(4):
                        proj_h(s, j, j)
                        act_sigmoid(ft[3 + j][:, 0:512], pp[j][:], [("pp", j)], [("f", 3 + j)])
                    s = load_w(w_up[br][l], [(0, dg * 512, 512)], nk=8)
                    for j in range(4):
                        proj(s, j, pq[j][:], ("pq", j), lambda kc, br=br: ybr[:, br * 8 + kc, :], lambda kc, br=br: [("y", br * 8 + kc)], nk=8)
                        acc = ft[7 + j]
                        if br == 0:
                            P.op("dve", lambda e, j=j, acc=acc: e.tensor_tensor(out=acc[:, 0:512], in0=pq[j][:], in1=ft[3 + j][:, 0:512], op=ALU.mult),
                                 rd=[("pq", j), ("f", 3 + j)], wr=[("f", 7 + j)])
                        else:
                            P.op("dve", lambda e, j=j: e.tensor_tensor(out=ft[3 + j][:, 0:512], in0=pq[j][:], in1=ft[3 + j][:, 0:512], op=ALU.mult),
                                 rd=[("pq", j), ("f", 3 + j)], wr=[("f", 3 + j)])
                            if br == 1:
                                P.op("dve", lambda e, j=j, acc=acc: e.tensor_tensor(out=acc[:, 0:512], in0=acc[:, 0:512], in1=ft[3 + j][:, 0:512], op=ALU.add),
                                     rd=[("f", 7 + j), ("f", 3 + j)], wr=[("f", 7 + j)])
                            else:
                                dc = dg * 4 + j
                                P.op("dve", lambda e, j=j, acc=acc, dc=dc: e.tensor_tensor(out=bt[dc][:], in0=acc[:, 0:512], in1=ft[3 + j][:, 0:512], op=ALU.add),
                                     rd=[("f", 7 + j), ("f", 3 + j)], wr=[("b", dc)])
            for dg in range(4):
                s = load_w(w_out[l], [(0, dg * 512, 512)])
                for j in range(4):
                    dc = dg * 4 + j
                    proj(s, j, pp[j][:], ("pp", j), lambda kc: bt[kc][:], lambda kc: [("b", kc)])
                    P.op("act", lambda e, j=j, dc=dc: e.activation(out=o_f[:, dc, :], in_=pp[j][:], func=AF.Copy), rd=[("pp", j)], wr=okeys(dc))
                    bi = 27 + (dc % 2)
                    t = bt[bi]
                    P.op("act", lambda e, dc=dc, t=t: e.activation(out=t[:], in_=o_f[:, dc, :], func=AF.Square), rd=okeys(dc), wr=[("b", bi)])
                    P.op("pe", lambda e, dc=dc, t=t: e.matmul(pq[0][:], lhsT=onesB, rhs=t[:], start=(dc == 0), stop=(dc == 15)),
                         rd=[("b", bi), "ccb"], wr=[("pq", 0)])
            rs2 = ft[2]
            act_rpow(rs2[:, 0:512], pq[0][:], [("pq", 0)], [("f", 2)], 0.5, scale=1.0 / D, bias=1e-6)
            for dc in range(16):
                t = ft[3 + (dc % 2)]
                P.op("dve", lambda e, dc=dc, t=t: e.scalar_tensor_tensor(out=t[:, 0:512], in0=o_f[:, dc, :], scalar=pvc(PV_GPOST, dc), in1=rs2[:, 0:512],
                                                                         op0=ALU.mult, op1=ALU.mult), rd=okeys(dc) + ["pv", ("f", 2)], wr=[("f", 3 + dc % 2)])
                P.op("dve", lambda e, dc=dc, t=t: e.tensor_tensor(out=x_res[:, dc, :], in0=x_res[:, dc, :], in1=t[:, 0:512], op=ALU.add),
                     rd=[("x", dc), ("f", 3 + dc % 2)], wr=[("x", dc)])
            if l == L - 1:
                for q in range(4):
                    P.dma("sp", lambda e, q=q: e.dma_start(out=outT[q * 512:(q + 1) * 512, t0:t0 + TT].rearrange("(dc p) t -> p dc t", p=128),
                                                           in_=x_res[:, q * 4:(q + 1) * 4, :]),
                          f"o{q}", rd=[("x", q * 4 + i) for i in range(4)], wr=[("out", q)])

        for ti in range(NT):
            for l in range(L):
                block(ti, l)
        P.wait_all("sp", [("out", q) for q in range(4)])
        P.emit()
        print("instructions:", P.n_inst, "sems:", P.sem_id)
    return nc


def _consts():
    cf = np.zeros((128, NCC), np.float32)
    cb = np.zeros((128, NCC), np.float32)
    p = np.arange(128)[:, None]
    c = np.arange(128)[None, :]
    cf[:, CF_ONES:CF_ONES + 128] = 1.0
    cb[:, CB_ONES:CB_ONES + 128] = 1.0
    cb[:, CB_BLK:CB_BLK + 128] = (p // 64 == c // 64)
    cb[:, CB_ID:CB_ID + 128] = (p == c)
    j = p % 64
    t = np.arange(64)[None, :]
    cf[:, CF_MA:CF_MA + 64] = (j <= t)
    cf[:, CF_MA + 64:CF_MA + 128] = (j < t)
    cf[:, CF_ML:CF_ML + 64] = (t < j)
    cf[:, CF_II:CF_II + 64] = (j == t)
    q = np.arange(128)[None, :]
    cb[:, CB_MS:CB_MS + 128] = (p > q)
    cb[:, CB_MS + 128:CB_MS + 256] = (q >= p)
    cb[:, CB_MS0 + 128:CB_MS0 + 256] = (q >= p)
    sc = np.ones((128, 512), np.float32)
    sc[:, 0::64] = 0.0
    cf[:, CF_SCAN:CF_SCAN + 512] = sc
    return cf, cb


def _layout(inputs, L):
    col = lambda v, n: np.ascontiguousarray(v.reshape(n, 128).T)
    pvs = []
    for l in range(L):
        sk = np.repeat(inputs["attn_sinks"][l], 64)
        pvs += [col(inputs["g_pre"][l], 16), col(inputs["g_post"][l], 16), col(inputs["g_mem"][l], 16),
                col(inputs["mu_shift"][l], 25), col(inputs["decay_base"][l], 8), col(inputs["iclr_base"][l], 8),
                col(inputs["k_k"][l], 8), col(inputs["k_a"][l], 8), col(inputs["r_k"][l].reshape(-1), 8),
                col(inputs["gn_w"][l], 8), col(inputs["gn_b"][l], 8), col(sk, 8)]
    pvd = np.ascontiguousarray(np.concatenate(pvs, axis=1), dtype=np.float32)
    dwd = np.ascontiguousarray(np.concatenate([inputs["decay_up"][:L], inputs["iclr_up"][:L]], axis=1), dtype=np.float32)
    return pvd, dwd


def run(inputs, T, L, B, trace=False):
    inputs = {k: np.asarray(v, dtype=np.float32) for k, v in inputs.items()}
    nc = build(T, L)
    pvd, dwd = _layout(inputs, L)
    cf, cb = _consts()
    shared = {
        "w_in": np.ascontiguousarray(inputs["w_in"][:L]), "w_mkv": np.ascontiguousarray(inputs["w_mem_kv"][:L]),
        "w_up0": np.ascontiguousarray(inputs["w_up_rwkv"][:L]), "w_up1": np.ascontiguousarray(inputs["w_up_swa"][:L]),
        "w_up2": np.ascontiguousarray(inputs["w_up_xattn"][:L]), "w_out": np.ascontiguousarray(inputs["w_out"][:L]),
        "dwd": dwd, "pvd": pvd, "ccdf": cf, "ccdb": cb,
    }
    in_maps = []
    for b in range(B):
        m = dict(shared)
        m["xT"] = np.ascontiguousarray(inputs["x"][b].T)
        m["memT"] = np.ascontiguousarray(inputs["mem"][b].T)
        in_maps.append(m)
    res = run_bass_kernel_spmd(nc, in_maps, core_ids=list(range(B)), trace=trace)
    out = np.stack([np.ascontiguousarray(r["outT"].T) for r in res.results], axis=0)
    return out.astype(np.float32), res


def kernel(**inputs):
    out, _ = run(inputs, 2048, 2, 8)
    return out
```

```python
import contextlib
import numpy as np
import concourse.bass as bass
import concourse.mybir as mybir
from concourse.bass_utils import run_bass_kernel_spmd

F32 = mybir.dt.float32
BF16 = mybir.dt.bfloat16
AF = mybir.ActivationFunctionType
ALU = mybir.AluOpType

D = 2048
DIN = 14720
MEM = 256
TT = 512
EPOCH = 30000

C_R, C_K, C_V, C_WD = 0, 1024, 2048, 3072
C_RG = 3200
C_SQ = 4224
C_SK = 5248
C_SV = 5376
C_SG = 5504
C_XQ = 6528
C_XG = 7552
C_MG = 8576

PV_GPRE, PV_GPOST, PV_GMEM, PV_MU = 0, 16, 32, 48
PV_DB, PV_IB, PV_KK, PV_KA, PV_RK, PV_GW, PV_GB, PV_SINK = 73, 81, 89, 97, 105, 113, 121, 129
NPV = 137
CF_ONES, CF_MA, CF_ML, CF_II, CF_SCAN = 0, 128, 256, 320, 384
CB_ONES, CB_BLK, CB_ID, CB_MS, CB_MS0 = 0, 128, 256, 384, 640
NCC = 896


class Prog:
    COMPUTE = ("pe", "act", "dve", "pool")

    def __init__(self, nc, stack):
        self.nc, self.stack = nc, stack
        self.eng_names = ("pe", "act", "dve", "pool", "sp")
        self.ops = {e: [] for e in self.eng_names}
        self.cnt = {e: 0 for e in self.COMPUTE}
        self.sem_objs, self.sem_id, self.cur_sem = {}, 0, {}
        for e in self.COMPUTE:
            self.cur_sem[e] = self._new_sem()
        self.waited = {e: {} for e in self.eng_names}
        self.buf, self.dma_sems = {}, {}
        self.n_inst = 0
        self.fin = {}
        self.efree = {e: 0.0 for e in self.eng_names}
        self.stream = None
        self.sclock = {}
        self.E = {"pe": nc.tensor, "act": nc.scalar, "dve": nc.vector, "pool": nc.gpsimd, "sp": nc.sync}

    def _new_sem(self):
        s = self.stack.enter_context(self.nc.semaphore(f"s{self.sem_id}"))
        self.sem_objs[self.sem_id] = s
        self.sem_id += 1
        return self.sem_id - 1

    def _deps(self, eng, reads, writes):
        need = {}
        self._ready = 0.0

        def add(tok):
            sidx, val, teng = tok
            f = self.fin.get((sidx, val), 0.0)
            if f > self._ready:
                self._ready = f
            if teng == "pe" and eng == "pe":
                return
            if need.get(sidx, 0) < val:
                need[sidx] = val
        for k in reads:
            st = self.buf.get(k)
            if st and st[0] is not None:
                add(st[0])
        for k in writes:
            st = self.buf.get(k)
            if st:
                if st[0] is not None:
                    add(st[0])
                for t in st[1]:
                    add(t)
        for sidx, val in need.items():
            if self.waited[eng].get(sidx, 0) >= val:
                continue
            self.waited[eng][sidx] = val
            self.E[eng].wait_ge(self.sem_objs[sidx], val)

    def _record(self, tok, reads, writes):
        for k in reads:
            self.buf.setdefault(k, [None, []])[1].append(tok)
        for k in writes:
            self.buf[k] = [tok, []]

    COST = {"pe": 0.06, "act": 0.68, "dve": 0.68, "pool": 0.7}

    def _time(self, eng, tok, c):
        st = max(self.efree[eng], self._ready + 0.15)
        f = st + c
        self.efree[eng] = f
        self.fin[(tok[0], tok[1])] = f
        if self.stream is not None:
            self.sclock[self.stream] = max(self.sclock.get(self.stream, 0.0), f)

    def op(self, eng, fn, rd=(), wr=(), c=None):
        self._deps(eng, rd, wr)
        if self.cnt[eng] >= EPOCH:
            self.cur_sem[eng] = self._new_sem()
            self.cnt[eng] = 0
        self.cnt[eng] += 1
        tok = (self.cur_sem[eng], self.cnt[eng], eng)
        self._time(eng, tok, self.COST[eng] if c is None else c)
        fn(self.E[eng]).then_inc(self.sem_objs[self.cur_sem[eng]], 1)
        self._record(tok, rd, wr)
        self.n_inst += 1

    def dma(self, eng, fn, semkey, rd=(), wr=()):
        self._deps(eng, rd, wr)
        if semkey not in self.dma_sems:
            self.dma_sems[semkey] = [self._new_sem(), 0]
        ds = self.dma_sems[semkey]
        ds[1] += 16
        tok = (ds[0], ds[1], "dma")
        self.fin[(tok[0], tok[1])] = max(self.efree[eng], self._ready) + 4.0
        fn(self.E[eng]).then_inc(self.sem_objs[ds[0]], 16)
        self._record(tok, rd, wr)
        self.n_inst += 1

    def wait_all(self, eng, keys):
        self._deps(eng, keys, ())

    def emit(self):
        return

    def emit_old(self):
        engmap = {"pe": "tensor", "act": "scalar", "dve": "vector", "pool": "gpsimd", "sp": "sync"}
        with self.nc.Block() as block:
            for e in self.eng_names:
                ops = self.ops[e]
                if not ops:
                    continue

                def body(eng, ops=ops):
                    for o in ops:
                        if o[0] == "wait":
                            eng.wait_ge(self.sem_objs[o[1]], o[2])
                        else:
                            o[1](eng).then_inc(self.sem_objs[o[2]], o[3])
                getattr(block, engmap[e])(body)


def build(T, L):
    NT = T // TT
    nc = bass.Bass("TRN2", target_bir_lowering=False)
    dr = lambda name, shape, kind="ExternalInput": nc.dram_tensor(name, shape, F32, kind=kind).ap()
    xT = dr("xT", [D, T])
    memT = dr("memT", [D, MEM])
    w_in = dr("w_in", [L, D, DIN])
    w_mkv = dr("w_mkv", [L, D, 2048])
    w_up = [dr(f"w_up{i}", [L, 1024, D]) for i in range(3)]
    w_out = dr("w_out", [L, D, D])
    dwd = dr("dwd", [L, 128, 1024])
    pvd = dr("pvd", [128, L * NPV])
    ccdf = dr("ccdf", [128, NCC])
    ccdb = dr("ccdb", [128, NCC])
    outT = dr("outT", [D, T], kind="ExternalOutput")

    with contextlib.ExitStack() as st:
        P = Prog(nc, st)
        sb = lambda name, shape, dt: st.enter_context(nc.sbuf_tensor(name, shape, dt))
        x_res = sb("x_res", [128, 16, TT], F32)
        HY = sb("HY", [128, 10240], F32)
        hT = HY[:, 0:4096].bitcast(BF16).rearrange("p (a b) -> p a b", b=TT)
        ybr = HY[:, 4096:10240].bitcast(BF16).rearrange("p (a b) -> p a b", b=TT)
        o_f = HY[:, 0:8192].rearrange("p (a b) -> p a b", b=TT)

        def okeys(dc):
            return [("h", 2 * dc), ("h", 2 * dc + 1)] if dc < 8 else [("y", 2 * (dc - 8)), ("y", 2 * (dc - 8) + 1)]
        wbuf = [sb(f"wbuf{i}", [128, 16, 512], BF16) for i in range(2)]
        NF, NB = 15, 29
        ft = [sb(f"ft{i}", [128, 514], F32) for i in range(NF)]
        bt_all = sb("bt_all", [128, NB, 512], BF16)
        bt = [bt_all[:, i, :] for i in range(NB)]
        f32v = lambda i: bt_all[:, i:i + 2, :].rearrange("p a b -> p (a b)").bitcast(F32)
        kmT = sb("kmT", [128, L * 8, MEM], BF16)
        vm = sb("vm", [128, L * 2, 1024], BF16)
        kd = sb("kd", [128, L * 2, 640], BF16)
        vt = sb("vt", [128, L * 5, 128], BF16)
        dw = sb("dw", [128, L, 1024], BF16)
        pv = sb("pv", [128, L * NPV], F32)
        omka = sb("omka", [128, L * 8], F32)
        nb = sb("nb", [128, L * 16], F32)
        esink = sb("esink", [128, L * 8], F32)
        ccf = sb("ccf", [128, NCC], F32)
        ccb = sb("ccb", [128, NCC], BF16)
        gs = sb("gs", [128, 64], F32)
        shp = sb("shp", [128, L * 25], F32)
        Hf = sb("Hf", [128, L * 8, 64], F32)
        Hb = sb("Hb", [128, L * 8, 64], BF16)
        ec = sb("ec", [128, 2, 8], F32)
        sm = sb("sm", [128, 256], BF16)
        pp = [st.enter_context(nc.psum_tensor(f"pp{i}", [128, 512], F32)) for i in range(4)]
        pq = [st.enter_context(nc.psum_tensor(f"pq{i}", [128, 512], F32)) for i in range(4)]
        PPK = [("pp", i) for i in range(4)]
        PQK = [("pq", i) for i in range(4)]

        onesF = ccf[:, CF_ONES:CF_ONES + 128]
        onesB = ccb[:, CB_ONES:CB_ONES + 128]
        blkB = ccb[:, CB_BLK:CB_BLK + 128]
        identB = ccb[:, CB_ID:CB_ID + 128]
        maskA = ccf[:, CF_MA:CF_MA + 128]
        maskL = ccf[:, CF_ML:CF_ML + 64]
        identI = ccf[:, CF_II:CF_II + 64]
        maskS = ccb[:, CB_MS:CB_MS + 256]
        maskS0 = ccb[:, CB_MS0:CB_MS0 + 256]
        scanm = ccf[:, CF_SCAN:CF_SCAN + 512]

        P.dma("sp", lambda e: e.dma_start(out=pv[:], in_=pvd), "pv", wr=["pv"])
        P.dma("sp", lambda e: e.dma_start(out=ccf[:], in_=ccdf), "ccf", wr=["ccf"])
        P.dma("pool", lambda e: e.dma_start(out=ccb[:], in_=ccdb), "ccb", wr=["ccb"])
        for l in range(L):
            P.dma("pool", lambda e, l=l: e.dma_start(out=dw[:, l, :], in_=dwd[l]), "dw", wr=["dw"])
        P.op("dve", lambda e: e.memset(shp[:], 0.0), wr=["shp"])
        P.op("dve", lambda e: e.memset(Hf[:], 0.0), wr=["Hf"])
        P.op("dve", lambda e: e.memset(Hb[:], 0.0), wr=["Hb"])
        P.op("dve", lambda e: e.memset(kd[:], 0.0), wr=["kd"])
        P.op("dve", lambda e: e.memset(vt[:], 0.0), wr=["vt"])
        for l in range(L):
            b = l * NPV
            P.op("dve", lambda e, l=l, b=b: e.tensor_scalar(out=omka[:, l * 8:(l + 1) * 8], in0=pv[:, b + PV_KA:b + PV_KA + 8],
                                                             scalar1=-1.0, scalar2=1.0, op0=ALU.mult, op1=ALU.add), rd=["pv"], wr=["omka"])
            P.op("act", lambda e, l=l, b=b: e.activation(out=esink[:, l * 8:(l + 1) * 8], in_=pv[:, b + PV_SINK:b + PV_SINK + 8], func=AF.Exp),
                 rd=["pv"], wr=["esink"])
            P.op("dve", lambda e, l=l, b=b: e.tensor_scalar(out=nb[:, l * 16:(l + 1) * 16], in0=pv[:, b + PV_DB:b + PV_DB + 16],
                                                             scalar1=-1.0, scalar2=None, op0=ALU.mult), rd=["pv"], wr=["nb"])

        wstate = {"slot": 0, "gid": None, "ti": 0}
        NG = 46
        wscr = nc.dram_tensor("wscr", [L * NG, 128, 8192], BF16, kind="Internal").ap()

        def load_w(src3, pieces, nk=16):
            s = wstate["slot"]
            wstate["slot"] = 1 - s
            gid = wstate["gid"]
            if gid is not None:
                wstate["gid"] = gid + 1
            if gid is not None and wstate["ti"] > 0:
                P.dma("sp", lambda e: e.dma_start(out=wbuf[s][:, 0:nk, :].rearrange("p a b -> p (a b)"), in_=wscr[gid][:, 0:nk * 512]),
                      f"w{s}", rd=[("scr", gid)], wr=[("w", s)])
                return s
            for (doff, c0, n) in pieces:
                P.dma("pool", lambda e, s=s, doff=doff, c0=c0, n=n: e.dma_start(
                    out=wbuf[s][:, 0:nk, doff:doff + n],
                    in_=src3[:, c0:c0 + n].rearrange("(kc p) c -> p kc c", p=128)), f"w{s}", wr=[("w", s)])
            if gid is not None and NT > 1:
                P.dma("sp", lambda e: e.dma_start(out=wscr[gid][:, 0:nk * 512], in_=wbuf[s][:, 0:nk, :].rearrange("p a b -> p (a b)")),
                      f"ws{s}", rd=[("w", s)], wr=[("scr", gid)])
            return s

        def proj(s, j, out_ps, out_key, rhs_fn, rhs_keys, nk=16, ncol=128):
            for kc in range(nk):
                P.op("pe", lambda e, kc=kc: e.matmul(out_ps, lhsT=wbuf[s][:, kc, j * 128:j * 128 + ncol], rhs=rhs_fn(kc),
                                                      start=(kc == 0), stop=(kc == nk - 1)),
                     rd=[("w", s)] + rhs_keys(kc), wr=[out_key])

        HK = [("h", i) for i in range(16)]

        def proj_h(s, j, bank):
            proj(s, j, pp[bank][:], ("pp", bank), lambda kc: hT[:, kc, :], lambda kc: [("h", kc)])

        def act_sigmoid(out, in_, rd, wrk, nbias=None, final_bias=None):
            kw = {"bias": nbias} if nbias is not None else {}
            P.op("act", lambda e: e.activation(out=out, in_=in_, func=AF.Exp, scale=-1.0, **kw), rd=rd, wr=wrk)
            P.op("act", lambda e: e.activation(out=out, in_=out, func=AF.Ln, bias=1.0), rd=wrk, wr=wrk)
            kw2 = {"bias": final_bias} if final_bias is not None else {}
            P.op("act", lambda e: e.activation(out=out, in_=out, func=AF.Exp, scale=-1.0, **kw2), rd=wrk, wr=wrk)

        def act_silu(out, in_ps, rd, wrk):
            act_sigmoid(out, in_ps, rd, wrk)
            P.op("dve", lambda e: e.tensor_tensor(out=out, in0=in_ps, in1=out, op=ALU.mult), rd=rd + wrk, wr=wrk)

        def act_rpow(out, in_, rd, wrk, p, scale=1.0, bias=None):
            kw = {"bias": bias} if bias is not None else {}
            P.op("act", lambda e: e.activation(out=out, in_=in_, func=AF.Ln, scale=scale, **kw), rd=rd, wr=wrk)
            P.op("act", lambda e: e.activation(out=out, in_=out, func=AF.Exp, scale=-p), rd=wrk, wr=wrk)

        def rms_stats(src_fn, src_keys, ncols, nchunks, out_rstd, out_key, tmpi):
            for dc in range(nchunks):
                bi = 27 + (dc % 2)
                t = bt[bi]
                P.op("act", lambda e, dc=dc, t=t: e.activation(out=t[:, 0:ncols], in_=src_fn(dc), func=AF.Square),
                     rd=src_keys(dc), wr=[("b", bi)])
                P.op("pe", lambda e, dc=dc, t=t: e.matmul(pq[0][:, 0:ncols], lhsT=onesB, rhs=t[:, 0:ncols],
                                                           start=(dc == 0), stop=(dc == nchunks - 1)),
                     rd=[("b", bi), "ccb"], wr=[("pq", 0)])
            act_rpow(out_rstd, pq[0][:, 0:ncols], [("pq", 0)], [out_key], 0.5, scale=1.0 / D, bias=1e-6)

        mT = x_res
        P.dma("sp", lambda e: e.dma_start(out=mT[:, :, 0:MEM], in_=memT.rearrange("(dc p) m -> p dc m", p=128)), "x0",
              wr=[("x", i) for i in range(16)])
        rstd_m = ft[2]
        rms_stats(lambda dc: mT[:, dc, 0:MEM], lambda dc: [("x", dc)], MEM, 16, rstd_m[:, 0:MEM], ("f", 2), 0)
        for l in range(L):
            b = l * NPV
            for dc in range(16):
                P.op("dve", lambda e, dc=dc, b=b: e.scalar_tensor_tensor(out=hT[:, dc, 0:MEM], in0=mT[:, dc, 0:MEM],
                                                                          scalar=pv[:, b + PV_GMEM + dc:b + PV_GMEM + dc + 1],
                                                                          in1=rstd_m[:, 0:MEM], op0=ALU.mult, op1=ALU.mult),
                     rd=[("x", dc), "pv", ("f", 2)], wr=[("h", dc)])
            for g in range(4):
                s = load_w(w_mkv[l], [(0, g * 512, 512)])
                if g < 2:
                    for j in range(4):
                        proj(s, j, pp[j][:, 0:MEM], ("pp", j), lambda kc: hT[:, kc, 0:MEM], lambda kc: [("h", kc)])
                        ci = l * 8 + g * 4 + j
                        P.op("act", lambda e, j=j, ci=ci: e.activation(out=kmT[:, ci, :], in_=pp[j][:, 0:MEM], func=AF.Copy),
                             rd=[("pp", j)], wr=["kmT"])
                else:
                    for mt in range(2):
                        for kc in range(16):
                            P.op("pe", lambda e, kc=kc, mt=mt, s=s: e.matmul(pp[mt][:], lhsT=hT[:, kc, mt * 128:(mt + 1) * 128],
                                                                                rhs=wbuf[s][:, kc, :], start=(kc == 0), stop=(kc == 15)),
                                 rd=[("w", s), ("h", kc)], wr=[("pp", mt)])
                        P.op("act", lambda e, mt=mt, l=l, g=g: e.activation(out=vm[:, l * 2 + mt, (g - 2) * 512:(g - 1) * 512], in_=pp[mt][:],
                                                                             func=AF.Copy), rd=[("pp", mt)], wr=["vm"])

        def block(ti, l):
            b = l * NPV
            wstate["gid"] = l * NG
            wstate["ti"] = ti
            pvc = lambda off, c: pv[:, b + off + c:b + off + c + 1]
            t0 = ti * TT
            if l == 0:
                for q in range(4):
                    P.dma("sp", lambda e, q=q: e.dma_start(out=x_res[:, q * 4:(q + 1) * 4, :],
                                                           in_=xT[q * 512:(q + 1) * 512, t0:t0 + TT].rearrange("(dc p) t -> p dc t", p=128)),
                          f"x{q}", wr=[("x", q * 4 + i) for i in range(4)])
            rstd = ft[2]
            rms_stats(lambda dc: x_res[:, dc, :], lambda dc: [("x", dc)], TT, 16, rstd[:, 0:TT], ("f", 2), 0)
            for dc in range(16):
                P.op("dve", lambda e, dc=dc: e.scalar_tensor_tensor(out=hT[:, dc, :], in0=x_res[:, dc, :], scalar=pvc(PV_GPRE, dc),
                                                                     in1=rstd[:, 0:TT], op0=ALU.mult, op1=ALU.mult),
                     rd=[("x", dc), "pv", ("f", 2)], wr=[("h", dc)])

            def shift(bank, fi_raw, fi_out, chunk_idx, rows=slice(0, 128)):
                raw = ft[fi_raw]
                si = l * 25 + chunk_idx
                P.op("act", lambda e: e.activation(out=raw[:, 1:513], in_=pp[bank][:], func=AF.Copy), rd=[("pp", bank)], wr=[("f", fi_raw)])
                P.op("dve", lambda e: e.tensor_copy(out=raw[:, 0:1], in_=shp[:, si:si + 1]), rd=["shp"], wr=[("f", fi_raw)])
                P.op("dve", lambda e: e.tensor_copy(out=shp[:, si:si + 1], in_=raw[:, 512:513]), rd=[("f", fi_raw)], wr=["shp"])
                P.op("dve", lambda e: e.tensor_tensor(out=ft[fi_out][:, 0:512], in0=raw[:, 0:512], in1=raw[:, 1:513], op=ALU.subtract),
                     rd=[("f", fi_raw)], wr=[("f", fi_out)])
                P.op("dve", lambda e: e.scalar_tensor_tensor(out=ft[fi_out][:, 0:512], in0=ft[fi_out][:, 0:512], scalar=pvc(PV_MU, chunk_idx),
                                                             in1=raw[:, 1:513], op0=ALU.mult, op1=ALU.add),
                     rd=[("f", fi_out), ("f", fi_raw), "pv"], wr=[("f", fi_out)])

            s = load_w(w_in[l], [(0, C_WD, 128)])
            proj_h(s, 0, 0)
            shift(0, 0, 1, 24)
            twd, adb = bt[7], bt[8]
            P.op("act", lambda e: e.activation(out=twd[0:64, :], in_=ft[1][0:64, 0:512], func=AF.Tanh), rd=[("f", 1)], wr=[("b", 7)])
            P.op("dve", lambda e: e.tensor_copy(out=adb[64:128, :], in_=ft[1][64:128, 0:512]), rd=[("f", 1)], wr=[("b", 8)])
            v3 = lambda ap: ap.rearrange("p (n t) -> p n t", t=64)
            r4 = lambda x_: x_.rearrange("p (n w t) -> p n w t", w=2, t=64)
            c3 = lambda x_: x_.rearrange("p (n c) -> p n c", c=128)
            BONS = [(ft[11], 11), (ft[13], 13)]
            SGS = [(ft[6], 6), (ft[14], 14)]
            KB4 = [r4(bt[1]), r4(bt[2])]
            KBK = [("b", 1), ("b", 2)]
            Vb, vbi = bt[3], 3

            def pset(par):
                b0 = 9 + par * 10
                d = {"VT": (bt[b0], b0), "KIT": (bt[b0 + 1], b0 + 1), "BIT": (bt[b0 + 2], b0 + 2),
                     "ATk": [(bt[b0 + 3], b0 + 3), (bt[b0 + 4], b0 + 4)], "ATb": [(bt[b0 + 5], b0 + 5), (bt[b0 + 6], b0 + 6)],
                     "Mt": (bt[b0 + 7], b0 + 7), "RA": [(bt[b0 + 8], b0 + 8), (bt[b0 + 9], b0 + 9)], "b0": b0}
                return d

            def stage1(hp, par):
                S = pset(par)
                RA4 = [r4(S["RA"][h][0]) for h in range(2)]
                RAK = [("b", S["RA"][h][1]) for h in range(2)]
                BON, boni = BONS[par]
                sg, sgi = SGS[par]
                ecp, eck = ec[:, par, :], ("ec", par)
                s = load_w(w_in[l], [(0, C_R + hp * 128, 128), (128, C_K + hp * 128, 128), (256, C_V + hp * 128, 128),
                                     (384, C_RG + hp * 128, 128)])
                for j in range(4):
                    for k4 in range(4):
                        for kc in range(k4 * 4, k4 * 4 + 4):
                            P.op("pe", lambda e, kc=kc, j=j: e.matmul(pp[j][:], lhsT=wbuf[s][:, kc, j * 128:(j + 1) * 128], rhs=hT[:, kc, :],
                                                                      start=(kc == 0), stop=(kc == 15)), rd=[("w", s), ("h", kc)], wr=[("pp", j)], c=0.29)
                        yield
                Rs, Ks, Vs = ft[3], ft[4], ft[5]
                shift(0, 0, 3, hp)
                yield
                shift(1, 1, 4, 8 + hp)
                yield
                shift(2, 0, 5, 16 + hp)
                yield
                act_silu(sg[:, 0:512], pp[3][:], [("pp", 3)], [("f", sgi)])
                P.op("pe", lambda e: e.matmul(pp[0][:], lhsT=dw[0:64, l, hp * 128:(hp + 1) * 128], rhs=twd[0:64, :], start=True, stop=True),
                     rd=["dw", ("b", 7)], wr=[("pp", 0)])
                P.op("pe", lambda e: e.matmul(pp[1][:], lhsT=dw[64:128, l, hp * 128:(hp + 1) * 128], rhs=adb[64:128, :], start=True, stop=True),
                     rd=["dw", ("b", 8)], wr=[("pp", 1)])
                yield
                LW, A = ft[7], ft[8]
                act_sigmoid(LW[:, 0:512], pp[0][:], [("pp", 0), "nb"], [("f", 7)], nbias=nb[:, l * 16 + hp:l * 16 + hp + 1], final_bias=-0.5)
                yield
                act_sigmoid(A[:, 0:512], pp[1][:], [("pp", 1), "nb"], [("f", 8)], nbias=nb[:, l * 16 + 8 + hp:l * 16 + 8 + hp + 1])
                yield
                KK, TMP = ft[9], ft[0]
                P.op("act", lambda e: e.activation(out=KK[:, 0:512], in_=Ks[:, 0:512], func=AF.Copy, scale=pvc(PV_KK, hp)),
                     rd=[("f", 4), "pv"], wr=[("f", 9)])
                P.op("dve", lambda e: e.tensor_tensor(out=bt[6][:], in0=KK[:, 0:512], in1=KK[:, 0:512], op=ALU.mult), rd=[("f", 9)], wr=[("b", 6)])
                P.op("pe", lambda e: e.matmul(pp[2][:], lhsT=blkB, rhs=bt[6][:], start=True, stop=True), rd=["ccb", ("b", 6)], wr=[("pp", 2)])
                yield
                K2 = ft[10]
                P.op("act", lambda e: e.activation(out=K2[:, 0:512], in_=A[:, 0:512], func=AF.Identity, scale=pvc(PV_KA, hp),
                                                   bias=omka[:, l * 8 + hp:l * 8 + hp + 1]), rd=[("f", 8), "pv", "omka"], wr=[("f", 10)])
                P.op("dve", lambda e: e.tensor_scalar(out=TMP[:, 0:512], in0=pp[2][:], scalar1=1e-24, scalar2=None, op0=ALU.max),
                     rd=[("pp", 2)], wr=[("f", 0)])
                yield
                act_rpow(TMP[:, 0:512], TMP[:, 0:512], [("f", 0)], [("f", 0)], 0.5)
                yield
                P.op("dve", lambda e: e.tensor_tensor(out=KK[:, 0:512], in0=KK[:, 0:512], in1=TMP[:, 0:512], op=ALU.mult),
                     rd=[("f", 9), ("f", 0)], wr=[("f", 9)])
                yield
                P.op("dve", lambda e: e.tensor_tensor(out=K2[:, 0:512], in0=K2[:, 0:512], in1=Ks[:, 0:512], op=ALU.mult),
                     rd=[("f", 10), ("f", 4)], wr=[("f", 10)])
                yield
                P.op("dve", lambda e: e.scalar_tensor_tensor(out=bt[6][:], in0=Rs[:, 0:512], scalar=pvc(PV_RK, hp), in1=K2[:, 0:512],
                                                             op0=ALU.mult, op1=ALU.mult), rd=[("f", 3), ("f", 10), "pv"], wr=[("b", 6)])
                P.op("pe", lambda e: e.matmul(pp[3][:], lhsT=blkB, rhs=bt[6][:], start=True, stop=True), rd=["ccb", ("b", 6)], wr=[("pp", 3)])
                yield
                CUM = ft[1]
                P.op("dve", lambda e: e.tensor_tensor_scan(out=CUM[:, 0:512], data0=scanm, data1=LW[:, 0:512], initial=0.0,
                                                           op0=ALU.mult, op1=ALU.subtract), rd=["ccf", ("f", 7)], wr=[("f", 1)])
                yield
                P.op("dve", lambda e: e.tensor_tensor(out=BON[:, 0:512], in0=pp[3][:], in1=Vs[:, 0:512], op=ALU.mult),
                     rd=[("pp", 3), ("f", 5)], wr=[("f", boni)])
                EP, EM = ft[12], ft[0]
                P.op("act", lambda e: e.activation(out=EP[:, 0:512], in_=CUM[:, 0:512], func=AF.Exp), rd=[("f", 1)], wr=[("f", 12)])
                yield
                P.op("dve", lambda e: e.tensor_tensor(out=EM[:, 0:512], in0=CUM[:, 0:512], in1=LW[:, 0:512], op=ALU.add),
                     rd=[("f", 1), ("f", 7)], wr=[("f", 0)])
                P.op("act", lambda e: e.activation(out=EM[:, 0:512], in_=EM[:, 0:512], func=AF.Exp), rd=[("f", 0)], wr=[("f", 0)])
                yield
                P.op("dve", lambda e: e.tensor_copy(out=ecp, in_=v3(EP[:, 0:512])[:, :, 63]), rd=[("f", 12)], wr=[eck])
                for h in range(2):
                    P.op("dve", lambda e, h=h: e.tensor_tensor(out=RA4[h][:, :, 0, :], in0=v3(Rs[:, h * 256:(h + 1) * 256]),
                                                               in1=v3(EP[:, h * 256:(h + 1) * 256]), op=ALU.mult),
                         rd=[("f", 3), ("f", 12)], wr=[RAK[h]])
                    yield
                for h in range(2):
                    P.op("dve", lambda e, h=h: e.scalar_tensor_tensor(out=RA4[h][:, :, 1, :], in0=v3(KK[:, h * 256:(h + 1) * 256]), scalar=-1.0,
                                                                      in1=v3(EM[:, h * 256:(h + 1) * 256]), op0=ALU.mult, op1=ALU.mult),
                         rd=[("f", 9), ("f", 0)], wr=[RAK[h]])
                    yield
                EN = ft[12]
                P.op("act", lambda e: e.activation(out=EN[:, 0:512], in_=CUM[:, 0:512], func=AF.Exp, scale=-1.0), rd=[("f", 1)], wr=[("f", 12)])
                BP = ft[7]
                P.op("dve", lambda e: e.tensor_tensor(out=BP[:, 0:512], in0=KK[:, 0:512], in1=A[:, 0:512], op=ALU.mult),
                     rd=[("f", 9), ("f", 8)], wr=[("f", 7)])
                yield
                for h in range(2):
                    sl = slice(h * 256, (h + 1) * 256)
                    P.op("dve", lambda e, h=h, sl=sl: e.tensor_tensor(out=KB4[h][:, :, 0, :], in0=v3(K2[:, sl]), in1=v3(EN[:, sl]), op=ALU.mult),
                         rd=[("f", 10), ("f", 12)], wr=[KBK[h]])
                    yield
                    P.op("dve", lambda e, h=h, sl=sl: e.tensor_tensor(out=KB4[h][:, :, 1, :], in0=v3(BP[:, sl]), in1=v3(EN[:, sl]), op=ALU.mult),
                         rd=[("f", 7), ("f", 12)], wr=[KBK[h]])
                    yield
                P.op("act", lambda e: e.activation(out=Vb[:], in_=Vs[:, 0:512], func=AF.Copy), rd=[("f", 5)], wr=[("b", vbi)])
                yield
                ppb = [pp[i][:].bitcast(BF16) for i in range(4)]
                for (nm, bank, srcfn, skeys, eng) in (
                        ("VT", 0, lambda n, hs: Vb[hs, n * 64:(n + 1) * 64], lambda n: [("b", vbi)], "act"),
                        ("KIT", 1, lambda n, hs: KB4[n // 4][hs, n % 4, 0, :], lambda n: [KBK[n // 4]], "dve"),
                        ("BIT", 2, lambda n, hs: KB4[n // 4][hs, n % 4, 1, :], lambda n: [KBK[n // 4]], "act")):
                    dst, di = S[nm]
                    for n in range(8):
                        for h2 in range(2):
                            hs = slice(h2 * 64, (h2 + 1) * 64)
                            P.op("pe", lambda e, n=n, hs=hs, bank=bank, srcfn=srcfn: e.transpose(
                                out=ppb[bank][hs, n * 64:(n + 1) * 64], in_=srcfn(n, hs), identity=identB[hs, hs]),
                                rd=skeys(n) + ["ccb"], wr=[("pp", bank)])
                    if eng == "act":
                        P.op("act", lambda e, dst=dst, bank=bank: e.activation(out=dst[:], in_=ppb[bank][:, 0:512], func=AF.Copy),
                             rd=[("pp", bank)], wr=[("b", di)])
                    else:
                        P.op("dve", lambda e, dst=dst, bank=bank: e.tensor_copy(out=dst[:], in_=ppb[bank][:, 0:512]),
                             rd=[("pp", bank)], wr=[("b", di)])
                    yield
                X0, Xt0 = bt[4], bt[5]
                mA = maskA.unsqueeze(1).broadcast_to([128, 4, 128])
                mL = maskL.unsqueeze(1).broadcast_to([128, 4, 64])
                for h in range(2):
                    ATk, atki = S["ATk"][h]
                    ATb, atbi = S["ATb"][h]
                    for n4 in range(4):
                        for h2 in range(2):
                            hs = slice(h2 * 64, (h2 + 1) * 64)
                            P.op("pe", lambda e, h=h, n4=n4, hs=hs: e.matmul(pp[0][hs, n4 * 128:(n4 + 1) * 128], lhsT=KB4[h][hs, n4, 0, :],
                                                                             rhs=RA4[h][hs, n4, :, :], start=True, stop=True),
                                 rd=[KBK[h], RAK[h]], wr=[("pp", 0)])
                            P.op("pe", lambda e, h=h, n4=n4, hs=hs: e.matmul(pp[1][hs, n4 * 128:(n4 + 1) * 128], lhsT=KB4[h][hs, n4, 1, :],
                                                                             rhs=RA4[h][hs, n4, :, :], start=True, stop=True),
                                 rd=[KBK[h], RAK[h]], wr=[("pp", 1)])
                            P.op("pe", lambda e, h=h, n4=n4, hs=hs: e.matmul(pp[2][hs, n4 * 64:(n4 + 1) * 64], lhsT=RA4[h][hs, n4, 1, :],
                                                                             rhs=KB4[h][hs, n4, 1, :], start=True, stop=True),
                                 rd=[KBK[h], RAK[h]], wr=[("pp", 2)])
                    yield
                    P.op("dve", lambda e, ATk=ATk: e.tensor_tensor(out=c3(ATk[:]), in0=c3(pp[0][:]), in1=mA, op=ALU.mult),
                         rd=[("pp", 0), "ccf"], wr=[("b", atki)])
                    P.op("dve", lambda e, ATb=ATb: e.tensor_tensor(out=c3(ATb[:]), in0=c3(pp[1][:]), in1=mA, op=ALU.mult),
                         rd=[("pp", 1), "ccf"], wr=[("b", atbi)])
                    P.op("dve", lambda e, h=h: e.tensor_tensor(out=X0[:, h * 256:(h + 1) * 256].rearrange("p (n c) -> p n c", c=64),
                                                               in0=pp[2][:, 0:256].rearrange("p (n c) -> p n c", c=64), in1=mL, op=ALU.mult),
                         rd=[("pp", 2), "ccf"], wr=[("b", 4)])
                    P.op("act", lambda e, h=h, ATb=ATb: e.activation(out=Xt0[:, h * 256:(h + 1) * 256].rearrange("p (n c) -> p n c", c=64),
                                                                     in_=c3(ATb[:])[:, :, 64:128], func=AF.Copy), rd=[("b", atbi)], wr=[("b", 5)])
                    yield
                Mt, mti = S["Mt"]
                iI = identI.unsqueeze(1).broadcast_to([128, 8, 64])
                P.op("dve", lambda e: e.tensor_tensor(out=v3(Mt[:]), in0=v3(Xt0[:]), in1=iI, op=ALU.add), rd=[("b", 5), "ccf"], wr=[("b", mti)])
                yield
                pingX, pingXt = [(bt[4], 4), (bt[0], 0)], [(bt[5], 5), (Vb, vbi)]

                def sq(Xc, Xtc, xi, xti):
                    for n in range(8):
                        for h2 in range(2):
                            hs = slice(h2 * 64, (h2 + 1) * 64)
                            cs = slice(n * 64, (n + 1) * 64)
                            P.op("pe", lambda e, hs=hs, cs=cs: e.matmul(pp[0][hs, cs], lhsT=Xc[hs, cs], rhs=Xtc[hs, cs], start=True, stop=True),
                                 rd=[("b", xi), ("b", xti)], wr=[("pp", 0)])
                            P.op("pe", lambda e, hs=hs, cs=cs: e.matmul(pp[1][hs, cs], lhsT=Xtc[hs, cs], rhs=Xc[hs, cs], start=True, stop=True),
                                 rd=[("b", xi), ("b", xti)], wr=[("pp", 1)])

                def ev(lvl):
                    Xn, xni = pingX[lvl % 2]
                    Xtn, xtni = pingXt[lvl % 2]
                    P.op("act", lambda e: e.activation(out=Xtn[:], in_=pp[0][:], func=AF.Copy), rd=[("pp", 0)], wr=[("b", xtni)])
                    P.op("dve", lambda e: e.tensor_copy(out=Xn[:], in_=pp[1][:]), rd=[("pp", 1)], wr=[("b", xni)])
                    return Xn, Xtn, xni, xtni

                def mtmm(Xn, xni):
                    for n in range(8):
                        for h2 in range(2):
                            hs = slice(h2 * 64, (h2 + 1) * 64)
                            cs = slice(n * 64, (n + 1) * 64)
                            P.op("pe", lambda e, hs=hs, cs=cs: e.matmul(pp[2][hs, cs], lhsT=Xn[hs, cs], rhs=Mt[hs, cs], start=True, stop=True),
                                 rd=[("b", xni), ("b", mti)], wr=[("pp", 2)])

                def madd():
                    P.op("dve", lambda e: e.tensor_tensor(out=Mt[:], in0=pp[2][:], in1=Mt[:], op=ALU.add), rd=[("pp", 2), ("b", mti)], wr=[("b", mti)])

                sq(X0, Xt0, 4, 5)
                yield
                Xc, Xtc, xi, xti = ev(1)
                yield
                for lvl in range(2, 6):
                    sq(Xc, Xtc, xi, xti)
                    yield
                    mtmm(Xc, xi)
                    yield
                    Xc, Xtc, xi, xti = ev(lvl)
                    yield
                    madd()
                    yield
                mtmm(Xc, xi)
                yield
                madd()
                yield

            def stage2(hp, par):
                S = pset(par)
                RA4 = [r4(S["RA"][h][0]) for h in range(2)]
                RAK = [("b", S["RA"][h][1]) for h in range(2)]
                ATk3 = [c3(S["ATk"][h][0][:]) for h in range(2)]
                ATb3 = [c3(S["ATb"][h][0][:]) for h in range(2)]
                ATKK = [("b", S["ATk"][h][1]) for h in range(2)]
                ATBK = [("b", S["ATb"][h][1]) for h in range(2)]
                VT, vti = S["VT"]
                KIT, kiti = S["KIT"]
                BIT, biti = S["BIT"]
                Mt, mti = S["Mt"]
                BON, boni = BONS[par]
                sg, sgi = SGS[par]
                ecp, eck = ec[:, par, :], ("ec", par)
                hh = l * 8 + hp
                R0b, Ub = sm[:, 0:64], sm[:, 64:128]
                for n in range(8):
                    h, n4 = n // 4, n % 4
                    cs = slice(n * 64, (n + 1) * 64)
                    P.op("act", lambda e, n=n: e.activation(out=gs[:, 0:64], in_=Hf[:, hh, :], func=AF.Copy, scale=ecp[:, n:n + 1]),
                         rd=["Hf", eck], wr=["gs"], c=0.25)
                    for h2 in range(2):
                        hs = slice(h2 * 64, (h2 + 1) * 64)
                        P.op("pe", lambda e, hs=hs, h=h, n4=n4: e.matmul(pq[3][hs, 0:64], lhsT=RA4[h][hs, n4, 1, :], rhs=Hb[hs, hh, :], start=True, stop=False),
                             rd=[RAK[h], "Hb"], wr=[("pq", 3)])
                        P.op("pe", lambda e, hs=hs, h=h, n4=n4, cs=cs: e.matmul(pq[3][hs, 0:64], lhsT=ATk3[h][hs, n4, 64:128], rhs=VT[hs, cs], start=False, stop=True),
                             rd=[ATKK[h], ("b", vti)], wr=[("pq", 3)])
                    yield
                    P.op("act", lambda e: e.activation(out=R0b, in_=pq[3][:, 0:64], func=AF.Copy), rd=[("pq", 3)], wr=["sm0"], c=0.25)
                    for h2 in range(2):
                        hs = slice(h2 * 64, (h2 + 1) * 64)
                        P.op("pe", lambda e, hs=hs, cs=cs: e.matmul(pq[3][hs, 64:128], lhsT=Mt[hs, cs], rhs=sm[hs, 0:64], start=True, stop=True),
                             rd=[("b", mti), "sm0"], wr=[("pq", 3)])
                    yield
                    P.op("dve", lambda e: e.tensor_copy(out=Ub, in_=pq[3][:, 64:128]), rd=[("pq", 3)], wr=["sm1"], c=0.25)
                    for h2 in range(2):
                        hs = slice(h2 * 64, (h2 + 1) * 64)
                        P.op("pe", lambda e, hs=hs, cs=cs: e.matmul(pq[3][hs, 128:192], lhsT=KIT[hs, cs], rhs=VT[hs, cs], start=True, stop=False),
                             rd=[("b", kiti), ("b", vti)], wr=[("pq", 3)])
                        P.op("pe", lambda e, hs=hs, cs=cs: e.matmul(pq[3][hs, 128:192], lhsT=BIT[hs, cs], rhs=sm[hs, 64:128], start=False, stop=True),
                             rd=[("b", biti), "sm1"], wr=[("pq", 3)])
                    for h2 in range(2):
                        hs = slice(h2 * 64, (h2 + 1) * 64)
                        P.op("pe", lambda e, hs=hs, h=h, n4=n4, cs=cs: e.matmul(pq[0][hs, cs], lhsT=Hb[hs, hh, :], rhs=RA4[h][hs, n4, 0, :], start=True, stop=False),
                             rd=["Hb", RAK[h]], wr=[("pq", 0)])
                        P.op("pe", lambda e, hs=hs, h=h, n4=n4, cs=cs: e.matmul(pq[0][hs, cs], lhsT=sm[hs, 64:128], rhs=ATb3[h][hs, n4, 0:64], start=False, stop=False),
                             rd=["sm1", ATBK[h]], wr=[("pq", 0)])
                        P.op("pe", lambda e, hs=hs, h=h, n4=n4, cs=cs: e.matmul(pq[0][hs, cs], lhsT=VT[hs, cs], rhs=ATk3[h][hs, n4, 0:64], start=False, stop=True),
                             rd=[("b", vti), ATKK[h]], wr=[("pq", 0)])
                    yield
                    P.op("dve", lambda e, n=n: e.scalar_tensor_tensor(out=Hb[:, hh, :], in0=pq[3][:, 128:192], scalar=ecp[:, n:n + 1], in1=gs[:, 0:64],
                                                                      op0=ALU.mult, op1=ALU.add), rd=[("pq", 3), eck, "gs"], wr=["Hb"], c=0.25)
                    P.op("dve", lambda e, n=n: e.scalar_tensor_tensor(out=Hf[:, hh, :], in0=pq[3][:, 128:192], scalar=ecp[:, n:n + 1], in1=gs[:, 0:64],
                                                                      op0=ALU.mult, op1=ALU.add), rd=[("pq", 3), eck, "gs"], wr=["Hf"], c=0.25)
                    yield
                b0 = S["b0"]
                Yf, MEAN, VAR = f32v(b0), f32v(b0 + 3), f32v(b0 + 5)
                YFK, MK, VK = [("b", b0), ("b", b0 + 1)], [("b", b0 + 3), ("b", b0 + 4)], [("b", b0 + 5), ("b", b0 + 6)]
                Yb, ybi, Y2, y2i = bt[b0 + 2], b0 + 2, bt[b0 + 7], b0 + 7
                P.op("act", lambda e: e.activation(out=Yf, in_=pq[0][:], func=AF.Copy), rd=[("pq", 0)], wr=YFK)
                yield
                P.op("dve", lambda e: e.tensor_copy(out=Yb[:], in_=Yf), rd=YFK, wr=[("b", ybi)])
                P.op("act", lambda e: e.activation(out=Y2[:], in_=Yf, func=AF.Square), rd=YFK, wr=[("b", y2i)])
                P.op("pe", lambda e: e.matmul(pq[1][:], lhsT=blkB, rhs=Yb[:], start=True, stop=True), rd=["ccb", ("b", ybi)], wr=[("pq", 1)])
                P.op("pe", lambda e: e.matmul(pq[2][:], lhsT=blkB, rhs=Y2[:], start=True, stop=True), rd=["ccb", ("b", y2i)], wr=[("pq", 2)])
                yield
                P.op("act", lambda e: e.activation(out=MEAN, in_=pq[1][:], func=AF.Copy, scale=1.0 / 64), rd=[("pq", 1)], wr=MK)
                yield
                P.op("dve", lambda e: e.tensor_tensor(out=VAR, in0=MEAN, in1=MEAN, op=ALU.mult), rd=MK, wr=VK)
                yield
                P.op("dve", lambda e: e.scalar_tensor_tensor(out=VAR, in0=pq[2][:], scalar=1.0 / 64, in1=VAR,
                                                             op0=ALU.mult, op1=ALU.subtract), rd=[("pq", 2)] + VK, wr=VK)
                yield
                act_rpow(VAR, VAR, VK, VK, 0.5, bias=64e-5)
                P.op("dve", lambda e: e.tensor_tensor(out=Yf, in0=Yf, in1=MEAN, op=ALU.subtract), rd=YFK + MK, wr=YFK)
                yield
                P.op("dve", lambda e: e.tensor_tensor(out=Yf, in0=Yf, in1=VAR, op=ALU.mult), rd=YFK + VK, wr=YFK)
                yield
                P.op("act", lambda e: e.activation(out=Yf, in_=Yf, func=AF.Identity, scale=pvc(PV_GW, hp), bias=pvc(PV_GB, hp)),
                     rd=YFK + ["pv"], wr=YFK)
                yield
                P.op("dve", lambda e: e.tensor_tensor(out=Yf, in0=Yf, in1=BON[:, 0:512], op=ALU.add), rd=YFK + [("f", boni)], wr=YFK)
                yield
                P.op("dve", lambda e: e.tensor_tensor(out=ybr[:, hp, :], in0=Yf, in1=sg[:, 0:512], op=ALU.mult),
                     rd=YFK + [("f", sgi)], wr=[("y", hp)])
                yield

            def drive(gens):
                live = {i: g for i, g in enumerate(gens) if g is not None}
                for i in live:
                    P.sclock[("s", ti, l, id(live[i]))] = max(P.efree.values()) if False else min(P.efree.values())
                keyof = {i: ("s", ti, l, id(live[i])) for i in live}
                while live:
                    i = min(live, key=lambda k: P.sclock[keyof[k]])
                    P.stream = keyof[i]
                    try:
                        next(live[i])
                    except StopIteration:
                        del live[i]
                P.stream = None

            drive([stage1(0, 0)])
            for hp in range(8):
                drive([stage2(hp, hp % 2), stage1(hp + 1, (hp + 1) % 2) if hp < 7 else None])

            s = load_w(w_in[l], [(0, C_SK, 64), (64, C_SK, 64), (128, C_SK + 64, 64), (192, C_SK + 64, 64), (256, C_SV, 128)])
            for g in range(2):
                proj_h(s, g, g)
                P.op("act", lambda e, g=g: e.activation(out=kd[:, l * 2 + g, 128:640], in_=pp[g][:], func=AF.Copy), rd=[("pp", g)], wr=[("kd", g)])
            for blk in range(4):
                for kc in range(16):
                    P.op("pe", lambda e, kc=kc, blk=blk, s=s: e.matmul(pp[2][:, blk * 128:(blk + 1) * 128], lhsT=hT[:, kc, blk * 128:(blk + 1) * 128],
                                                                       rhs=wbuf[s][:, kc, 256:384], start=(kc == 0), stop=(kc == 15)),
                         rd=[("w", s), ("h", kc)], wr=[("pp", 2)])
            P.op("act", lambda e: e.activation(out=vt[:, l * 5 + 1:l * 5 + 5, :], in_=pp[2][:].rearrange("p (a b) -> p a b", b=128), func=AF.Copy),
                 rd=[("pp", 2)], wr=["vt"])
            for cp in range(4):
                c0 = 2 * cp
                s = load_w(w_in[l], [(0, C_SQ + c0 * 128, 128), (128, C_SG + c0 * 128, 128), (256, C_SQ + (c0 + 1) * 128, 128),
                                     (384, C_SG + (c0 + 1) * 128, 128)])
                for j in range(4):
                    proj_h(s, j, j)
                for ci in range(2):
                    c = c0 + ci
                    g = c // 4
                    qTb, sgs = bt[0], ft[3]
                    P.op("act", lambda e, ci=ci: e.activation(out=qTb[:], in_=pp[2 * ci][:], func=AF.Copy), rd=[("pp", 2 * ci)], wr=[("b", 0)])
                    act_silu(sgs[:, 0:512], pp[2 * ci + 1][:], [("pp", 2 * ci + 1)], [("f", 3)])
                    for blk in range(4):
                        bs = slice(blk * 128, (blk + 1) * 128)
                        ex, exm = bt[1], bt[2]
                        for h2 in range(2):
                            hs = slice(h2 * 64, (h2 + 1) * 64)
                            for w_ in range(2):
                                P.op("pe", lambda e, hs=hs, h2=h2, w_=w_, blk=blk, bs=bs, g=g: e.matmul(
                                    pq[h2][:, w_ * 128:(w_ + 1) * 128], lhsT=kd[hs, l * 2 + g, (blk + w_) * 128:(blk + w_ + 1) * 128],
                                    rhs=qTb[hs, bs], start=True, stop=True), rd=[("kd", g), ("b", 0)], wr=[("pq", h2)])
                            P.op("act", lambda e, h2=h2: e.activation(out=ex[:, h2 * 256:(h2 + 1) * 256], in_=pq[h2][:, 0:256], func=AF.Exp, scale=0.125),
                                 rd=[("pq", h2)], wr=[("b", 1)])
                        mk = (maskS0 if (ti == 0 and blk == 0) else maskS).unsqueeze(1).broadcast_to([128, 2, 256])
                        P.op("dve", lambda e, mk=mk: e.tensor_tensor(out=exm[:].rearrange("p (a b) -> p a b", b=256),
                                                                   in0=ex[:].rearrange("p (a b) -> p a b", b=256), in1=mk, op=ALU.mult),
                             rd=[("b", 1), "ccb"], wr=[("b", 2)])
                        for h2 in range(2):
                            hs = slice(h2 * 64, (h2 + 1) * 64)
                            for w_ in range(2):
                                P.op("pe", lambda e, hs=hs, h2=h2, w_=w_, blk=blk, bs=bs, g=g: e.matmul(
                                    pq[2][hs, bs], lhsT=vt[:, l * 5 + blk + w_, g * 64:(g + 1) * 64],
                                    rhs=exm[:, h2 * 256 + w_ * 128:h2 * 256 + (w_ + 1) * 128], start=(w_ == 0), stop=(w_ == 1)),
                                    rd=["vt", ("b", 2)], wr=[("pq", 2)])
                            for w_ in range(2):
                                P.op("pe", lambda e, hs=hs, h2=h2, w_=w_, bs=bs: e.matmul(
                                    pq[3][hs, bs], lhsT=onesB[:, 0:64], rhs=exm[:, h2 * 256 + w_ * 128:h2 * 256 + (w_ + 1) * 128],
                                    start=(w_ == 0), stop=(w_ == 1)), rd=["ccb", ("b", 2)], wr=[("pq", 3)])
                    DEN = ft[4]
                    act_rpow(DEN[:, 0:512], pq[3][:], [("pq", 3), "esink"], [("f", 4)], 1.0, bias=esink[:, l * 8 + c:l * 8 + c + 1])
                    P.op("dve", lambda e: e.tensor_tensor(out=DEN[:, 0:512], in0=pq[2][:], in1=DEN[:, 0:512], op=ALU.mult),
                         rd=[("pq", 2), ("f", 4)], wr=[("f", 4)])
                    P.op("dve", lambda e, c=c: e.tensor_tensor(out=ybr[:, 8 + c, :], in0=DEN[:, 0:512], in1=sgs[:, 0:512], op=ALU.mult),
                         rd=[("f", 4), ("f", 3)], wr=[("y", 8 + c)])
            for g in range(2):
                P.op("dve", lambda e, g=g: e.tensor_copy(out=kd[:, l * 2 + g, 0:128], in_=kd[:, l * 2 + g, 512:640]), rd=[("kd", g)], wr=[("kd", g)])
            P.op("dve", lambda e: e.tensor_copy(out=vt[:, l * 5, :], in_=vt[:, l * 5 + 4, :]), rd=["vt"], wr=["vt"])

            for h in range(4):
                s = load_w(w_in[l], [(0, C_XQ + h * 256, 256), (256, C_XG + h * 256, 256)])
                for j in range(4):
                    proj_h(s, j, j)
                qx, sgx = [bt[0], bt[1]], [ft[3], ft[4]]
                for j in range(2):
                    P.op("act", lambda e, j=j: e.activation(out=qx[j][:], in_=pp[j][:], func=AF.Copy), rd=[("pp", j)], wr=[("b", j)])
                    act_silu(sgx[j][:, 0:512], pp[2 + j][:], [("pp", 2 + j)], [("f", 3 + j)])
                exx = [bt[2], bt[3]]
                for mt in range(2):
                    for j in range(2):
                        P.op("pe", lambda e, mt=mt, j=j, h=h: e.matmul(pq[mt][:], lhsT=kmT[:, l * 8 + h * 2 + j, mt * 128:(mt + 1) * 128], rhs=qx[j][:],
                                                                       start=(j == 0), stop=(j == 1)), rd=["kmT", ("b", j)], wr=[("pq", mt)])
                    P.op("act", lambda e, mt=mt: e.activation(out=exx[mt][:], in_=pq[mt][:], func=AF.Exp, scale=1.0 / 16), rd=[("pq", mt)], wr=[("b", 2 + mt)])
                for j in range(2):
                    for mt in range(2):
                        P.op("pe", lambda e, mt=mt, j=j, h=h: e.matmul(pp[j][:], lhsT=vm[:, l * 2 + mt, h * 256 + j * 128:h * 256 + (j + 1) * 128],
                                                                       rhs=exx[mt][:], start=(mt == 0), stop=(mt == 1)),
                             rd=["vm", ("b", 2 + mt)], wr=[("pp", j)])
                for mt in range(2):
                    P.op("pe", lambda e, mt=mt: e.matmul(pq[2][:], lhsT=onesB, rhs=exx[mt][:], start=(mt == 0), stop=(mt == 1)),
                         rd=["ccb", ("b", 2 + mt)], wr=[("pq", 2)])
                REC = ft[5]
                act_rpow(REC[:, 0:512], pq[2][:], [("pq", 2)], [("f", 5)], 1.0)
                for j in range(2):
                    P.op("dve", lambda e, j=j: e.tensor_tensor(out=sgx[j][:, 0:512], in0=sgx[j][:, 0:512], in1=REC[:, 0:512], op=ALU.mult),
                         rd=[("f", 3 + j), ("f", 5)], wr=[("f", 3 + j)])
                    P.op("dve", lambda e, j=j, h=h: e.tensor_tensor(out=ybr[:, 16 + h * 2 + j, :], in0=pp[j][:], in1=sgx[j][:, 0:512], op=ALU.mult),
                         rd=[("pp", j), ("f", 3 + j)], wr=[("y", 16 + h * 2 + j)])

            for dg in range(4):
                for br in range(3):
                    s = load_w(w_in[l], [(0, C_MG + br * 2048 + dg * 512, 512)])
                    for j in range(4):
                        proj_h(s, j, j)
                        act_sigmoid(ft[3 + j][:, 0:512], pp[j][:], [("pp", j)], [("f", 3 + j)])
                    s = load_w(w_up[br][l], [(0, dg * 512, 512)], nk=8)
                    for j in range(4):
                        proj(s, j, pq[j][:], ("pq", j), lambda kc, br=br: ybr[:, br * 8 + kc, :], lambda kc, br=br: [("y", br * 8 + kc)], nk=8)
                        acc = ft[7 + j]
                        if br == 0:
                            P.op("dve", lambda e, j=j, acc=acc: e.tensor_tensor(out=acc[:, 0:512], in0=pq[j][:], in1=ft[3 + j][:, 0:512], op=ALU.mult),
                                 rd=[("pq", j), ("f", 3 + j)], wr=[("f", 7 + j)])
                        else:
                            P.op("dve", lambda e, j=j: e.tensor_tensor(out=ft[3 + j][:, 0:512], in0=pq[j][:], in1=ft[3 + j][:, 0:512], op=ALU.mult),
                                 rd=[("pq", j), ("f", 3 + j)], wr=[("f", 3 + j)])
                            if br == 1:
                                P.op("dve", lambda e, j=j, acc=acc: e.tensor_tensor(out=acc[:, 0:512], in0=acc[:, 0:512], in1=ft[3 + j][:, 0:512], op=ALU.add),
                                     rd=[("f", 7 + j), ("f", 3 + j)], wr=[("f", 7 + j)])
                            else:
                                dc = dg * 4 + j
                                P.op("dve", lambda e, j=j, acc=acc, dc=dc: e.tensor_tensor(out=bt[dc][:], in0=acc[:, 0:512], in1=ft[3 + j][:, 0:512], op=ALU.add),
                                     rd=[("f", 7 + j), ("f", 3 + j)], wr=[("b", dc)])
            for dg in range(4):
                s = load_w(w_out[l], [(0, dg * 512, 512)])
                for j in range(4):
                    dc = dg * 4 + j
                    proj(s, j, pp[j][:], ("pp", j), lambda kc: bt[kc][:], lambda kc: [("b", kc)])
                    P.op("act", lambda e, j=j, dc=dc: e.activation(out=o_f[:, dc, :], in_=pp[j][:], func=AF.Copy), rd=[("pp", j)], wr=okeys(dc))
                    bi = 27 + (dc % 2)
                    t = bt[bi]
                    P.op("act", lambda e, dc=dc, t=t: e.activation(out=t[:], in_=o_f[:, dc, :], func=AF.Square), rd=okeys(dc), wr=[("b", bi)])
                    P.op("pe", lambda e, dc=dc, t=t: e.matmul(pq[0][:], lhsT=onesB, rhs=t[:], start=(dc == 0), stop=(dc == 15)),
                         rd=[("b", bi), "ccb"], wr=[("pq", 0)])
            rs2 = ft[2]
            act_rpow(rs2[:, 0:512], pq[0][:], [("pq", 0)], [("f", 2)], 0.5, scale=1.0 / D, bias=1e-6)
            for dc in range(16):
                t = ft[3 + (dc % 2)]
                P.op("dve", lambda e, dc=dc, t=t: e.scalar_tensor_tensor(out=t[:, 0:512], in0=o_f[:, dc, :], scalar=pvc(PV_GPOST, dc), in1=rs2[:, 0:512],
                                                                         op0=ALU.mult, op1=ALU.mult), rd=okeys(dc) + ["pv", ("f", 2)], wr=[("f", 3 + dc % 2)])
                P.op("dve", lambda e, dc=dc, t=t: e.tensor_tensor(out=x_res[:, dc, :], in0=x_res[:, dc, :], in1=t[:, 0:512], op=ALU.add),
                     rd=[("x", dc), ("f", 3 + dc % 2)], wr=[("x", dc)])
            if l == L - 1:
                for q in range(4):
                    P.dma("sp", lambda e, q=q: e.dma_start(out=outT[q * 512:(q + 1) * 512, t0:t0 + TT].rearrange("(dc p) t -> p dc t", p=128),
                                                           in_=x_res[:, q * 4:(q + 1) * 4, :]),
                          f"o{q}", rd=[("x", q * 4 + i) for i in range(4)], wr=[("out", q)])

        for ti in range(NT):
            for l in range(L):
                block(ti, l)
        P.wait_all("sp", [("out", q) for q in range(4)])
        P.emit()
        print("instructions:", P.n_inst, "sems:", P.sem_id)
    return nc


def _consts():
    cf = np.zeros((128, NCC), np.float32)
    cb = np.zeros((128, NCC), np.float32)
    p = np.arange(128)[:, None]
    c = np.arange(128)[None, :]
    cf[:, CF_ONES:CF_ONES + 128] = 1.0
    cb[:, CB_ONES:CB_ONES + 128] = 1.0
    cb[:, CB_BLK:CB_BLK + 128] = (p // 64 == c // 64)
    cb[:, CB_ID:CB_ID + 128] = (p == c)
    j = p % 64
    t = np.arange(64)[None, :]
    cf[:, CF_MA:CF_MA + 64] = (j <= t)
    cf[:, CF_MA + 64:CF_MA + 128] = (j < t)
    cf[:, CF_ML:CF_ML + 64] = (t < j)
    cf[:, CF_II:CF_II + 64] = (j == t)
    q = np.arange(128)[None, :]
    cb[:, CB_MS:CB_MS + 128] = (p > q)
    cb[:, CB_MS + 128:CB_MS + 256] = (q >= p)
    cb[:, CB_MS0 + 128:CB_MS0 + 256] = (q >= p)
    sc = np.ones((128, 512), np.float32)
    sc[:, 0::64] = 0.0
    cf[:, CF_SCAN:CF_SCAN + 512] = sc
    return cf, cb


def _layout(inputs, L):
    col = lambda v, n: np.ascontiguousarray(v.reshape(n, 128).T)
    pvs = []
    for l in range(L):
        sk = np.repeat(inputs["attn_sinks"][l], 64)
        pvs += [col(inputs["g_pre"][l], 16), col(inputs["g_post"][l], 16), col(inputs["g_mem"][l], 16),
                col(inputs["mu_shift"][l], 25), col(inputs["decay_base"][l], 8), col(inputs["iclr_base"][l], 8),
                col(inputs["k_k"][l], 8), col(inputs["k_a"][l], 8), col(inputs["r_k"][l].reshape(-1), 8),
                col(inputs["gn_w"][l], 8), col(inputs["gn_b"][l], 8), col(sk, 8)]
    pvd = np.ascontiguousarray(np.concatenate(pvs, axis=1), dtype=np.float32)
    dwd = np.ascontiguousarray(np.concatenate([inputs["decay_up"][:L], inputs["iclr_up"][:L]], axis=1), dtype=np.float32)
    return pvd, dwd


def run(inputs, T, L, B, trace=False):
    inputs = {k: np.asarray(v, dtype=np.float32) for k, v in inputs.items()}
    nc = build(T, L)
    pvd, dwd = _layout(inputs, L)
    cf, cb = _consts()
    shared = {
        "w_in": np.ascontiguousarray(inputs["w_in"][:L]), "w_mkv": np.ascontiguousarray(inputs["w_mem_kv"][:L]),
        "w_up0": np.ascontiguousarray(inputs["w_up_rwkv"][:L]), "w_up1": np.ascontiguousarray(inputs["w_up_swa"][:L]),
        "w_up2": np.ascontiguousarray(inputs["w_up_xattn"][:L]), "w_out": np.ascontiguousarray(inputs["w_out"][:L]),
        "dwd": dwd, "pvd": pvd, "ccdf": cf, "ccdb": cb,
    }
    in_maps = []
    for b in range(B):
        m = dict(shared)
        m["xT"] = np.ascontiguousarray(inputs["x"][b].T)
        m["memT"] = np.ascontiguousarray(inputs["mem"][b].T)
        in_maps.append(m)
    res = run_bass_kernel_spmd(nc, in_maps, core_ids=list(range(B)), trace=trace)
    out = np.stack([np.ascontiguousarray(r["outT"].T) for r in res.results], axis=0)
    return out.astype(np.float32), res


def kernel(**inputs):
    out, _ = run(inputs, 2048, 2, 8)
    return out
```

```python
import contextlib
import numpy as np
import concourse.bass as bass
import concourse.mybir as mybir
from concourse.bass_utils import run_bass_kernel_spmd

F32 = mybir.dt.float32
BF16 = mybir.dt.bfloat16
AF = mybir.ActivationFunctionType
ALU = mybir.AluOpType

D = 2048
DIN = 14720
MEM = 256
TT = 512
EPOCH = 30000

C_R, C_K, C_V, C_WD = 0, 1024, 2048, 3072
C_RG = 3200
C_SQ = 4224
C_SK = 5248
C_SV = 5376
C_SG = 5504
C_XQ = 6528
C_XG = 7552
C_MG = 8576

PV_GPRE, PV_GPOST, PV_GMEM, PV_MU = 0, 16, 32, 48
PV_DB, PV_IB, PV_KK, PV_KA, PV_RK, PV_GW, PV_GB, PV_SINK = 73, 81, 89, 97, 105, 113, 121, 129
NPV = 137
CF_ONES, CF_MA, CF_ML, CF_II, CF_SCAN = 0, 128, 256, 320, 384
CB_ONES, CB_BLK, CB_ID, CB_MS, CB_MS0 = 0, 128, 256, 384, 640
NCC = 896


class Prog:
    COMPUTE = ("pe", "act", "dve", "pool")

    def __init__(self, nc, stack):
        self.nc, self.stack = nc, stack
        self.eng_names = ("pe", "act", "dve", "pool", "sp")
        self.ops = {e: [] for e in self.eng_names}
        self.cnt = {e: 0 for e in self.COMPUTE}
        self.sem_objs, self.sem_id, self.cur_sem = {}, 0, {}
        for e in self.COMPUTE:
            self.cur_sem[e] = self._new_sem()
        self.waited = {e: {} for e in self.eng_names}
        self.buf, self.dma_sems = {}, {}
        self.n_inst = 0
        self.E = {"pe": nc.tensor, "act": nc.scalar, "dve": nc.vector, "pool": nc.gpsimd, "sp": nc.sync}

    def _new_sem(self):
        s = self.stack.enter_context(self.nc.semaphore(f"s{self.sem_id}"))
        self.sem_objs[self.sem_id] = s
        self.sem_id += 1
        return self.sem_id - 1

    def _deps(self, eng, reads, writes):
        need = {}

        def add(tok):
            sidx, val, teng = tok
            if teng == "pe" and eng == "pe":
                return
            if need.get(sidx, 0) < val:
                need[sidx] = val
        for k in reads:
            st = self.buf.get(k)
            if st and st[0] is not None:
                add(st[0])
        for k in writes:
            st = self.buf.get(k)
            if st:
                if st[0] is not None:
                    add(st[0])
                for t in st[1]:
                    add(t)
        for sidx, val in need.items():
            if self.waited[eng].get(sidx, 0) >= val:
                continue
            self.waited[eng][sidx] = val
            self.E[eng].wait_ge(self.sem_objs[sidx], val)

    def _record(self, tok, reads, writes):
        for k in reads:
            self.buf.setdefault(k, [None, []])[1].append(tok)
        for k in writes:
            self.buf[k] = [tok, []]

    def op(self, eng, fn, rd=(), wr=()):
        self._deps(eng, rd, wr)
        if self.cnt[eng] >= EPOCH:
            self.cur_sem[eng] = self._new_sem()
            self.cnt[eng] = 0
        self.cnt[eng] += 1
        tok = (self.cur_sem[eng], self.cnt[eng], eng)
        fn(self.E[eng]).then_inc(self.sem_objs[self.cur_sem[eng]], 1)
        self._record(tok, rd, wr)
        self.n_inst += 1

    def dma(self, eng, fn, semkey, rd=(), wr=(), nodep=False):
        if not nodep:
            self._deps(eng, rd, wr)
        if semkey not in self.dma_sems:
            self.dma_sems[semkey] = [self._new_sem(), 0]
        ds = self.dma_sems[semkey]
        ds[1] += 16
        tok = (ds[0], ds[1], "dma")
        fn(self.E[eng]).then_inc(self.sem_objs[ds[0]], 16)
        self._record(tok, rd, wr)
        self.n_inst += 1

    def wait_all(self, eng, keys):
        self._deps(eng, keys, ())

    def emit(self):
        return

    def emit_old(self):
        engmap = {"pe": "tensor", "act": "scalar", "dve": "vector", "pool": "gpsimd", "sp": "sync"}
        with self.nc.Block() as block:
            for e in self.eng_names:
                ops = self.ops[e]
                if not ops:
                    continue

                def body(eng, ops=ops):
                    for o in ops:
                        if o[0] == "wait":
                            eng.wait_ge(self.sem_objs[o[1]], o[2])
                        else:
                            o[1](eng).then_inc(self.sem_objs[o[2]], o[3])
                getattr(block, engmap[e])(body)


def build(T, L):
    NT = T // TT
    nc = bass.Bass("TRN2", target_bir_lowering=False)
    dr = lambda name, shape, kind="ExternalInput": nc.dram_tensor(name, shape, F32, kind=kind).ap()
    xT = dr("xT", [D, T])
    memT = dr("memT", [D, MEM])
    w_in = dr("w_in", [L, D, DIN])
    w_mkv = dr("w_mkv", [L, D, 2048])
    w_up = [dr(f"w_up{i}", [L, 1024, D]) for i in range(3)]
    w_out = dr("w_out", [L, D, D])
    dwd = dr("dwd", [L, 128, 1024])
    pvd = dr("pvd", [128, L * NPV])
    ccdf = dr("ccdf", [128, NCC])
    ccdb = dr("ccdb", [128, NCC])
    outT = dr("outT", [D, T], kind="ExternalOutput")

    with contextlib.ExitStack() as st:
        P = Prog(nc, st)
        sb = lambda name, shape, dt: st.enter_context(nc.sbuf_tensor(name, shape, dt))
        x_res = sb("x_res", [128, 16, TT], F32)
        HY = sb("HY", [128, 10240], F32)
        hT = HY[:, 0:4096].bitcast(BF16).rearrange("p (a b) -> p a b", b=TT)
        ybr = HY[:, 4096:10240].bitcast(BF16).rearrange("p (a b) -> p a b", b=TT)
        o_f = HY[:, 0:8192].rearrange("p (a b) -> p a b", b=TT)

        def okeys(dc):
            return [("h", 2 * dc), ("h", 2 * dc + 1)] if dc < 8 else [("y", 2 * (dc - 8)), ("y", 2 * (dc - 8) + 1)]
        wbuf = [sb(f"wbuf{i}", [128, 16, 512], BF16) for i in range(2)]
        NF, NB = 15, 29
        ft = [sb(f"ft{i}", [128, 514], F32) for i in range(NF)]
        bt_all = sb("bt_all", [128, NB, 512], BF16)
        bt = [bt_all[:, i, :] for i in range(NB)]
        f32v = lambda i: bt_all[:, i:i + 2, :].rearrange("p a b -> p (a b)").bitcast(F32)
        kmT = sb("kmT", [128, L * 8, MEM], BF16)
        vm = sb("vm", [128, L * 2, 1024], BF16)
        kd = sb("kd", [128, L * 2, 640], BF16)
        vt = sb("vt", [128, L * 5, 128], BF16)
        dw = sb("dw", [128, L, 1024], BF16)
        pv = sb("pv", [128, L * NPV], F32)
        omka = sb("omka", [128, L * 8], F32)
        nb = sb("nb", [128, L * 16], F32)
        esink = sb("esink", [128, L * 8], F32)
        ccf = sb("ccf", [128, NCC], F32)
        ccb = sb("ccb", [128, NCC], BF16)
        gs = sb("gs", [128, 64], F32)
        shp = sb("shp", [128, L * 25], F32)
        Hf = sb("Hf", [128, L * 8, 64], F32)
        Hb = sb("Hb", [128, L * 8, 64], BF16)
        ec = sb("ec", [128, 2, 8], F32)
        sm = sb("sm", [128, 256], BF16)
        pp = [st.enter_context(nc.psum_tensor(f"pp{i}", [128, 512], F32)) for i in range(4)]
        pq = [st.enter_context(nc.psum_tensor(f"pq{i}", [128, 512], F32)) for i in range(4)]
        PPK = [("pp", i) for i in range(4)]
        PQK = [("pq", i) for i in range(4)]

        onesF = ccf[:, CF_ONES:CF_ONES + 128]
        onesB = ccb[:, CB_ONES:CB_ONES + 128]
        blkB = ccb[:, CB_BLK:CB_BLK + 128]
        identB = ccb[:, CB_ID:CB_ID + 128]
        maskA = ccf[:, CF_MA:CF_MA + 128]
        maskL = ccf[:, CF_ML:CF_ML + 64]
        identI = ccf[:, CF_II:CF_II + 64]
        maskS = ccb[:, CB_MS:CB_MS + 256]
        maskS0 = ccb[:, CB_MS0:CB_MS0 + 256]
        scanm = ccf[:, CF_SCAN:CF_SCAN + 512]

        P.dma("sp", lambda e: e.dma_start(out=pv[:], in_=pvd), "pv", wr=["pv"])
        P.dma("sp", lambda e: e.dma_start(out=ccf[:], in_=ccdf), "ccf", wr=["ccf"])
        P.dma("pool", lambda e: e.dma_start(out=ccb[:], in_=ccdb), "ccb", wr=["ccb"])
        for l in range(L):
            P.dma("pool", lambda e, l=l: e.dma_start(out=dw[:, l, :], in_=dwd[l]), "dw", wr=["dw"])
        P.op("dve", lambda e: e.memset(shp[:], 0.0), wr=["shp"])
        P.op("dve", lambda e: e.memset(Hf[:], 0.0), wr=["Hf"])
        P.op("dve", lambda e: e.memset(Hb[:], 0.0), wr=["Hb"])
        P.op("dve", lambda e: e.memset(kd[:], 0.0), wr=["kd"])
        P.op("dve", lambda e: e.memset(vt[:], 0.0), wr=["vt"])
        for l in range(L):
            b = l * NPV
            P.op("dve", lambda e, l=l, b=b: e.tensor_scalar(out=omka[:, l * 8:(l + 1) * 8], in0=pv[:, b + PV_KA:b + PV_KA + 8],
                                                             scalar1=-1.0, scalar2=1.0, op0=ALU.mult, op1=ALU.add), rd=["pv"], wr=["omka"])
            P.op("act", lambda e, l=l, b=b: e.activation(out=esink[:, l * 8:(l + 1) * 8], in_=pv[:, b + PV_SINK:b + PV_SINK + 8], func=AF.Exp),
                 rd=["pv"], wr=["esink"])
            P.op("dve", lambda e, l=l, b=b: e.tensor_scalar(out=nb[:, l * 16:(l + 1) * 16], in0=pv[:, b + PV_DB:b + PV_DB + 16],
                                                             scalar1=-1.0, scalar2=None, op0=ALU.mult), rd=["pv"], wr=["nb"])

        wstate = {"slot": 0, "gid": None, "ti": 0}
        NG = 46
        wscr = nc.dram_tensor("wscr", [L * NG, 128, 8192], BF16, kind="Internal").ap()

        def load_w(src3, pieces, nk=16):
            s = wstate["slot"]
            wstate["slot"] = 1 - s
            gid = wstate["gid"]
            if gid is not None:
                wstate["gid"] = gid + 1
            if gid is not None and wstate["ti"] > 0:
                P.dma("sp", lambda e: e.dma_start(out=wbuf[s][:, 0:nk, :].rearrange("p a b -> p (a b)"), in_=wscr[gid][:, 0:nk * 512]),
                      f"w{s}", rd=[("scr", gid)], wr=[("w", s)])
                return s
            for pi, (doff, c0, n) in enumerate(pieces):
                P.dma("pool", lambda e, s=s, doff=doff, c0=c0, n=n: e.dma_start(
                    out=wbuf[s][:, 0:nk, doff:doff + n],
                    in_=src3[:, c0:c0 + n].rearrange("(kc p) c -> p kc c", p=128)), f"w{s}", wr=[("w", s)], nodep=(pi > 0))
            if gid is not None and NT > 1:
                P.dma("sp", lambda e: e.dma_start(out=wscr[gid][:, 0:nk * 512], in_=wbuf[s][:, 0:nk, :].rearrange("p a b -> p (a b)")),
                      f"ws{s}", rd=[("w", s)], wr=[("scr", gid)])
            return s

        def proj(s, j, out_ps, out_key, rhs_fn, rhs_keys, nk=16, ncol=128):
            for kc in range(nk):
                P.op("pe", lambda e, kc=kc: e.matmul(out_ps, lhsT=wbuf[s][:, kc, j * 128:j * 128 + ncol], rhs=rhs_fn(kc),
                                                      start=(kc == 0), stop=(kc == nk - 1)),
                     rd=[("w", s)] + rhs_keys(kc), wr=[out_key])

        HK = [("h", i) for i in range(16)]

        def proj_h(s, j, bank):
            proj(s, j, pp[bank][:], ("pp", bank), lambda kc: hT[:, kc, :], lambda kc: [("h", kc)])

        def act_sigmoid(out, in_, rd, wrk, nbias=None, final_bias=None):
            kw = {"bias": nbias} if nbias is not None else {}
            P.op("act", lambda e: e.activation(out=out, in_=in_, func=AF.Exp, scale=-1.0, **kw), rd=rd, wr=wrk)
            P.op("act", lambda e: e.activation(out=out, in_=out, func=AF.Ln, bias=1.0), rd=wrk, wr=wrk)
            kw2 = {"bias": final_bias} if final_bias is not None else {}
            P.op("act", lambda e: e.activation(out=out, in_=out, func=AF.Exp, scale=-1.0, **kw2), rd=wrk, wr=wrk)

        def act_silu(out, in_ps, rd, wrk):
            act_sigmoid(out, in_ps, rd, wrk)
            P.op("dve", lambda e: e.tensor_tensor(out=out, in0=in_ps, in1=out, op=ALU.mult), rd=rd + wrk, wr=wrk)

        def act_rpow(out, in_, rd, wrk, p, scale=1.0, bias=None):
            kw = {"bias": bias} if bias is not None else {}
            P.op("act", lambda e: e.activation(out=out, in_=in_, func=AF.Ln, scale=scale, **kw), rd=rd, wr=wrk)
            P.op("act", lambda e: e.activation(out=out, in_=out, func=AF.Exp, scale=-p), rd=wrk, wr=wrk)

        def rms_stats(src_fn, src_keys, ncols, nchunks, out_rstd, out_key, tmpi):
            for dc in range(nchunks):
                bi = 27 + (dc % 2)
                t = bt[bi]
                P.op("act", lambda e, dc=dc, t=t: e.activation(out=t[:, 0:ncols], in_=src_fn(dc), func=AF.Square),
                     rd=src_keys(dc), wr=[("b", bi)])
                P.op("pe", lambda e, dc=dc, t=t: e.matmul(pq[0][:, 0:ncols], lhsT=onesB, rhs=t[:, 0:ncols],
                                                           start=(dc == 0), stop=(dc == nchunks - 1)),
                     rd=[("b", bi), "ccb"], wr=[("pq", 0)])
            act_rpow(out_rstd, pq[0][:, 0:ncols], [("pq", 0)], [out_key], 0.5, scale=1.0 / D, bias=1e-6)

        mT = x_res
        P.dma("sp", lambda e: e.dma_start(out=mT[:, :, 0:MEM], in_=memT.rearrange("(dc p) m -> p dc m", p=128)), "x0",
              wr=[("x", i) for i in range(16)])
        rstd_m = ft[2]
        rms_stats(lambda dc: mT[:, dc, 0:MEM], lambda dc: [("x", dc)], MEM, 16, rstd_m[:, 0:MEM], ("f", 2), 0)
        for l in range(L):
            b = l * NPV
            for dc in range(16):
                P.op("dve", lambda e, dc=dc, b=b: e.scalar_tensor_tensor(out=hT[:, dc, 0:MEM], in0=mT[:, dc, 0:MEM],
                                                                          scalar=pv[:, b + PV_GMEM + dc:b + PV_GMEM + dc + 1],
                                                                          in1=rstd_m[:, 0:MEM], op0=ALU.mult, op1=ALU.mult),
                     rd=[("x", dc), "pv", ("f", 2)], wr=[("h", dc)])
            for g in range(4):
                s = load_w(w_mkv[l], [(0, g * 512, 512)])
                if g < 2:
                    for j in range(4):
                        proj(s, j, pp[j][:, 0:MEM], ("pp", j), lambda kc: hT[:, kc, 0:MEM], lambda kc: [("h", kc)])
                        ci = l * 8 + g * 4 + j
                        P.op("act", lambda e, j=j, ci=ci: e.activation(out=kmT[:, ci, :], in_=pp[j][:, 0:MEM], func=AF.Copy),
                             rd=[("pp", j)], wr=["kmT"])
                else:
                    for mt in range(2):
                        for kc in range(16):
                            P.op("pe", lambda e, kc=kc, mt=mt, s=s: e.matmul(pp[mt][:], lhsT=hT[:, kc, mt * 128:(mt + 1) * 128],
                                                                                rhs=wbuf[s][:, kc, :], start=(kc == 0), stop=(kc == 15)),
                                 rd=[("w", s), ("h", kc)], wr=[("pp", mt)])
                        P.op("act", lambda e, mt=mt, l=l, g=g: e.activation(out=vm[:, l * 2 + mt, (g - 2) * 512:(g - 1) * 512], in_=pp[mt][:],
                                                                             func=AF.Copy), rd=[("pp", mt)], wr=["vm"])

        def block(ti, l):
            b = l * NPV
            wstate["gid"] = l * NG
            wstate["ti"] = ti
            pvc = lambda off, c: pv[:, b + off + c:b + off + c + 1]
            t0 = ti * TT
            if l == 0:
                for q in range(4):
                    P.dma("sp", lambda e, q=q: e.dma_start(out=x_res[:, q * 4:(q + 1) * 4, :],
                                                           in_=xT[q * 512:(q + 1) * 512, t0:t0 + TT].rearrange("(dc p) t -> p dc t", p=128)),
                          f"x{q}", wr=[("x", q * 4 + i) for i in range(4)])
            rstd = ft[2]
            rms_stats(lambda dc: x_res[:, dc, :], lambda dc: [("x", dc)], TT, 16, rstd[:, 0:TT], ("f", 2), 0)
            for dc in range(16):
                P.op("dve", lambda e, dc=dc: e.scalar_tensor_tensor(out=hT[:, dc, :], in0=x_res[:, dc, :], scalar=pvc(PV_GPRE, dc),
                                                                     in1=rstd[:, 0:TT], op0=ALU.mult, op1=ALU.mult),
                     rd=[("x", dc), "pv", ("f", 2)], wr=[("h", dc)])

            def shift(bank, fi_raw, fi_out, chunk_idx, rows=slice(0, 128)):
                raw = ft[fi_raw]
                si = l * 25 + chunk_idx
                P.op("act", lambda e: e.activation(out=raw[:, 1:513], in_=pp[bank][:], func=AF.Copy), rd=[("pp", bank)], wr=[("f", fi_raw)])
                P.op("dve", lambda e: e.tensor_copy(out=raw[:, 0:1], in_=shp[:, si:si + 1]), rd=["shp"], wr=[("f", fi_raw)])
                P.op("dve", lambda e: e.tensor_copy(out=shp[:, si:si + 1], in_=raw[:, 512:513]), rd=[("f", fi_raw)], wr=["shp"])
                P.op("dve", lambda e: e.tensor_tensor(out=ft[fi_out][:, 0:512], in0=raw[:, 0:512], in1=raw[:, 1:513], op=ALU.subtract),
                     rd=[("f", fi_raw)], wr=[("f", fi_out)])
                P.op("dve", lambda e: e.scalar_tensor_tensor(out=ft[fi_out][:, 0:512], in0=ft[fi_out][:, 0:512], scalar=pvc(PV_MU, chunk_idx),
                                                             in1=raw[:, 1:513], op0=ALU.mult, op1=ALU.add),
                     rd=[("f", fi_out), ("f", fi_raw), "pv"], wr=[("f", fi_out)])

            s = load_w(w_in[l], [(0, C_WD, 128)])
            proj_h(s, 0, 0)
            shift(0, 0, 1, 24)
            twd, adb = bt[7], bt[8]
            P.op("act", lambda e: e.activation(out=twd[0:64, :], in_=ft[1][0:64, 0:512], func=AF.Tanh), rd=[("f", 1)], wr=[("b", 7)])
            P.op("dve", lambda e: e.tensor_copy(out=adb[64:128, :], in_=ft[1][64:128, 0:512]), rd=[("f", 1)], wr=[("b", 8)])
            v3 = lambda ap: ap.rearrange("p (n t) -> p n t", t=64)
            r4 = lambda x_: x_.rearrange("p (n w t) -> p n w t", w=2, t=64)
            c3 = lambda x_: x_.rearrange("p (n c) -> p n c", c=128)
            BONS = [(ft[11], 11), (ft[13], 13)]
            SGS = [(ft[6], 6), (ft[14], 14)]
            KB4 = [r4(bt[1]), r4(bt[2])]
            KBK = [("b", 1), ("b", 2)]
            Vb, vbi = bt[3], 3

            def pset(par):
                b0 = 9 + par * 10
                d = {"VT": (bt[b0], b0), "KIT": (bt[b0 + 1], b0 + 1), "BIT": (bt[b0 + 2], b0 + 2),
                     "ATk": [(bt[b0 + 3], b0 + 3), (bt[b0 + 4], b0 + 4)], "ATb": [(bt[b0 + 5], b0 + 5), (bt[b0 + 6], b0 + 6)],
                     "Mt": (bt[b0 + 7], b0 + 7), "RA": [(bt[b0 + 8], b0 + 8), (bt[b0 + 9], b0 + 9)], "b0": b0}
                return d

            def stage1(hp, par):
                S = pset(par)
                RA4 = [r4(S["RA"][h][0]) for h in range(2)]
                RAK = [("b", S["RA"][h][1]) for h in range(2)]
                BON, boni = BONS[par]
                sg, sgi = SGS[par]
                ecp, eck = ec[:, par, :], ("ec", par)
                s = load_w(w_in[l], [(0, C_R + hp * 128, 128), (128, C_K + hp * 128, 128), (256, C_V + hp * 128, 128),
                                     (384, C_RG + hp * 128, 128)])
                for j in range(4):
                    for k4 in range(4):
                        for kc in range(k4 * 4, k4 * 4 + 4):
                            P.op("pe", lambda e, kc=kc, j=j: e.matmul(pp[j][:], lhsT=wbuf[s][:, kc, j * 128:(j + 1) * 128], rhs=hT[:, kc, :],
                                                                      start=(kc == 0), stop=(kc == 15)), rd=[("w", s), ("h", kc)], wr=[("pp", j)])
                        yield
                Rs, Ks, Vs = ft[3], ft[4], ft[5]
                shift(0, 0, 3, hp)
                yield
                shift(1, 1, 4, 8 + hp)
                yield
                shift(2, 0, 5, 16 + hp)
                yield
                act_silu(sg[:, 0:512], pp[3][:], [("pp", 3)], [("f", sgi)])
                P.op("pe", lambda e: e.matmul(pp[0][:], lhsT=dw[0:64, l, hp * 128:(hp + 1) * 128], rhs=twd[0:64, :], start=True, stop=True),
                     rd=["dw", ("b", 7)], wr=[("pp", 0)])
                P.op("pe", lambda e: e.matmul(pp[1][:], lhsT=dw[64:128, l, hp * 128:(hp + 1) * 128], rhs=adb[64:128, :], start=True, stop=True),
                     rd=["dw", ("b", 8)], wr=[("pp", 1)])
                yield
                LW, A = ft[7], ft[8]
                act_sigmoid(LW[:, 0:512], pp[0][:], [("pp", 0), "nb"], [("f", 7)], nbias=nb[:, l * 16 + hp:l * 16 + hp + 1], final_bias=-0.5)
                yield
                act_sigmoid(A[:, 0:512], pp[1][:], [("pp", 1), "nb"], [("f", 8)], nbias=nb[:, l * 16 + 8 + hp:l * 16 + 8 + hp + 1])
                yield
                KK, TMP = ft[9], ft[0]
                P.op("act", lambda e: e.activation(out=KK[:, 0:512], in_=Ks[:, 0:512], func=AF.Copy, scale=pvc(PV_KK, hp)),
                     rd=[("f", 4), "pv"], wr=[("f", 9)])
                P.op("dve", lambda e: e.tensor_tensor(out=bt[6][:], in0=KK[:, 0:512], in1=KK[:, 0:512], op=ALU.mult), rd=[("f", 9)], wr=[("b", 6)])
                P.op("pe", lambda e: e.matmul(pp[2][:], lhsT=blkB, rhs=bt[6][:], start=True, stop=True), rd=["ccb", ("b", 6)], wr=[("pp", 2)])
                yield
                K2 = ft[10]
                P.op("act", lambda e: e.activation(out=K2[:, 0:512], in_=A[:, 0:512], func=AF.Identity, scale=pvc(PV_KA, hp),
                                                   bias=omka[:, l * 8 + hp:l * 8 + hp + 1]), rd=[("f", 8), "pv", "omka"], wr=[("f", 10)])
                P.op("dve", lambda e: e.tensor_scalar(out=TMP[:, 0:512], in0=pp[2][:], scalar1=1e-24, scalar2=None, op0=ALU.max),
                     rd=[("pp", 2)], wr=[("f", 0)])
                yield
                act_rpow(TMP[:, 0:512], TMP[:, 0:512], [("f", 0)], [("f", 0)], 0.5)
                yield
                P.op("dve", lambda e: e.tensor_tensor(out=KK[:, 0:512], in0=KK[:, 0:512], in1=TMP[:, 0:512], op=ALU.mult),
                     rd=[("f", 9), ("f", 0)], wr=[("f", 9)])
                yield
                P.op("dve", lambda e: e.tensor_tensor(out=K2[:, 0:512], in0=K2[:, 0:512], in1=Ks[:, 0:512], op=ALU.mult),
                     rd=[("f", 10), ("f", 4)], wr=[("f", 10)])
                yield
                P.op("dve", lambda e: e.scalar_tensor_tensor(out=bt[6][:], in0=Rs[:, 0:512], scalar=pvc(PV_RK, hp), in1=K2[:, 0:512],
                                                             op0=ALU.mult, op1=ALU.mult), rd=[("f", 3), ("f", 10), "pv"], wr=[("b", 6)])
                P.op("pe", lambda e: e.matmul(pp[3][:], lhsT=blkB, rhs=bt[6][:], start=True, stop=True), rd=["ccb", ("b", 6)], wr=[("pp", 3)])
                yield
                CUM = ft[1]
                P.op("dve", lambda e: e.tensor_tensor_scan(out=CUM[:, 0:512], data0=scanm, data1=LW[:, 0:512], initial=0.0,
                                                           op0=ALU.mult, op1=ALU.subtract), rd=["ccf", ("f", 7)], wr=[("f", 1)])
                yield
                P.op("dve", lambda e: e.tensor_tensor(out=BON[:, 0:512], in0=pp[3][:], in1=Vs[:, 0:512], op=ALU.mult),
                     rd=[("pp", 3), ("f", 5)], wr=[("f", boni)])
                EP, EM = ft[12], ft[0]
                P.op("act", lambda e: e.activation(out=EP[:, 0:512], in_=CUM[:, 0:512], func=AF.Exp), rd=[("f", 1)], wr=[("f", 12)])
                yield
                P.op("dve", lambda e: e.tensor_tensor(out=EM[:, 0:512], in0=CUM[:, 0:512], in1=LW[:, 0:512], op=ALU.add),
                     rd=[("f", 1), ("f", 7)], wr=[("f", 0)])
                P.op("act", lambda e: e.activation(out=EM[:, 0:512], in_=EM[:, 0:512], func=AF.Exp), rd=[("f", 0)], wr=[("f", 0)])
                yield
                P.op("dve", lambda e: e.tensor_copy(out=ecp, in_=v3(EP[:, 0:512])[:, :, 63]), rd=[("f", 12)], wr=[eck])
                for h in range(2):
                    P.op("dve", lambda e, h=h: e.tensor_tensor(out=RA4[h][:, :, 0, :], in0=v3(Rs[:, h * 256:(h + 1) * 256]),
                                                               in1=v3(EP[:, h * 256:(h + 1) * 256]), op=ALU.mult),
                         rd=[("f", 3), ("f", 12)], wr=[RAK[h]])
                    yield
                for h in range(2):
                    P.op("dve", lambda e, h=h: e.scalar_tensor_tensor(out=RA4[h][:, :, 1, :], in0=v3(KK[:, h * 256:(h + 1) * 256]), scalar=-1.0,
                                                                      in1=v3(EM[:, h * 256:(h + 1) * 256]), op0=ALU.mult, op1=ALU.mult),
                         rd=[("f", 9), ("f", 0)], wr=[RAK[h]])
                    yield
                EN = ft[12]
                P.op("act", lambda e: e.activation(out=EN[:, 0:512], in_=CUM[:, 0:512], func=AF.Exp, scale=-1.0), rd=[("f", 1)], wr=[("f", 12)])
                BP = ft[7]
                P.op("dve", lambda e: e.tensor_tensor(out=BP[:, 0:512], in0=KK[:, 0:512], in1=A[:, 0:512], op=ALU.mult),
                     rd=[("f", 9), ("f", 8)], wr=[("f", 7)])
                yield
                for h in range(2):
                    sl = slice(h * 256, (h + 1) * 256)
                    P.op("dve", lambda e, h=h, sl=sl: e.tensor_tensor(out=KB4[h][:, :, 0, :], in0=v3(K2[:, sl]), in1=v3(EN[:, sl]), op=ALU.mult),
                         rd=[("f", 10), ("f", 12)], wr=[KBK[h]])
                    yield
                    P.op("dve", lambda e, h=h, sl=sl: e.tensor_tensor(out=KB4[h][:, :, 1, :], in0=v3(BP[:, sl]), in1=v3(EN[:, sl]), op=ALU.mult),
                         rd=[("f", 7), ("f", 12)], wr=[KBK[h]])
                    yield
                P.op("act", lambda e: e.activation(out=Vb[:], in_=Vs[:, 0:512], func=AF.Copy), rd=[("f", 5)], wr=[("b", vbi)])
                yield
                ppb = [pp[i][:].bitcast(BF16) for i in range(4)]
                for (nm, bank, srcfn, skeys, eng) in (
                        ("VT", 0, lambda n, hs: Vb[hs, n * 64:(n + 1) * 64], lambda n: [("b", vbi)], "act"),
                        ("KIT", 1, lambda n, hs: KB4[n // 4][hs, n % 4, 0, :], lambda n: [KBK[n // 4]], "dve"),
                        ("BIT", 2, lambda n, hs: KB4[n // 4][hs, n % 4, 1, :], lambda n: [KBK[n // 4]], "act")):
                    dst, di = S[nm]
                    for n in range(8):
                        for h2 in range(2):
                            hs = slice(h2 * 64, (h2 + 1) * 64)
                            P.op("pe", lambda e, n=n, hs=hs, bank=bank, srcfn=srcfn: e.transpose(
                                out=ppb[bank][hs, n * 64:(n + 1) * 64], in_=srcfn(n, hs), identity=identB[hs, hs]),
                                rd=skeys(n) + ["ccb"], wr=[("pp", bank)])
                    if eng == "act":
                        P.op("act", lambda e, dst=dst, bank=bank: e.activation(out=dst[:], in_=ppb[bank][:, 0:512], func=AF.Copy),
                             rd=[("pp", bank)], wr=[("b", di)])
                    else:
                        P.op("dve", lambda e, dst=dst, bank=bank: e.tensor_copy(out=dst[:], in_=ppb[bank][:, 0:512]),
                             rd=[("pp", bank)], wr=[("b", di)])
                    yield
                X0, Xt0 = bt[4], bt[5]
                mA = maskA.unsqueeze(1).broadcast_to([128, 4, 128])
                mL = maskL.unsqueeze(1).broadcast_to([128, 4, 64])
                for h in range(2):
                    ATk, atki = S["ATk"][h]
                    ATb, atbi = S["ATb"][h]
                    for n4 in range(4):
                        for h2 in range(2):
                            hs = slice(h2 * 64, (h2 + 1) * 64)
                            P.op("pe", lambda e, h=h, n4=n4, hs=hs: e.matmul(pp[0][hs, n4 * 128:(n4 + 1) * 128], lhsT=KB4[h][hs, n4, 0, :],
                                                                             rhs=RA4[h][hs, n4, :, :], start=True, stop=True),
                                 rd=[KBK[h], RAK[h]], wr=[("pp", 0)])
                            P.op("pe", lambda e, h=h, n4=n4, hs=hs: e.matmul(pp[1][hs, n4 * 128:(n4 + 1) * 128], lhsT=KB4[h][hs, n4, 1, :],
                                                                             rhs=RA4[h][hs, n4, :, :], start=True, stop=True),
                                 rd=[KBK[h], RAK[h]], wr=[("pp", 1)])
                            P.op("pe", lambda e, h=h, n4=n4, hs=hs: e.matmul(pp[2][hs, n4 * 64:(n4 + 1) * 64], lhsT=RA4[h][hs, n4, 1, :],
                                                                             rhs=KB4[h][hs, n4, 1, :], start=True, stop=True),
                                 rd=[KBK[h], RAK[h]], wr=[("pp", 2)])
                    yield
                    P.op("dve", lambda e, ATk=ATk: e.tensor_tensor(out=c3(ATk[:]), in0=c3(pp[0][:]), in1=mA, op=ALU.mult),
                         rd=[("pp", 0), "ccf"], wr=[("b", atki)])
                    P.op("dve", lambda e, ATb=ATb: e.tensor_tensor(out=c3(ATb[:]), in0=c3(pp[1][:]), in1=mA, op=ALU.mult),
                         rd=[("pp", 1), "ccf"], wr=[("b", atbi)])
                    P.op("dve", lambda e, h=h: e.tensor_tensor(out=X0[:, h * 256:(h + 1) * 256].rearrange("p (n c) -> p n c", c=64),
                                                               in0=pp[2][:, 0:256].rearrange("p (n c) -> p n c", c=64), in1=mL, op=ALU.mult),
                         rd=[("pp", 2), "ccf"], wr=[("b", 4)])
                    P.op("act", lambda e, h=h, ATb=ATb: e.activation(out=Xt0[:, h * 256:(h + 1) * 256].rearrange("p (n c) -> p n c", c=64),
                                                                     in_=c3(ATb[:])[:, :, 64:128], func=AF.Copy), rd=[("b", atbi)], wr=[("b", 5)])
                    yield
                Mt, mti = S["Mt"]
                iI = identI.unsqueeze(1).broadcast_to([128, 8, 64])
                P.op("dve", lambda e: e.tensor_tensor(out=v3(Mt[:]), in0=v3(Xt0[:]), in1=iI, op=ALU.add), rd=[("b", 5), "ccf"], wr=[("b", mti)])
                yield
                pingX, pingXt = [(bt[4], 4), (bt[0], 0)], [(bt[5], 5), (Vb, vbi)]

                def sq(Xc, Xtc, xi, xti):
                    for n in range(8):
                        for h2 in range(2):
                            hs = slice(h2 * 64, (h2 + 1) * 64)
                            cs = slice(n * 64, (n + 1) * 64)
                            P.op("pe", lambda e, hs=hs, cs=cs: e.matmul(pp[0][hs, cs], lhsT=Xc[hs, cs], rhs=Xtc[hs, cs], start=True, stop=True),
                                 rd=[("b", xi), ("b", xti)], wr=[("pp", 0)])
                            P.op("pe", lambda e, hs=hs, cs=cs: e.matmul(pp[1][hs, cs], lhsT=Xtc[hs, cs], rhs=Xc[hs, cs], start=True, stop=True),
                                 rd=[("b", xi), ("b", xti)], wr=[("pp", 1)])

                def ev(lvl):
                    Xn, xni = pingX[lvl % 2]
                    Xtn, xtni = pingXt[lvl % 2]
                    P.op("act", lambda e: e.activation(out=Xtn[:], in_=pp[0][:], func=AF.Copy), rd=[("pp", 0)], wr=[("b", xtni)])
                    P.op("dve", lambda e: e.tensor_copy(out=Xn[:], in_=pp[1][:]), rd=[("pp", 1)], wr=[("b", xni)])
                    return Xn, Xtn, xni, xtni

                def mtmm(Xn, xni):
                    for n in range(8):
                        for h2 in range(2):
                            hs = slice(h2 * 64, (h2 + 1) * 64)
                            cs = slice(n * 64, (n + 1) * 64)
                            P.op("pe", lambda e, hs=hs, cs=cs: e.matmul(pp[2][hs, cs], lhsT=Xn[hs, cs], rhs=Mt[hs, cs], start=True, stop=True),
                                 rd=[("b", xni), ("b", mti)], wr=[("pp", 2)])

                def madd():
                    P.op("dve", lambda e: e.tensor_tensor(out=Mt[:], in0=pp[2][:], in1=Mt[:], op=ALU.add), rd=[("pp", 2), ("b", mti)], wr=[("b", mti)])

                sq(X0, Xt0, 4, 5)
                yield
                Xc, Xtc, xi, xti = ev(1)
                yield
                for lvl in range(2, 6):
                    sq(Xc, Xtc, xi, xti)
                    yield
                    mtmm(Xc, xi)
                    yield
                    Xc, Xtc, xi, xti = ev(lvl)
                    yield
                    madd()
                    yield
                mtmm(Xc, xi)
                yield
                madd()
                yield

            def stage2(hp, par):
                S = pset(par)
                RA4 = [r4(S["RA"][h][0]) for h in range(2)]
                RAK = [("b", S["RA"][h][1]) for h in range(2)]
                ATk3 = [c3(S["ATk"][h][0][:]) for h in range(2)]
                ATb3 = [c3(S["ATb"][h][0][:]) for h in range(2)]
                ATKK = [("b", S["ATk"][h][1]) for h in range(2)]
                ATBK = [("b", S["ATb"][h][1]) for h in range(2)]
                VT, vti = S["VT"]
                KIT, kiti = S["KIT"]
                BIT, biti = S["BIT"]
                Mt, mti = S["Mt"]
                BON, boni = BONS[par]
                sg, sgi = SGS[par]
                ecp, eck = ec[:, par, :], ("ec", par)
                hh = l * 8 + hp
                R0b, Ub = sm[:, 0:64], sm[:, 64:128]
                for n in range(8):
                    h, n4 = n // 4, n % 4
                    cs = slice(n * 64, (n + 1) * 64)
                    P.op("act", lambda e, n=n: e.activation(out=gs[:, 0:64], in_=Hf[:, hh, :], func=AF.Copy, scale=ecp[:, n:n + 1]),
                         rd=["Hf", eck], wr=["gs"])
                    for h2 in range(2):
                        hs = slice(h2 * 64, (h2 + 1) * 64)
                        P.op("pe", lambda e, hs=hs, h=h, n4=n4: e.matmul(pq[3][hs, 0:64], lhsT=RA4[h][hs, n4, 1, :], rhs=Hb[hs, hh, :], start=True, stop=False),
                             rd=[RAK[h], "Hb"], wr=[("pq", 3)])
                        P.op("pe", lambda e, hs=hs, h=h, n4=n4, cs=cs: e.matmul(pq[3][hs, 0:64], lhsT=ATk3[h][hs, n4, 64:128], rhs=VT[hs, cs], start=False, stop=True),
                             rd=[ATKK[h], ("b", vti)], wr=[("pq", 3)])
                    yield
                    P.op("act", lambda e: e.activation(out=R0b, in_=pq[3][:, 0:64], func=AF.Copy), rd=[("pq", 3)], wr=["sm0"])
                    for h2 in range(2):
                        hs = slice(h2 * 64, (h2 + 1) * 64)
                        P.op("pe", lambda e, hs=hs, cs=cs: e.matmul(pq[3][hs, 64:128], lhsT=Mt[hs, cs], rhs=sm[hs, 0:64], start=True, stop=True),
                             rd=[("b", mti), "sm0"], wr=[("pq", 3)])
                    yield
                    P.op("dve", lambda e: e.tensor_copy(out=Ub, in_=pq[3][:, 64:128]), rd=[("pq", 3)], wr=["sm1"])
                    for h2 in range(2):
                        hs = slice(h2 * 64, (h2 + 1) * 64)
                        P.op("pe", lambda e, hs=hs, cs=cs: e.matmul(pq[3][hs, 128:192], lhsT=KIT[hs, cs], rhs=VT[hs, cs], start=True, stop=False),
                             rd=[("b", kiti), ("b", vti)], wr=[("pq", 3)])
                        P.op("pe", lambda e, hs=hs, cs=cs: e.matmul(pq[3][hs, 128:192], lhsT=BIT[hs, cs], rhs=sm[hs, 64:128], start=False, stop=True),
                             rd=[("b", biti), "sm1"], wr=[("pq", 3)])
                    for h2 in range(2):
                        hs = slice(h2 * 64, (h2 + 1) * 64)
                        P.op("pe", lambda e, hs=hs, h=h, n4=n4, cs=cs: e.matmul(pq[0][hs, cs], lhsT=Hb[hs, hh, :], rhs=RA4[h][hs, n4, 0, :], start=True, stop=False),
                             rd=["Hb", RAK[h]], wr=[("pq", 0)])
                        P.op("pe", lambda e, hs=hs, h=h, n4=n4, cs=cs: e.matmul(pq[0][hs, cs], lhsT=sm[hs, 64:128], rhs=ATb3[h][hs, n4, 0:64], start=False, stop=False),
                             rd=["sm1", ATBK[h]], wr=[("pq", 0)])
                        P.op("pe", lambda e, hs=hs, h=h, n4=n4, cs=cs: e.matmul(pq[0][hs, cs], lhsT=VT[hs, cs], rhs=ATk3[h][hs, n4, 0:64], start=False, stop=True),
                             rd=[("b", vti), ATKK[h]], wr=[("pq", 0)])
                    yield
                    P.op("dve", lambda e, n=n: e.scalar_tensor_tensor(out=Hb[:, hh, :], in0=pq[3][:, 128:192], scalar=ecp[:, n:n + 1], in1=gs[:, 0:64],
                                                                      op0=ALU.mult, op1=ALU.add), rd=[("pq", 3), eck, "gs"], wr=["Hb"])
                    P.op("dve", lambda e, n=n: e.scalar_tensor_tensor(out=Hf[:, hh, :], in0=pq[3][:, 128:192], scalar=ecp[:, n:n + 1], in1=gs[:, 0:64],
                                                                      op0=ALU.mult, op1=ALU.add), rd=[("pq", 3), eck, "gs"], wr=["Hf"])
                    yield
                b0 = S["b0"]
                Yf, MEAN, VAR = f32v(b0), f32v(b0 + 3), f32v(b0 + 5)
                YFK, MK, VK = [("b", b0), ("b", b0 + 1)], [("b", b0 + 3), ("b", b0 + 4)], [("b", b0 + 5), ("b", b0 + 6)]
                Yb, ybi, Y2, y2i = bt[b0 + 2], b0 + 2, bt[b0 + 7], b0 + 7
                P.op("act", lambda e: e.activation(out=Yf, in_=pq[0][:], func=AF.Copy), rd=[("pq", 0)], wr=YFK)
                yield
                P.op("dve", lambda e: e.tensor_copy(out=Yb[:], in_=Yf), rd=YFK, wr=[("b", ybi)])
                P.op("act", lambda e: e.activation(out=Y2[:], in_=Yf, func=AF.Square), rd=YFK, wr=[("b", y2i)])
                P.op("pe", lambda e: e.matmul(pq[1][:], lhsT=blkB, rhs=Yb[:], start=True, stop=True), rd=["ccb", ("b", ybi)], wr=[("pq", 1)])
                P.op("pe", lambda e: e.matmul(pq[2][:], lhsT=blkB, rhs=Y2[:], start=True, stop=True), rd=["ccb", ("b", y2i)], wr=[("pq", 2)])
                yield
                P.op("act", lambda e: e.activation(out=MEAN, in_=pq[1][:], func=AF.Copy, scale=1.0 / 64), rd=[("pq", 1)], wr=MK)
                yield
                P.op("dve", lambda e: e.tensor_tensor(out=VAR, in0=MEAN, in1=MEAN, op=ALU.mult), rd=MK, wr=VK)
                yield
                P.op("dve", lambda e: e.scalar_tensor_tensor(out=VAR, in0=pq[2][:], scalar=1.0 / 64, in1=VAR,
                                                             op0=ALU.mult, op1=ALU.subtract), rd=[("pq", 2)] + VK, wr=VK)
                yield
                act_rpow(VAR, VAR, VK, VK, 0.5, bias=64e-5)
                P.op("dve", lambda e: e.tensor_tensor(out=Yf, in0=Yf, in1=MEAN, op=ALU.subtract), rd=YFK + MK, wr=YFK)
                yield
                P.op("dve", lambda e: e.tensor_tensor(out=Yf, in0=Yf, in1=VAR, op=ALU.mult), rd=YFK + VK, wr=YFK)
                yield
                P.op("act", lambda e: e.activation(out=Yf, in_=Yf, func=AF.Identity, scale=pvc(PV_GW, hp), bias=pvc(PV_GB, hp)),
                     rd=YFK + ["pv"], wr=YFK)
                yield
                P.op("dve", lambda e: e.tensor_tensor(out=Yf, in0=Yf, in1=BON[:, 0:512], op=ALU.add), rd=YFK + [("f", boni)], wr=YFK)
                yield
                P.op("dve", lambda e: e.tensor_tensor(out=ybr[:, hp, :], in0=Yf, in1=sg[:, 0:512], op=ALU.mult),
                     rd=YFK + [("f", sgi)], wr=[("y", hp)])
                yield

            def drive(gb, ga, ratio):
                acc, a_done, b_done = 0.0, ga is None, gb is None
                while not (a_done and b_done):
                    if not b_done:
                        try:
                            next(gb)
                        except StopIteration:
                            b_done = True
                    if not a_done:
                        acc += ratio if not b_done else 1e9
                        while acc >= 1.0 and not a_done:
                            acc -= 1.0
                            try:
                                next(ga)
                            except StopIteration:
                                a_done = True

            drive(None, stage1(0, 0), 1.0)
            for hp in range(7):
                drive(stage2(hp, hp % 2), stage1(hp + 1, (hp + 1) % 2), 1.7)

            QS = [[(bt[0], 0), (bt[1], 1)], [(bt[2], 2), (bt[3], 3)]]
            GS = [[(ft[3], 3), (ft[4], 4)], [(ft[5], 5), (ft[6], 6)]]
            ex3 = bt_all[:, 4:6, :]
            exm3 = bt_all[:, 6:8, :]
            EXK, EXMK = [("b", 4), ("b", 5)], [("b", 6), ("b", 7)]

            def swa_pre():
                s = load_w(w_in[l], [(0, C_SK, 64), (64, C_SK, 64), (128, C_SK + 64, 64), (192, C_SK + 64, 64), (256, C_SV, 128)])
                for g in range(2):
                    for k4 in range(4):
                        for kc in range(k4 * 4, k4 * 4 + 4):
                            P.op("pe", lambda e, kc=kc, g=g: e.matmul(pp[g][:], lhsT=wbuf[s][:, kc, g * 128:(g + 1) * 128], rhs=hT[:, kc, :],
                                                                      start=(kc == 0), stop=(kc == 15)), rd=[("w", s), ("h", kc)], wr=[("pp", g)])
                        yield
                    P.op("act", lambda e, g=g: e.activation(out=kd[:, l * 2 + g, 128:640], in_=pp[g][:], func=AF.Copy), rd=[("pp", g)], wr=[("kd", g)])
                    yield
                for blk in range(4):
                    for k4 in range(4):
                        for kc in range(k4 * 4, k4 * 4 + 4):
                            P.op("pe", lambda e, kc=kc, blk=blk: e.matmul(pp[2][:, blk * 128:(blk + 1) * 128], lhsT=hT[:, kc, blk * 128:(blk + 1) * 128],
                                                                          rhs=wbuf[s][:, kc, 256:384], start=(kc == 0), stop=(kc == 15)),
                                 rd=[("w", s), ("h", kc)], wr=[("pp", 2)])
                        yield
                P.op("act", lambda e: e.activation(out=vt[:, l * 5 + 1:l * 5 + 5, :], in_=pp[2][:].rearrange("p (a b) -> p a b", b=128), func=AF.Copy),
                     rd=[("pp", 2)], wr=["vt"])
                yield

            def item_proj(i):
                par = i % 2
                if i < 4:
                    c0 = 2 * i
                    pieces = [(0, C_SQ + c0 * 128, 128), (128, C_SG + c0 * 128, 128), (256, C_SQ + (c0 + 1) * 128, 128), (384, C_SG + (c0 + 1) * 128, 128)]
                    qb, gb_ = [0, 2], [1, 3]
                else:
                    h = i - 4
                    pieces = [(0, C_XQ + h * 256, 256), (256, C_XG + h * 256, 256)]
                    qb, gb_ = [0, 1], [2, 3]
                s = load_w(w_in[l], pieces)
                for j in range(4):
                    for k4 in range(4):
                        for kc in range(k4 * 4, k4 * 4 + 4):
                            P.op("pe", lambda e, kc=kc, j=j: e.matmul(pp[j][:], lhsT=wbuf[s][:, kc, j * 128:(j + 1) * 128], rhs=hT[:, kc, :],
                                                                      start=(kc == 0), stop=(kc == 15)), rd=[("w", s), ("h", kc)], wr=[("pp", j)])
                        yield
                for j in range(2):
                    q, qi = QS[par][j]
                    gt, gi = GS[par][j]
                    P.op("act", lambda e, q=q, j=j: e.activation(out=q[:], in_=pp[qb[j]][:], func=AF.Copy), rd=[("pp", qb[j])], wr=[("b", qi)])
                    yield
                    act_silu(gt[:, 0:512], pp[gb_[j]][:], [("pp", gb_[j])], [("f", gi)])
                    yield

            def attn_swa(cp, par):
                for ci in range(2):
                    c = 2 * cp + ci
                    g = c // 4
                    qTb, qi = QS[par][ci]
                    sgs, gi = GS[par][ci]
                    for bp in range(2):
                        for h2 in range(2):
                            hs = slice(h2 * 64, (h2 + 1) * 64)
                            for bb in range(2):
                                blk = 2 * bp + bb
                                for w_ in range(2):
                                    P.op("pe", lambda e, hs=hs, h2=h2, w_=w_, blk=blk, bb=bb: e.matmul(
                                        pq[h2][:, (bb * 2 + w_) * 128:(bb * 2 + w_ + 1) * 128], lhsT=kd[hs, l * 2 + g, (blk + w_) * 128:(blk + w_ + 1) * 128],
                                        rhs=qTb[hs, blk * 128:(blk + 1) * 128], start=True, stop=True), rd=[("kd", g), ("b", qi)], wr=[("pq", h2)])
                        yield
                        for h2 in range(2):
                            P.op("act", lambda e, h2=h2: e.activation(out=ex3[:, h2, :], in_=pq[h2][:], func=AF.Exp, scale=0.125),
                                 rd=[("pq", h2)], wr=[EXK[h2]])
                        yield
                        if ti == 0 and bp == 0:
                            for bb in range(2):
                                mk = (maskS0 if bb == 0 else maskS).unsqueeze(1).broadcast_to([128, 2, 256])
                                P.op("dve", lambda e, mk=mk, bb=bb: e.tensor_tensor(out=exm3[:, :, bb * 256:(bb + 1) * 256], in0=ex3[:, :, bb * 256:(bb + 1) * 256],
                                                                                   in1=mk, op=ALU.mult), rd=EXK + ["ccb"], wr=EXMK)
                        else:
                            mk = maskS.unsqueeze(1).broadcast_to([128, 4, 256])
                            P.op("dve", lambda e, mk=mk: e.tensor_tensor(out=exm3.rearrange("p a (b c) -> p (a b) c", c=256),
                                                                       in0=ex3.rearrange("p a (b c) -> p (a b) c", c=256), in1=mk, op=ALU.mult),
                                 rd=EXK + ["ccb"], wr=EXMK)
                        yield
                        for h2 in range(2):
                            hs = slice(h2 * 64, (h2 + 1) * 64)
                            for bb in range(2):
                                blk = 2 * bp + bb
                                bs = slice(blk * 128, (blk + 1) * 128)
                                for w_ in range(2):
                                    P.op("pe", lambda e, hs=hs, h2=h2, w_=w_, blk=blk, bs=bs, bb=bb: e.matmul(
                                        pq[2][hs, bs], lhsT=vt[:, l * 5 + blk + w_, g * 64:(g + 1) * 64],
                                        rhs=exm3[:, h2, (bb * 2 + w_) * 128:(bb * 2 + w_ + 1) * 128], start=(w_ == 0), stop=(w_ == 1)),
                                        rd=["vt"] + EXMK, wr=[("pq", 2)])
                                for w_ in range(2):
                                    P.op("pe", lambda e, hs=hs, h2=h2, w_=w_, bs=bs, bb=bb: e.matmul(
                                        pq[3][hs, bs], lhsT=onesB[:, 0:64], rhs=exm3[:, h2, (bb * 2 + w_) * 128:(bb * 2 + w_ + 1) * 128],
                                        start=(w_ == 0), stop=(w_ == 1)), rd=["ccb"] + EXMK, wr=[("pq", 3)])
                        yield
                    DEN = ft[7]
                    act_rpow(DEN[:, 0:512], pq[3][:], [("pq", 3), "esink"], [("f", 7)], 1.0, bias=esink[:, l * 8 + c:l * 8 + c + 1])
                    yield
                    P.op("dve", lambda e: e.tensor_tensor(out=DEN[:, 0:512], in0=pq[2][:], in1=DEN[:, 0:512], op=ALU.mult),
                         rd=[("pq", 2), ("f", 7)], wr=[("f", 7)])
                    yield
                    P.op("dve", lambda e, c=c: e.tensor_tensor(out=ybr[:, 8 + c, :], in0=DEN[:, 0:512], in1=sgs[:, 0:512], op=ALU.mult),
                         rd=[("f", 7), ("f", gi)], wr=[("y", 8 + c)])
                    yield

            def attn_xa(h, par):
                qx = [QS[par][j][0] for j in range(2)]
                qxi = [QS[par][j][1] for j in range(2)]
                sgx = [GS[par][j][0] for j in range(2)]
                sgi = [GS[par][j][1] for j in range(2)]
                for mt in range(2):
                    for j in range(2):
                        P.op("pe", lambda e, mt=mt, j=j: e.matmul(pq[mt][:], lhsT=kmT[:, l * 8 + h * 2 + j, mt * 128:(mt + 1) * 128], rhs=qx[j][:],
                                                                  start=(j == 0), stop=(j == 1)), rd=["kmT", ("b", qxi[j])], wr=[("pq", mt)])
                    yield
                    P.op("act", lambda e, mt=mt: e.activation(out=ex3[:, mt, :], in_=pq[mt][:], func=AF.Exp, scale=1.0 / 16), rd=[("pq", mt)], wr=[EXK[mt]])
                    yield
                for j in range(2):
                    for mt in range(2):
                        P.op("pe", lambda e, mt=mt, j=j: e.matmul(pq[2 + j][:], lhsT=vm[:, l * 2 + mt, h * 256 + j * 128:h * 256 + (j + 1) * 128],
                                                                  rhs=ex3[:, mt, :], start=(mt == 0), stop=(mt == 1)),
                             rd=["vm", EXK[mt]], wr=[("pq", 2 + j)])
                yield
                for mt in range(2):
                    P.op("pe", lambda e, mt=mt: e.matmul(pq[0][:], lhsT=onesB, rhs=ex3[:, mt, :], start=(mt == 0), stop=(mt == 1)),
                         rd=["ccb", EXK[mt]], wr=[("pq", 0)])
                yield
                REC = ft[7]
                act_rpow(REC[:, 0:512], pq[0][:], [("pq", 0)], [("f", 7)], 1.0)
                yield
                for j in range(2):
                    P.op("dve", lambda e, j=j: e.tensor_tensor(out=sgx[j][:, 0:512], in0=sgx[j][:, 0:512], in1=REC[:, 0:512], op=ALU.mult),
                         rd=[("f", sgi[j]), ("f", 7)], wr=[("f", sgi[j])])
                    yield
                    P.op("dve", lambda e, j=j: e.tensor_tensor(out=ybr[:, 16 + h * 2 + j, :], in0=pq[2 + j][:], in1=sgx[j][:, 0:512], op=ALU.mult),
                         rd=[("pq", 2 + j), ("f", sgi[j])], wr=[("y", 16 + h * 2 + j)])
                    yield

            def chain(*gens):
                for g_ in gens:
                    yield from g_

            drive(stage2(7, 1), chain(swa_pre(), item_proj(0)), 1.3)
            for i in range(8):
                at = attn_swa(i, i % 2) if i < 4 else attn_xa(i - 4, i % 2)
                drive(at, item_proj(i + 1) if i < 7 else None, 1.2 if i < 4 else 2.5)
            for g in range(2):
                P.op("dve", lambda e, g=g: e.tensor_copy(out=kd[:, l * 2 + g, 0:128], in_=kd[:, l * 2 + g, 512:640]), rd=[("kd", g)], wr=[("kd", g)])
            P.op("dve", lambda e: e.tensor_copy(out=vt[:, l * 5, :], in_=vt[:, l * 5 + 4, :]), rd=["vt"], wr=["vt"])

            for dg in range(4):
                for br in range(3):
                    s = load_w(w_in[l], [(0, C_MG + br * 2048 + dg * 512, 512)])
                    for j in range(4):
                        proj_h(s, j, j)
                        act_sigmoid(ft[3 + j][:, 0:512], pp[j][:], [("pp", j)], [("f", 3 + j)])
                    s = load_w(w_up[br][l], [(0, dg * 512, 512)], nk=8)
                    for j in range(4):
                        proj(s, j, pq[j][:], ("pq", j), lambda kc, br=br: ybr[:, br * 8 + kc, :], lambda kc, br=br: [("y", br * 8 + kc)], nk=8)
                        acc = ft[7 + j]
                        if br == 0:
                            P.op("dve", lambda e, j=j, acc=acc: e.tensor_tensor(out=acc[:, 0:512], in0=pq[j][:], in1=ft[3 + j][:, 0:512], op=ALU.mult),
                                 rd=[("pq", j), ("f", 3 + j)], wr=[("f", 7 + j)])
                        else:
                            P.op("dve", lambda e, j=j: e.tensor_tensor(out=ft[3 + j][:, 0:512], in0=pq[j][:], in1=ft[3 + j][:, 0:512], op=ALU.mult),
                                 rd=[("pq", j), ("f", 3 + j)], wr=[("f", 3 + j)])
                            if br == 1:
                                P.op("dve", lambda e, j=j, acc=acc: e.tensor_tensor(out=acc[:, 0:512], in0=acc[:, 0:512], in1=ft[3 + j][:, 0:512], op=ALU.add),
                                     rd=[("f", 7 + j), ("f", 3 + j)], wr=[("f", 7 + j)])
                            else:
                                dc = dg * 4 + j
                                P.op("dve", lambda e, j=j, acc=acc, dc=dc: e.tensor_tensor(out=bt[dc][:], in0=acc[:, 0:512], in1=ft[3 + j][:, 0:512], op=ALU.add),
                                     rd=[("f", 7 + j), ("f", 3 + j)], wr=[("b", dc)])
            for dg in range(4):
                s = load_w(w_out[l], [(0, dg * 512, 512)])
                for j in range(4):
                    dc = dg * 4 + j
                    proj(s, j, pp[j][:], ("pp", j), lambda kc: bt[kc][:], lambda kc: [("b", kc)])
                    P.op("act", lambda e, j=j, dc=dc: e.activation(out=o_f[:, dc, :], in_=pp[j][:], func=AF.Copy), rd=[("pp", j)], wr=okeys(dc))
                    bi = 27 + (dc % 2)
                    t = bt[bi]
                    P.op("act", lambda e, dc=dc, t=t: e.activation(out=t[:], in_=o_f[:, dc, :], func=AF.Square), rd=okeys(dc), wr=[("b", bi)])
                    P.op("pe", lambda e, dc=dc, t=t: e.matmul(pq[0][:], lhsT=onesB, rhs=t[:], start=(dc == 0), stop=(dc == 15)),
                         rd=[("b", bi), "ccb"], wr=[("pq", 0)])
            rs2 = ft[2]
            act_rpow(rs2[:, 0:512], pq[0][:], [("pq", 0)], [("f", 2)], 0.5, scale=1.0 / D, bias=1e-6)
            for dc in range(16):
                t = ft[3 + (dc % 2)]
                P.op("dve", lambda e, dc=dc, t=t: e.scalar_tensor_tensor(out=t[:, 0:512], in0=o_f[:, dc, :], scalar=pvc(PV_GPOST, dc), in1=rs2[:, 0:512],
                                                                         op0=ALU.mult, op1=ALU.mult), rd=okeys(dc) + ["pv", ("f", 2)], wr=[("f", 3 + dc % 2)])
                P.op("dve", lambda e, dc=dc, t=t: e.tensor_tensor(out=x_res[:, dc, :], in0=x_res[:, dc, :], in1=t[:, 0:512], op=ALU.add),
                     rd=[("x", dc), ("f", 3 + dc % 2)], wr=[("x", dc)])
            if l == L - 1:
                for q in range(4):
                    P.dma("sp", lambda e, q=q: e.dma_start(out=outT[q * 512:(q + 1) * 512, t0:t0 + TT].rearrange("(dc p) t -> p dc t", p=128),
                                                           in_=x_res[:, q * 4:(q + 1) * 4, :]),
                          f"o{q}", rd=[("x", q * 4 + i) for i in range(4)], wr=[("out", q)])

        for ti in range(NT):
            for l in range(L):
                block(ti, l)
        P.wait_all("sp", [("out", q) for q in range(4)])
        P.emit()
        print("instructions:", P.n_inst, "sems:", P.sem_id)
    return nc


def _consts():
    cf = np.zeros((128, NCC), np.float32)
    cb = np.zeros((128, NCC), np.float32)
    p = np.arange(128)[:, None]
    c = np.arange(128)[None, :]
    cf[:, CF_ONES:CF_ONES + 128] = 1.0
    cb[:, CB_ONES:CB_ONES + 128] = 1.0
    cb[:, CB_BLK:CB_BLK + 128] = (p // 64 == c // 64)
    cb[:, CB_ID:CB_ID + 128] = (p == c)
    j = p % 64
    t = np.arange(64)[None, :]
    cf[:, CF_MA:CF_MA + 64] = (j <= t)
    cf[:, CF_MA + 64:CF_MA + 128] = (j < t)
    cf[:, CF_ML:CF_ML + 64] = (t < j)
    cf[:, CF_II:CF_II + 64] = (j == t)
    q = np.arange(128)[None, :]
    cb[:, CB_MS:CB_MS + 128] = (p > q)
    cb[:, CB_MS + 128:CB_MS + 256] = (q >= p)
    cb[:, CB_MS0 + 128:CB_MS0 + 256] = (q >= p)
    sc = np.ones((128, 512), np.float32)
    sc[:, 0::64] = 0.0
    cf[:, CF_SCAN:CF_SCAN + 512] = sc
    return cf, cb


def _layout(inputs, L):
    col = lambda v, n: np.ascontiguousarray(v.reshape(n, 128).T)
    pvs = []
    for l in range(L):
        sk = np.repeat(inputs["attn_sinks"][l], 64)
        pvs += [col(inputs["g_pre"][l], 16), col(inputs["g_post"][l], 16), col(inputs["g_mem"][l], 16),
                col(inputs["mu_shift"][l], 25), col(inputs["decay_base"][l], 8), col(inputs["iclr_base"][l], 8),
                col(inputs["k_k"][l], 8), col(inputs["k_a"][l], 8), col(inputs["r_k"][l].reshape(-1), 8),
                col(inputs["gn_w"][l], 8), col(inputs["gn_b"][l], 8), col(sk, 8)]
    pvd = np.ascontiguousarray(np.concatenate(pvs, axis=1), dtype=np.float32)
    dwd = np.ascontiguousarray(np.concatenate([inputs["decay_up"][:L], inputs["iclr_up"][:L]], axis=1), dtype=np.float32)
    return pvd, dwd


def run(inputs, T, L, B, trace=False):
    inputs = {k: np.asarray(v, dtype=np.float32) for k, v in inputs.items()}
    nc = build(T, L)
    pvd, dwd = _layout(inputs, L)
    cf, cb = _consts()
    shared = {
        "w_in": np.ascontiguousarray(inputs["w_in"][:L]), "w_mkv": np.ascontiguousarray(inputs["w_mem_kv"][:L]),
        "w_up0": np.ascontiguousarray(inputs["w_up_rwkv"][:L]), "w_up1": np.ascontiguousarray(inputs["w_up_swa"][:L]),
        "w_up2": np.ascontiguousarray(inputs["w_up_xattn"][:L]), "w_out": np.ascontiguousarray(inputs["w_out"][:L]),
        "dwd": dwd, "pvd": pvd, "ccdf": cf, "ccdb": cb,
    }
    in_maps = []
    for b in range(B):
        m = dict(shared)
        m["xT"] = np.ascontiguousarray(inputs["x"][b].T)
        m["memT"] = np.ascontiguousarray(inputs["mem"][b].T)
        in_maps.append(m)
    res = run_bass_kernel_spmd(nc, in_maps, core_ids=list(range(B)), trace=trace)
    out = np.stack([np.ascontiguousarray(r["outT"].T) for r in res.results], axis=0)
    return out.astype(np.float32), res


def kernel(**inputs):
    out, _ = run(inputs, 2048, 2, 8)
    return out
```
